# Optimizing a Trainium2 kernel written in Bass

```python
import math
import jax, jax.numpy as jnp
from jax import lax
import numpy as np

D_MODEL = 1024
BATCH = 2
SEQ = 16384
DEPTH = 2

HEAD_DIM = 64
EPS = 1e-6
S5_WIDTH = 256
S5_GROUP = 16
S5_GROUPS = S5_WIDTH // S5_GROUP
S5_STATE = 64
CONV_WIDTH = D_MODEL - S5_WIDTH
CONV_HEADS = CONV_WIDTH // HEAD_DIM
CONV_K = 3
IN_EVEN = S5_WIDTH + 3 * CONV_WIDTH
MOBA_HEADS = 4
NSA_HEADS = 12
NSA_KV_HEADS = 2
NSA_GROUP = NSA_HEADS // NSA_KV_HEADS
MOBA_W = MOBA_HEADS * HEAD_DIM
NSA_W = NSA_HEADS * HEAD_DIM
KV_W = NSA_KV_HEADS * HEAD_DIM
IN_ODD = 3 * MOBA_W + NSA_W + 6 * KV_W + 3 * NSA_HEADS
MOBA_BLOCK = 256
MOBA_TOPK = 3
CMP_BLOCK = 32
CMP_STRIDE = 16
CMP_HIDDEN = 256
SEL_BLOCK = 64
SEL_TOPK = 8
WINDOW = 512
Q_BLOCK = 64
FORCED_SCORE = 1e4
N_GROUPS = 4
EXPERTS_PER_GROUP = 4
N_EXPERTS = N_GROUPS * EXPERTS_PER_GROUP
EXPERT_TOPK = 2
EXPERT_FF = 256

kernel_name = 'hybrid_s5_conv_moba_nsa_hmoe'


def rms_norm(x, g):
    xf = x.astype(jnp.float32)
    y = xf * lax.rsqrt(jnp.mean(xf * xf, axis=-1, keepdims=True) + EPS)
    return (y * g.astype(jnp.float32)).astype(x.dtype)


def masked_softmax(s, mask):
    s = jnp.where(mask, s.astype(jnp.float32), -jnp.inf)
    m = jnp.max(s, axis=-1, keepdims=True)
    m = jnp.where(jnp.isfinite(m), m, 0.0)
    p = jnp.exp(s - m)
    return p / jnp.maximum(jnp.sum(p, axis=-1, keepdims=True), 1e-30)


def gather_blocks(blocks, idx):
    return jax.vmap(jax.vmap(lambda b, i: b[i]))(blocks, idx)


def s5_mixer(u, lam_re, lam_im, log_dt, b_re, b_im, c_re, c_im, d, w_glu):
    B, L, _ = u.shape
    uf = u.astype(jnp.float32).reshape(B, L, S5_GROUPS, S5_GROUP)
    lr = lam_re.astype(jnp.float32)
    li = lam_im.astype(jnp.float32)
    dt = jnp.exp(log_dt.astype(jnp.float32))[:, None]
    mag = jnp.exp(lr * dt)
    a_re = mag * jnp.cos(li * dt)
    a_im = mag * jnp.sin(li * dt)
    den = lr * lr + li * li
    f_re = ((a_re - 1.0) * lr + a_im * li) / den
    f_im = (a_im * lr - (a_re - 1.0) * li) / den
    br = b_re.astype(jnp.float32)
    bi = b_im.astype(jnp.float32)
    bb_re = f_re[..., None] * br - f_im[..., None] * bi
    bb_im = f_re[..., None] * bi + f_im[..., None] * br
    bu_re = jnp.einsum('blgh,gph->blgp', uf, bb_re)
    bu_im = jnp.einsum('blgh,gph->blgp', uf, bb_im)
    ar = jnp.broadcast_to(a_re, bu_re.shape)
    ai = jnp.broadcast_to(a_im, bu_re.shape)

    def combine(e1, e2):
        a1r, a1i, b1r, b1i = e1
        a2r, a2i, b2r, b2i = e2
        return (a2r * a1r - a2i * a1i, a2r * a1i + a2i * a1r,
                a2r * b1r - a2i * b1i + b2r, a2r * b1i + a2i * b1r + b2i)

    _, _, xr, xi = lax.associative_scan(combine, (ar, ai, bu_re, bu_im), axis=1)
    y = (jnp.einsum('blgp,ghp->blgh', xr, c_re.astype(jnp.float32))
         - jnp.einsum('blgp,ghp->blgh', xi, c_im.astype(jnp.float32))
         + d.astype(jnp.float32).reshape(S5_GROUPS, S5_GROUP) * uf)
    y = jax.nn.gelu(y.reshape(B, L, S5_WIDTH))
    y = y * jax.nn.sigmoid(y @ w_glu.astype(jnp.float32))
    return y.astype(u.dtype)


def short_conv_mixer(xc, gb, gc, conv_w, conv_b):
    z = gc * xc
    w = conv_w.astype(z.dtype)[:, None, :]
    y = lax.conv_general_dilated(z, w, window_strides=(1,), padding=[(CONV_K - 1, 0)],
                                 dimension_numbers=('NWC', 'WIO', 'NWC'),
                                 feature_group_count=CONV_WIDTH) + conv_b
    return gb * y


def even_mixer(h, w_in, w_out, lam_re, lam_im, log_dt, b_re, b_im, c_re, c_im, d, w_glu, conv_w, conv_b):
    proj = h @ w_in
    u, xc, gb, gc = jnp.split(proj, [S5_WIDTH, S5_WIDTH + CONV_WIDTH, S5_WIDTH + 2 * CONV_WIDTH], axis=-1)
    y_a = s5_mixer(u, lam_re, lam_im, log_dt, b_re, b_im, c_re, c_im, d, w_glu)
    y_b = short_conv_mixer(xc, gb, gc, conv_w, conv_b)
    return jnp.concatenate([y_a, y_b.astype(y_a.dtype)], axis=-1) @ w_out


def compress_blocks(k, pe, w1, w2):
    B, L, H, hd = k.shape
    c = k.reshape(B, L // CMP_STRIDE, CMP_STRIDE, H, hd)
    blocks = jnp.concatenate([c[:, :-1], c[:, 1:]], axis=2) + pe[:, None, :]
    flat = blocks.transpose(0, 1, 3, 2, 4).reshape(B, -1, H, CMP_BLOCK * hd)
    return jax.nn.gelu(flat @ w1) @ w2


def odd_mixer(h, w_in, w_out, moba_q_norm, moba_k_norm, nsa_q_norm, nsa_kcmp_norm, nsa_ksel_norm,
              nsa_kwin_norm, cmp_pe_k, cmp_w1_k, cmp_w2_k, cmp_pe_v, cmp_w1_v, cmp_w2_v):
    B, L, _ = h.shape
    hd = HEAD_DIM
    Lp = -(-L // MOBA_BLOCK) * MOBA_BLOCK
    hp = jnp.pad(h, ((0, 0), (0, Lp - L), (0, 0)))
    proj = hp @ w_in
    sizes = [MOBA_W] * 3 + [NSA_W] + [KV_W] * 6 + [3 * NSA_HEADS]
    qm, km, vm, qd, kc, vc, ks, vs, kw, vw, gl = jnp.split(proj, np.cumsum(sizes)[:-1].tolist(), axis=-1)
    qm = rms_norm(qm.reshape(B, Lp, MOBA_HEADS, hd), moba_q_norm)
    km = rms_norm(km.reshape(B, Lp, MOBA_HEADS, hd), moba_k_norm)
    vm = vm.reshape(B, Lp, MOBA_HEADS, hd)
    qd = rms_norm(qd.reshape(B, Lp, NSA_KV_HEADS, NSA_GROUP, hd), nsa_q_norm)
    kv_shape = (B, Lp, NSA_KV_HEADS, hd)
    k_cmp = rms_norm(compress_blocks(kc.reshape(kv_shape), cmp_pe_k, cmp_w1_k, cmp_w2_k), nsa_kcmp_norm)
    v_cmp = compress_blocks(vc.reshape(kv_shape), cmp_pe_v, cmp_w1_v, cmp_w2_v)
    n_cmp = k_cmp.shape[1]
    n_sel = Lp // SEL_BLOCK
    k_sel = rms_norm(ks.reshape(kv_shape), nsa_ksel_norm).reshape(
        B, n_sel, SEL_BLOCK, NSA_KV_HEADS, hd).transpose(0, 3, 1, 2, 4)
    v_sel = vs.reshape(B, n_sel, SEL_BLOCK, NSA_KV_HEADS, hd).transpose(0, 3, 1, 2, 4)
    pad_w = ((0, 0), (WINDOW, 0), (0, 0), (0, 0))
    k_win = jnp.pad(rms_norm(kw.reshape(kv_shape), nsa_kwin_norm), pad_w)
    v_win = jnp.pad(vw.reshape(kv_shape), pad_w)
    gates = jax.nn.sigmoid(gl.astype(jnp.float32)).reshape(B, Lp, 3, NSA_KV_HEADS, NSA_GROUP)
    n_mb = Lp // MOBA_BLOCK
    km_blocks = km.reshape(B, n_mb, MOBA_BLOCK, MOBA_HEADS, hd).transpose(0, 3, 1, 2, 4)
    vm_blocks = vm.reshape(B, n_mb, MOBA_BLOCK, MOBA_HEADS, hd).transpose(0, 3, 1, 2, 4)
    km_mean = jnp.mean(km_blocks.astype(jnp.float32), axis=3)
    k_moba = min(MOBA_TOPK, n_mb)
    k_nsa = min(SEL_TOPK, n_sel)
    scale = hd ** -0.5
    cmp_end = jnp.arange(n_cmp) * CMP_STRIDE + CMP_BLOCK - 1
    blk_ids = jnp.arange(n_sel)
    mb_ids = jnp.arange(n_mb)

    def block(qi):
        s = qi * Q_BLOCK
        t = s + jnp.arange(Q_BLOCK)
        q = lax.dynamic_slice_in_dim(qm, s, Q_BLOCK, axis=1)
        cur = s // MOBA_BLOCK
        k_own = lax.dynamic_slice_in_dim(km, cur * MOBA_BLOCK, MOBA_BLOCK, axis=1)
        v_own = lax.dynamic_slice_in_dim(vm, cur * MOBA_BLOCK, MOBA_BLOCK, axis=1)
        own_pos = cur * MOBA_BLOCK + jnp.arange(MOBA_BLOCK)
        s_own = jnp.einsum('bqhd,bkhd->bhqk', q, k_own) * scale
        m_own = jnp.broadcast_to(own_pos[None, :] <= t[:, None], s_own.shape)
        gate = jnp.einsum('bqhd,bhnd->bhqn', q.astype(jnp.float32), km_mean)
        gate = jnp.where(mb_ids < cur, gate, -jnp.inf)
        g_val, g_idx = lax.top_k(gate, k_moba)
        k_g = gather_blocks(km_blocks, g_idx)
        v_g = gather_blocks(vm_blocks, g_idx)
        s_g = jnp.einsum('bqhd,bhqnsd->bhqns', q, k_g).reshape(
            B, MOBA_HEADS, Q_BLOCK, k_moba * MOBA_BLOCK) * scale
        m_g = jnp.broadcast_to(jnp.isfinite(g_val)[..., None],
                               (B, MOBA_HEADS, Q_BLOCK, k_moba, MOBA_BLOCK)).reshape(s_g.shape)
        p = masked_softmax(jnp.concatenate([s_own, s_g], axis=-1), jnp.concatenate([m_own, m_g], axis=-1))
        o_moba = (jnp.einsum('bhqk,bkhd->bqhd', p[..., :MOBA_BLOCK], v_own)
                  + jnp.einsum('bhqm,bhqmd->bqhd', p[..., MOBA_BLOCK:],
                               v_g.reshape(B, MOBA_HEADS, Q_BLOCK, k_moba * MOBA_BLOCK, hd)))
        qn = lax.dynamic_slice_in_dim(qd, s, Q_BLOCK, axis=1)
        s_c = jnp.einsum('bqhgd,bnhd->bhgqn', qn, k_cmp) * scale
        p_c = masked_softmax(s_c, cmp_end[None, :] <= t[:, None])
        o_c = jnp.einsum('bhgqn,bnhd->bqhgd', p_c, v_cmp)
        imp = jnp.pad(p_c.sum(axis=2), ((0, 0), (0, 0), (0, 0), (0, 1)))
        r = imp.reshape(B, NSA_KV_HEADS, Q_BLOCK, n_sel, SEL_BLOCK // CMP_STRIDE)
        p_slc = r.sum(-1) + jnp.pad(r[..., :-1, -1], ((0, 0), (0, 0), (0, 0), (1, 0)))
        cur_s = (t // SEL_BLOCK)[:, None]
        forced = (blk_ids == 0) | (blk_ids == cur_s) | (blk_ids == cur_s - 1)
        score = jnp.where(forced, FORCED_SCORE, jnp.where(blk_ids < cur_s, p_slc, -jnp.inf))
        s_val, s_idx = lax.top_k(score, k_nsa)
        k_b = gather_blocks(k_sel, s_idx)
        v_b = gather_blocks(v_sel, s_idx)
        pos = s_idx[..., None] * SEL_BLOCK + jnp.arange(SEL_BLOCK)
        m_s = (jnp.isfinite(s_val)[..., None] & (pos <= t[:, None, None])).reshape(
            B, NSA_KV_HEADS, Q_BLOCK, k_nsa * SEL_BLOCK)[:, :, None]
        s_s = jnp.einsum('bqhgd,bhqnsd->bhgqns', qn, k_b).reshape(
            B, NSA_KV_HEADS, NSA_GROUP, Q_BLOCK, k_nsa * SEL_BLOCK) * scale
        o_s = jnp.einsum('bhgqm,bhqmd->bqhgd', masked_softmax(s_s, m_s),
                         v_b.reshape(B, NSA_KV_HEADS, Q_BLOCK, k_nsa * SEL_BLOCK, hd))
        k_w = lax.dynamic_slice_in_dim(k_win, s, Q_BLOCK + WINDOW, axis=1)
        v_w = lax.dynamic_slice_in_dim(v_win, s, Q_BLOCK + WINDOW, axis=1)
        wpos = s - WINDOW + jnp.arange(Q_BLOCK + WINDOW)
        m_w = (wpos[None, :] <= t[:, None]) & (wpos[None, :] > t[:, None] - WINDOW) & (wpos[None, :] >= 0)
        s_w = jnp.einsum('bqhgd,bkhd->bhgqk', qn, k_w) * scale
        o_w = jnp.einsum('bhgqk,bkhd->bqhgd', masked_softmax(s_w, m_w), v_w)
        g = lax.dynamic_slice_in_dim(gates, s, Q_BLOCK, axis=1)
        o_nsa = (g[:, :, 0][..., None] * o_c + g[:, :, 1][..., None] * o_s
                 + g[:, :, 2][..., None] * o_w)
        o = jnp.concatenate([o_moba.reshape(B, Q_BLOCK, MOBA_W),
                             o_nsa.reshape(B, Q_BLOCK, NSA_W)], axis=-1)
        return o.astype(h.dtype)

    out = lax.map(block, jnp.arange(Lp // Q_BLOCK))
    out = out.transpose(1, 0, 2, 3).reshape(B, Lp, D_MODEL)[:, :L]
    return out @ w_out


def hier_moe(h, w_group, b_group, w_expert, b_expert, w_gate, w_up, w_down):
    B, L, D = h.shape
    xt = h.reshape(B * L, D)
    gl = (xt @ w_group).astype(jnp.float32) + b_group.astype(jnp.float32)
    gp = jax.nn.softmax(gl, axis=-1)
    gi = jnp.argmax(gl, axis=-1)
    gw = jnp.take_along_axis(gp, gi[:, None], axis=-1)
    el = ((xt @ w_expert).astype(jnp.float32) + b_expert.astype(jnp.float32)).reshape(
        B * L, N_GROUPS, EXPERTS_PER_GROUP)
    el = jnp.take_along_axis(el, gi[:, None, None], axis=1)[:, 0]
    ev, ei = lax.top_k(jax.nn.softmax(el, axis=-1), EXPERT_TOPK)
    ev = ev / jnp.sum(ev, axis=-1, keepdims=True)
    eidx = gi[:, None] * EXPERTS_PER_GROUP + ei
    gate = jnp.sum(jax.nn.one_hot(eidx, N_EXPERTS, dtype=jnp.float32) * (gw * ev)[..., None], axis=1)
    h1 = jnp.einsum('td,edf->tef', xt, w_gate)
    h3 = jnp.einsum('td,edf->tef', xt, w_up)
    act = jax.nn.silu(h1) * h3 * gate[..., None].astype(h1.dtype)
    y = jnp.einsum('tef,efd->td', act, w_down)
    return y.reshape(B, L, D).astype(h.dtype)


def setup_inputs(seed: int = 0) -> dict:
    key = jax.random.key(seed)
    ks = iter(jax.random.split(key, 64))
    NE = (DEPTH + 1) // 2
    NO = DEPTH // 2

    def nrm(shape, scale):
        return jax.random.normal(next(ks), shape, jnp.float32) * scale

    def gain(shape):
        return 1.0 + nrm(shape, 0.01)

    x = nrm((BATCH, SEQ, D_MODEL), 1.0)
    ev_norm_mix = gain((NE, D_MODEL))
    ev_w_in = nrm((NE, D_MODEL, IN_EVEN), D_MODEL ** -0.5)
    ev_w_out = nrm((NE, D_MODEL, D_MODEL), D_MODEL ** -0.5)
    s5_lam_re = -0.5 * jnp.exp(nrm((NE, S5_GROUPS, S5_STATE), 0.1))
    s5_lam_im = math.pi * jnp.arange(S5_STATE, dtype=jnp.float32) + nrm((NE, S5_GROUPS, S5_STATE), 0.01)
    s5_log_dt = jax.random.uniform(next(ks), (NE, S5_GROUPS), jnp.float32, math.log(1e-3), math.log(1e-1))
    s5_b_re = nrm((NE, S5_GROUPS, S5_STATE, S5_GROUP), (2 * S5_GROUP) ** -0.5)
    s5_b_im = nrm((NE, S5_GROUPS, S5_STATE, S5_GROUP), (2 * S5_GROUP) ** -0.5)
    s5_c_re = nrm((NE, S5_GROUPS, S5_GROUP, S5_STATE), S5_STATE ** -0.5)
    s5_c_im = nrm((NE, S5_GROUPS, S5_GROUP, S5_STATE), S5_STATE ** -0.5)
    s5_d = nrm((NE, S5_WIDTH), 0.5)
    s5_w_glu = nrm((NE, S5_WIDTH, S5_WIDTH), S5_WIDTH ** -0.5)
    conv_w = nrm((NE, CONV_K, CONV_WIDTH), CONV_K ** -0.5)
    conv_b = nrm((NE, CONV_WIDTH), 0.01)
    od_norm_mix = gain((NO, D_MODEL))
    od_w_in = nrm((NO, D_MODEL, IN_ODD), D_MODEL ** -0.5)
    od_w_out = nrm((NO, D_MODEL, D_MODEL), D_MODEL ** -0.5)
    moba_q_norm = gain((NO, HEAD_DIM))
    moba_k_norm = gain((NO, HEAD_DIM))
    nsa_q_norm = gain((NO, HEAD_DIM))
    nsa_kcmp_norm = gain((NO, HEAD_DIM))
    nsa_ksel_norm = gain((NO, HEAD_DIM))
    nsa_kwin_norm = gain((NO, HEAD_DIM))
    cmp_pe_k = nrm((NO, CMP_BLOCK, HEAD_DIM), 0.02)
    cmp_w1_k = nrm((NO, CMP_BLOCK * HEAD_DIM, CMP_HIDDEN), (CMP_BLOCK * HEAD_DIM) ** -0.5)
    cmp_w2_k = nrm((NO, CMP_HIDDEN, HEAD_DIM), CMP_HIDDEN ** -0.5)
    cmp_pe_v = nrm((NO, CMP_BLOCK, HEAD_DIM), 0.02)
    cmp_w1_v = nrm((NO, CMP_BLOCK * HEAD_DIM, CMP_HIDDEN), (CMP_BLOCK * HEAD_DIM) ** -0.5)
    cmp_w2_v = nrm((NO, CMP_HIDDEN, HEAD_DIM), CMP_HIDDEN ** -0.5)
    moe_norm = gain((DEPTH, D_MODEL))
    moe_w_group = nrm((DEPTH, D_MODEL, N_GROUPS), D_MODEL ** -0.5)
    moe_b_group = nrm((DEPTH, N_GROUPS), 0.01)
    moe_w_expert = nrm((DEPTH, D_MODEL, N_EXPERTS), D_MODEL ** -0.5)
    moe_b_expert = nrm((DEPTH, N_EXPERTS), 0.01)
    moe_w_gate = nrm((DEPTH, N_EXPERTS, D_MODEL, EXPERT_FF), D_MODEL ** -0.5)
    moe_w_up = nrm((DEPTH, N_EXPERTS, D_MODEL, EXPERT_FF), D_MODEL ** -0.5)
    moe_w_down = nrm((DEPTH, N_EXPERTS, EXPERT_FF, D_MODEL), EXPERT_FF ** -0.5)
    return {'x': x, 'ev_norm_mix': ev_norm_mix, 'ev_w_in': ev_w_in, 'ev_w_out': ev_w_out,
            's5_lam_re': s5_lam_re, 's5_lam_im': s5_lam_im, 's5_log_dt': s5_log_dt,
            's5_b_re': s5_b_re, 's5_b_im': s5_b_im, 's5_c_re': s5_c_re, 's5_c_im': s5_c_im,
            's5_d': s5_d, 's5_w_glu': s5_w_glu, 'conv_w': conv_w, 'conv_b': conv_b,
            'od_norm_mix': od_norm_mix, 'od_w_in': od_w_in, 'od_w_out': od_w_out,
            'moba_q_norm': moba_q_norm, 'moba_k_norm': moba_k_norm, 'nsa_q_norm': nsa_q_norm,
            'nsa_kcmp_norm': nsa_kcmp_norm, 'nsa_ksel_norm': nsa_ksel_norm, 'nsa_kwin_norm': nsa_kwin_norm,
            'cmp_pe_k': cmp_pe_k, 'cmp_w1_k': cmp_w1_k, 'cmp_w2_k': cmp_w2_k,
            'cmp_pe_v': cmp_pe_v, 'cmp_w1_v': cmp_w1_v, 'cmp_w2_v': cmp_w2_v,
            'moe_norm': moe_norm, 'moe_w_group': moe_w_group, 'moe_b_group': moe_b_group,
            'moe_w_expert': moe_w_expert, 'moe_b_expert': moe_b_expert,
            'moe_w_gate': moe_w_gate, 'moe_w_up': moe_w_up, 'moe_w_down': moe_w_down}


def reference(x, ev_norm_mix, ev_w_in, ev_w_out, s5_lam_re, s5_lam_im, s5_log_dt, s5_b_re, s5_b_im,
              s5_c_re, s5_c_im, s5_d, s5_w_glu, conv_w, conv_b, od_norm_mix, od_w_in, od_w_out,
              moba_q_norm, moba_k_norm, nsa_q_norm, nsa_kcmp_norm, nsa_ksel_norm, nsa_kwin_norm,
              cmp_pe_k, cmp_w1_k, cmp_w2_k, cmp_pe_v, cmp_w1_v, cmp_w2_v,
              moe_norm, moe_w_group, moe_b_group, moe_w_expert, moe_b_expert,
              moe_w_gate, moe_w_up, moe_w_down):
    h = x
    for layer in range(DEPTH):
        i = layer // 2
        if layer % 2 == 0:
            h = h + even_mixer(rms_norm(h, ev_norm_mix[i]), ev_w_in[i], ev_w_out[i],
                               s5_lam_re[i], s5_lam_im[i], s5_log_dt[i], s5_b_re[i], s5_b_im[i],
                               s5_c_re[i], s5_c_im[i], s5_d[i], s5_w_glu[i], conv_w[i], conv_b[i])
        else:
            h = h + odd_mixer(rms_norm(h, od_norm_mix[i]), od_w_in[i], od_w_out[i],
                              moba_q_norm[i], moba_k_norm[i], nsa_q_norm[i], nsa_kcmp_norm[i],
                              nsa_ksel_norm[i], nsa_kwin_norm[i], cmp_pe_k[i], cmp_w1_k[i], cmp_w2_k[i],
                              cmp_pe_v[i], cmp_w1_v[i], cmp_w2_v[i])
        h = h + hier_moe(rms_norm(h, moe_norm[layer]), moe_w_group[layer], moe_b_group[layer],
                         moe_w_expert[layer], moe_b_expert[layer], moe_w_gate[layer],
                         moe_w_up[layer], moe_w_down[layer])
    return h
```

```python
import math
import numpy as np
import ml_dtypes
import numpy as np
import concourse.bass as bass
import concourse.mybir as mybir
from concourse.bass_utils import run_bass_kernel_spmd

F32 = mybir.dt.float32
BF16 = mybir.dt.bfloat16
I32 = mybir.dt.int32
AF = mybir.ActivationFunctionType
ALU = mybir.AluOpType
AX = mybir.AxisListType


class Res:
    __slots__ = ("name", "w", "r", "dsem", "dval")

    def __init__(self, name):
        self.name = name
        self.w = None
        self.r = []
        self.dsem = None
        self.dval = 0


class FW:
    ENG = ("pe", "dve", "act", "pool", "sp")

    def __init__(self, nc):
        self.nc = nc
        self.e = {"pe": nc.tensor, "dve": nc.vector, "act": nc.scalar,
                  "pool": nc.gpsimd, "sp": nc.sync}
        self.sem = {k: nc.alloc_semaphore(name="s_" + k) for k in self.ENG}
        self.cnt = {k: 0 for k in self.ENG}
        self.clock = {k: {} for k in self.ENG}
        self.vc = {}
        self.res = {}
        self.semobj = {k: self.sem[k] for k in self.ENG}
        self.ndsem = 0
        self.nwait = 0
        self.ninst = 0

    def R(self, key):
        r = self.res.get(key)
        if r is None:
            r = Res(key)
            self.res[key] = r
        return r

    def _key(self, x):
        if isinstance(x, Res):
            return x
        if isinstance(x, (str, tuple)):
            return self.R(x)
        t = getattr(x, "tensor", x)
        return self.R(t.name)

    def _wait(self, eng, tok):
        if tok is None:
            return
        semkey, val = tok
        if eng == "pe" and semkey == "pe":
            return
        ck = self.clock[eng]
        if ck.get(semkey, 0) >= val:
            return
        self.e[eng].wait_ge(self.semobj[semkey], val)
        self.nwait += 1
        ck[semkey] = val
        snap = self.vc.get(tok)
        if snap:
            for k, v in snap.items():
                if ck.get(k, 0) < v:
                    ck[k] = v

    def _deps(self, eng, reads, writes):
        rr = [self._key(x) for x in reads]
        ww = [self._key(x) for x in writes]
        for r in rr:
            self._wait(eng, r.w)
        for w in ww:
            self._wait(eng, w.w)
            for t in w.r:
                self._wait(eng, t)
        return rr, ww

    def op(self, eng, fn, reads=(), writes=(), sig=True):
        rr, ww = self._deps(eng, reads, writes)
        inst = fn()
        self.ninst += 1
        pend = self.__dict__.setdefault("pend", {})
        if not sig:
            pend.setdefault(eng, []).append((rr, ww))
            return None
        self.cnt[eng] += 1
        tok = (eng, self.cnt[eng])
        inst.then_inc(self.sem[eng], 1)
        snap = dict(self.clock[eng])
        snap[eng] = self.cnt[eng]
        self.vc[tok] = snap
        for (prr, pww) in pend.pop(eng, []) + [(rr, ww)]:
            for r in prr:
                r.r.append(tok)
            for w in pww:
                w.w = tok
                w.r = []
        return tok

    def dma(self, q, out, in_, reads=(), writes=(), owner=None, **kw):
        rr, ww = self._deps(q, reads, writes)
        own = self._key(owner) if owner is not None else (ww[0] if ww else rr[0])
        if own.dsem is None:
            own.dsem = ("d", self.ndsem)
            self.semobj[own.dsem] = self.nc.alloc_semaphore(name="d%d" % self.ndsem)
            self.ndsem += 1
        own.dval += 16
        tok = (own.dsem, own.dval)
        self.e[q].dma_start(out=out, in_=in_, **kw).then_inc(self.semobj[own.dsem], 16)
        self.ninst += 1
        self.vc[tok] = dict(self.clock[q])
        for r in rr:
            r.r.append(tok)
        for w in ww:
            w.w = tok
            w.r = []
        return tok

    def finish(self, eng="sp"):
        for r in list(self.res.values()):
            self._wait(eng, r.w)
            for t in r.r:
                self._wait(eng, t)
        for k in self.ENG:
            if self.cnt[k]:
                self._wait(eng, (k, self.cnt[k]))


EPS = 1e-6


class PS:
    def __init__(self, nc, n=8):
        self.b = [nc.alloc_psum_tensor("ps%d" % i, [128, 512], F32) for i in range(n)]
        self.i = 0

    def get(self):
        b = self.b[self.i % len(self.b)]
        self.i += 1
        return b


def make_ident(fw, nc, dt, name="ident"):
    ident = nc.alloc_sbuf_tensor(name, [128, 128], dt)
    P = nc.gpsimd
    fw.op("pool", lambda: P.memset(ident[:], 0.0), writes=[ident])
    fw.op("pool", lambda: P.affine_select(out=ident[:], in_=ident[:], pattern=[[-1, 128]],
                                          compare_op=ALU.not_equal, fill=1.0, base=0,
                                          channel_multiplier=1), reads=[ident], writes=[ident])
    return ident


def norm_chunk(fw, nc, ps, x, hn, T, ones_bf, sq, s_sb, rstd, engs=("dve",), g=None):
    V, A, P, TE = nc.vector, nc.scalar, nc.gpsimd, nc.tensor
    fw.op("act", lambda: A.activation(out=sq[:, :, :T], in_=x[:, :, :T], func=AF.Square),
          reads=[x], writes=[sq])
    pb = ps.get()
    for k in range(8):
        fw.op("pe", lambda: TE.matmul(pb[:, :T], lhsT=ones_bf[:], rhs=sq[:, k, :T],
                                      start=(k == 0), stop=(k == 7)),
              reads=[sq, ones_bf], writes=[pb], sig=(k == 7))
    fw.op("act", lambda: A.activation(out=s_sb[:, :T], in_=pb[:, :T], func=AF.Sqrt,
                                      scale=1.0 / 1024, bias=fw.eps_ap),
          reads=[pb, "eps"], writes=[s_sb])
    fw.op("dve", lambda: V.reciprocal(rstd[:, :T], s_sb[:, :T]), reads=[s_sb], writes=[rstd])
    for k in range(8):
        e = engs[k % len(engs)]
        E = fw.e[e]
        if g is None:
            fw.op(e, lambda: E.tensor_tensor(out=hn[:, k, :T], in0=x[:, k, :T], in1=rstd[:, :T],
                                             op=ALU.mult),
                  reads=[x, rstd], writes=[(hn.name, k)])
        else:
            fw.op(e, lambda: E.scalar_tensor_tensor(out=hn[:, k, :T], in0=x[:, k, :T], scalar=g[:, k:k + 1], in1=rstd[:, :T],
                                                    op0=ALU.mult, op1=ALU.mult),
                  reads=[x, rstd, g], writes=[(hn.name, k)])


def build_A(NTOK=4096, HALO=128):
    nc = bass.Bass("TRN2", target_bir_lowering=False)
    V, A, P, TE = nc.vector, nc.scalar, nc.gpsimd, nc.tensor
    TT = NTOK + HALO
    xT = nc.dram_tensor("xT", [1024, TT], F32, kind="ExternalInput").ap()
    w_in = nc.dram_tensor("w_in", [1024, 2560], F32, kind="ExternalInput").ap()
    g0 = nc.dram_tensor("g0", [128, 8], F32, kind="ExternalInput").ap()
    cw = nc.dram_tensor("cw", [128, 6, 3], F32, kind="ExternalInput").ap()
    cb = nc.dram_tensor("cb", [128, 6], F32, kind="ExternalInput").ap()
    uT = nc.dram_tensor("uT", [256, NTOK], F32, kind="ExternalOutput").ap()
    ybT = nc.dram_tensor("ybT", [768, NTOK], BF16, kind="ExternalOutput").ap()
    fw = FW(nc)
    ps = PS(nc)
    eps_t = nc.alloc_sbuf_tensor("eps", [128, 1], F32)
    fw.op("pool", lambda: P.memset(eps_t[:], EPS), writes=["eps"])
    fw.eps_ap = eps_t[:, 0:1]
    ones_bf = nc.alloc_sbuf_tensor("ones_bf", [128, 128], BF16)
    fw.op("pool", lambda: P.memset(ones_bf[:], 1.0), writes=[ones_bf])
    g_sb = nc.alloc_sbuf_tensor("g_sb", [128, 8], F32)
    cw_sb = nc.alloc_sbuf_tensor("cw_sb", [128, 6, 3], F32)
    cb_sb = nc.alloc_sbuf_tensor("cb_sb", [128, 6], F32)
    fw.dma("sp", g_sb[:], g0, writes=[g_sb])
    fw.dma("sp", cw_sb[:], cw, writes=[cw_sb])
    fw.dma("sp", cb_sb[:], cb, writes=[cb_sb])
    w_bf = nc.alloc_sbuf_tensor("w_bf", [128, 8, 2560], BF16)
    wst = [nc.alloc_sbuf_tensor("wst%d" % i, [128, 2560], F32) for i in range(2)]
    for k in range(8):
        st = wst[k % 2]
        fw.dma("sp", st[:], w_in[k * 128:(k + 1) * 128, :], writes=[st])
        fw.op("dve", lambda: V.tensor_scalar(w_bf[:, k, :], st[:], g_sb[:, k:k + 1], None, op0=ALU.mult),
              reads=[st, g_sb], writes=[("w_bf", k)])
    wres = [("w_bf", k) for k in range(8)]
    xv = xT.rearrange("(k p) t -> p k t", p=128)
    xb = [nc.alloc_sbuf_tensor("xb%d" % i, [128, 8, 512], F32) for i in range(2)]
    hnb = [nc.alloc_sbuf_tensor("hn%d" % i, [128, 8, 512], BF16) for i in range(2)]
    sq = nc.alloc_sbuf_tensor("sq", [128, 8, 512], BF16)
    s_sb = nc.alloc_sbuf_tensor("s_sb", [128, 512], F32)
    rstd = nc.alloc_sbuf_tensor("rstd", [128, 512], F32)
    zb = [nc.alloc_sbuf_tensor("z%d" % c, [128, 514], F32) for c in range(6)]
    xcs = [nc.alloc_sbuf_tensor("xcs%d" % i, [128, 512], F32) for i in range(2)]
    acc = [nc.alloc_sbuf_tensor("acc%d" % i, [128, 512], F32) for i in range(2)]
    ybo = [nc.alloc_sbuf_tensor("ybo%d" % i, [128, 512], BF16) for i in range(2)]
    uo = [nc.alloc_sbuf_tensor("uo%d" % i, [128, 512], F32) for i in range(2)]
    for c in range(6):
        fw.op("pool", lambda: P.memset(zb[c][:], 0.0), writes=[zb[c]])

    def proj_tile(hn, f0, T):
        pb = ps.get()
        for k in range(8):
            fw.op("pe", lambda: TE.matmul(pb[:, :T], lhsT=w_bf[:, k, f0:f0 + 128], rhs=hn[:, k, :T],
                                          start=(k == 0), stop=(k == 7)),
                  reads=[("w_bf", k), (hn.name, k)], writes=[pb], sig=(k == 7))
        return pb

    chunks = [(0, HALO, True)] + [(HALO + 512 * i, 512, False) for i in range(NTOK // 512)]
    ci = 0
    for (c0, T, halo) in chunks:
        x = xb[ci % 2]
        hn = hnb[ci % 2]
        if ci == 0:
            fw.dma("sp", x[:, :, :T], xv[:, :, c0:c0 + T], writes=[x])
        if ci + 1 < len(chunks):
            c1, T1, _ = chunks[ci + 1]
            xn = xb[(ci + 1) % 2]
            fw.dma("sp", xn[:, :, :T1], xv[:, :, c1:c1 + T1], writes=[xn])
        norm_chunk(fw, nc, ps, x, hn, T, ones_bf, sq, s_sb, rstd)
        o0 = c0 - HALO
        if not halo:
            for j in range(2):
                pb = proj_tile(hn, j * 128, T)
                u_sb = uo[j]
                fw.op("act", lambda: A.copy(out=u_sb[:, :T], in_=pb[:, :T]), reads=[pb], writes=[u_sb])
                fw.dma("pool", uT[j * 128:(j + 1) * 128, o0:o0 + T], u_sb[:, :T], reads=[u_sb])
        for c in range(6):
            z = zb[c]
            p_xc = proj_tile(hn, 256 + c * 128, T)
            p_gc = proj_tile(hn, 256 + 1536 + c * 128, T)
            xs = xcs[c % 2]
            fw.op("act", lambda: A.copy(out=xs[:, :T], in_=p_xc[:, :T]), reads=[p_xc], writes=[xs])
            fw.op("dve", lambda: V.tensor_tensor(out=z[:, 2:2 + T], in0=p_gc[:, :T], in1=xs[:, :T], op=ALU.mult),
                  reads=[p_gc, xs], writes=[z])
            if not halo:
                p_gb = proj_tile(hn, 256 + 768 + c * 128, T)
                a = acc[c % 2]
                fw.op("dve", lambda: V.tensor_scalar(a[:, :T], z[:, 0:T], cw_sb[:, c, 0:1], cb_sb[:, c:c + 1],
                                                     op0=ALU.mult, op1=ALU.add),
                      reads=[z, cw_sb, cb_sb], writes=[a])
                fw.op("dve", lambda: V.scalar_tensor_tensor(out=a[:, :T], in0=z[:, 1:1 + T], scalar=cw_sb[:, c, 1:2],
                                                            in1=a[:, :T], op0=ALU.mult, op1=ALU.add),
                      reads=[z, cw_sb, a], writes=[a])
                fw.op("dve", lambda: V.scalar_tensor_tensor(out=a[:, :T], in0=z[:, 2:2 + T], scalar=cw_sb[:, c, 2:3],
                                                            in1=a[:, :T], op0=ALU.mult, op1=ALU.add),
                      reads=[z, cw_sb, a], writes=[a])
                yo = ybo[c % 2]
                fw.op("dve", lambda: V.tensor_tensor(out=yo[:, :T], in0=p_gb[:, :T], in1=a[:, :T], op=ALU.mult),
                      reads=[p_gb, a], writes=[yo])
                fw.dma("pool", ybT[c * 128:(c + 1) * 128, o0:o0 + T], yo[:, :T], reads=[yo])
            fw.op("act", lambda: A.copy(out=z[:, 0:2], in_=z[:, T:T + 2]), reads=[z], writes=[z])
        ci += 1
    fw.finish("sp")
    print("A: ninst", fw.ninst, "nwait", fw.nwait, "dsems", fw.ndsem)
    return nc


import math

TWO_PI = 2.0 * math.pi
C1 = 6.28125
C2 = TWO_PI - C1
BND = 3.1415925


def sincos(fw, nc, ph, sin_o, cos_o, tmp_f, tmp_i, tmp_m, eng="dve"):
    V, A = nc.vector, nc.scalar
    rd = [ph.tensor, tmp_f.tensor, tmp_i.tensor, tmp_m.tensor]

    def vop(fn, reads, writes):
        fw.op("dve", fn, reads=reads, writes=writes)
    vop(lambda: V.tensor_scalar(tmp_f, ph, 1.0 / TWO_PI, None, op0=ALU.mult), [ph.tensor], [tmp_f.tensor])
    vop(lambda: V.tensor_copy(tmp_i, tmp_f), [tmp_f.tensor], [tmp_i.tensor])
    vop(lambda: V.tensor_copy(tmp_f, tmp_i), [tmp_i.tensor], [tmp_f.tensor])
    vop(lambda: V.scalar_tensor_tensor(out=tmp_m, in0=tmp_f, scalar=-C1, in1=ph, op0=ALU.mult, op1=ALU.add),
        [tmp_f.tensor, ph.tensor], [tmp_m.tensor])
    vop(lambda: V.scalar_tensor_tensor(out=tmp_m, in0=tmp_f, scalar=-C2, in1=tmp_m, op0=ALU.mult, op1=ALU.add),
        [tmp_f.tensor, tmp_m.tensor], [tmp_m.tensor])

    def wrap(r):
        vop(lambda: V.tensor_scalar(tmp_f, r, BND, -TWO_PI, op0=ALU.is_gt, op1=ALU.mult), [r.tensor], [tmp_f.tensor])
        vop(lambda: V.tensor_tensor(out=r, in0=r, in1=tmp_f, op=ALU.add), [r.tensor, tmp_f.tensor], [r.tensor])
        vop(lambda: V.tensor_scalar(tmp_f, r, -BND, TWO_PI, op0=ALU.is_lt, op1=ALU.mult), [r.tensor], [tmp_f.tensor])
        vop(lambda: V.tensor_tensor(out=r, in0=r, in1=tmp_f, op=ALU.add), [r.tensor, tmp_f.tensor], [r.tensor])
    wrap(tmp_m)
    fw.op("act", lambda: A.activation(out=sin_o, in_=tmp_m, func=AF.Sin), reads=[tmp_m.tensor], writes=[sin_o.tensor])
    vop(lambda: V.tensor_scalar(tmp_m, tmp_m, math.pi / 2, None, op0=ALU.add), [tmp_m.tensor], [tmp_m.tensor])
    wrap(tmp_m)
    fw.op("act", lambda: A.activation(out=cos_o, in_=tmp_m, func=AF.Sin), reads=[tmp_m.tensor], writes=[cos_o.tensor])


def build_B(NT=32768, LB=16384, TC=512):
    nc = bass.Bass("TRN2", target_bir_lowering=False)
    V, A, P, TE = nc.vector, nc.scalar, nc.gpsimd, nc.tensor
    u = nc.dram_tensor("u", [32, NT], F32, kind="ExternalInput").ap()
    lamP = nc.dram_tensor("lamP", [128, 3], F32, kind="ExternalInput").ap()
    lamF = nc.dram_tensor("lamF", [32, 3, 128], F32, kind="ExternalInput").ap()
    BreT = nc.dram_tensor("BreT", [32, 128], F32, kind="ExternalInput").ap()
    BimT = nc.dram_tensor("BimT", [32, 128], F32, kind="ExternalInput").ap()
    CreT = nc.dram_tensor("CreT", [128, 32], F32, kind="ExternalInput").ap()
    CimT = nc.dram_tensor("CimT", [128, 32], F32, kind="ExternalInput").ap()
    dP = nc.dram_tensor("dP", [32, 1], F32, kind="ExternalInput").ap()
    yT = nc.dram_tensor("yT", [32, NT], F32, kind="ExternalOutput").ap()
    fw = FW(nc)
    ps = PS(nc)

    def sb(name, shape, dt=F32):
        return nc.alloc_sbuf_tensor(name, shape, dt)
    lp = sb("lp", [128, 3]); lf = sb("lf", [32, 3, 128])
    bre = sb("bre", [32, 128]); bim = sb("bim", [32, 128]); cre = sb("cre", [128, 32]); cim = sb("cim", [128, 32])
    d_sb = sb("d_sb", [32, 1])
    for t, src in ((lp, lamP), (lf, lamF), (bre, BreT), (bim, BimT), (cre, CreT), (cim, CimT), (d_sb, dP)):
        fw.dma("sp", t[:], src, writes=[t])
    gu = nc.dram_tensor("gu", [2, 4096, 256], F32, kind="ExternalInput").ap()
    dn = nc.dram_tensor("dn", [1024, 1024], F32, kind="ExternalInput").ap()
    gu_o = nc.dram_tensor("gu_o", [2, 4096, 256], BF16, kind="ExternalOutput").ap()
    dn_o = nc.dram_tensor("dn_o", [1024, 1024], BF16, kind="ExternalOutput").ap()
    for i in range(3):
        wst_ = sb("Wst%d" % i, [128, 32, 256]); wob_ = sb("Wob%d" % i, [128, 32, 256], BF16)
        if i < 2:
            src_ap = gu[i].rearrange("(p k) f -> p k f", p=128)
            dst_ap = gu_o[i].rearrange("(p k) f -> p k f", p=128)
        else:
            src_ap = dn.rearrange("(p k) (a f) -> p (k a) f", p=128, f=256)
            dst_ap = dn_o.rearrange("(p k) (a f) -> p (k a) f", p=128, f=256)
        fw.dma("sp", wst_[:], src_ap, writes=[wst_])
        fw.op("act", lambda: A.copy(out=wob_[:], in_=wst_[:]), reads=[wst_], writes=[wob_])
        fw.dma("act", dst_ap, wob_[:], reads=[wob_])
    pp = sb("pp", [128, 4])
    fw.op("act", lambda: A.activation(out=pp[:, 0:1], in_=lp[:, 2:3], func=AF.Exp), reads=[lp], writes=[pp])
    fw.op("dve", lambda: V.tensor_tensor(out=pp[:, 3:4], in0=lp[:, 0:1], in1=pp[:, 0:1], op=ALU.mult), reads=[lp, pp], writes=[pp])
    fw.op("act", lambda: A.activation(out=pp[:, 1:2], in_=pp[:, 3:4], func=AF.Exp), reads=[pp], writes=[pp])
    fw.op("dve", lambda: V.tensor_tensor(out=pp[:, 2:3], in0=lp[:, 1:2], in1=pp[:, 0:1], op=ALU.mult), reads=[lp, pp], writes=[pp])
    NTB = TC + 1
    tau_i = sb("tau_i", [128, NTB], I32)
    fw.op("pool", lambda: P.iota(tau_i[:], pattern=[[1, NTB]], base=0, channel_multiplier=0), writes=[tau_i])
    ph = sb("ph", [128, NTB]); tf = sb("tf", [128, NTB]); ti = sb("ti", [128, NTB], I32); tm = sb("tm", [128, NTB])
    cosT = sb("cosT", [128, NTB]); sinT = sb("sinT", [128, NTB]); rhoT = sb("rhoT", [128, TC])
    fw.op("dve", lambda: V.tensor_copy(ph[:], tau_i[:]), reads=[tau_i], writes=[ph])
    fw.op("dve", lambda: V.tensor_scalar(ph[:], ph[:], pp[:, 2:3], None, op0=ALU.mult), reads=[ph, pp], writes=[ph])
    sincos(fw, nc, ph[:], sinT[:], cosT[:], tf[:], ti[:], tm[:])
    fw.op("pool", lambda: P.memset(rhoT[:], 1.0), writes=[rhoT])
    fw.op("dve", lambda: V.tensor_scalar(rhoT[:], rhoT[:], pp[:, 1:2], None, op0=ALU.mult), reads=[rhoT, pp], writes=[rhoT])
    dtF = sb("dtF", [32, 128]); magF = sb("magF", [32, 128]); thF = sb("thF", [32, 128])
    f1 = sb("f1", [32, 128]); f2 = sb("f2", [32, 128]); fi = sb("fi", [32, 128], I32)
    sF = sb("sF", [32, 128]); cF = sb("cF", [32, 128])
    are = sb("are", [32, 128]); aim = sb("aim", [32, 128]); fre = sb("fre", [32, 128]); fim = sb("fim", [32, 128])
    t1 = sb("t1", [32, 128]); t2 = sb("t2", [32, 128])
    lr, li, ld = lf[:, 0, :], lf[:, 1, :], lf[:, 2, :]
    fw.op("act", lambda: A.activation(out=dtF[:], in_=ld, func=AF.Exp), reads=[lf], writes=[dtF])
    fw.op("dve", lambda: V.tensor_tensor(out=magF[:], in0=lr, in1=dtF[:], op=ALU.mult), reads=[lf, dtF], writes=[magF])
    fw.op("act", lambda: A.activation(out=magF[:], in_=magF[:], func=AF.Exp), reads=[magF], writes=[magF])
    fw.op("dve", lambda: V.tensor_tensor(out=thF[:], in0=li, in1=dtF[:], op=ALU.mult), reads=[lf, dtF], writes=[thF])
    sincos(fw, nc, thF[:], sF[:], cF[:], f1[:], fi[:], f2[:])

    def tt(o, a, b, op):
        fw.op("dve", lambda: V.tensor_tensor(out=o, in0=a, in1=b, op=op), reads=[a, b], writes=[o])
    tt(are[:], magF[:], cF[:], ALU.mult)
    tt(aim[:], magF[:], sF[:], ALU.mult)
    fw.op("dve", lambda: V.tensor_scalar(are[:], are[:], -1.0, None, op0=ALU.add), reads=[are], writes=[are])
    tt(t1[:], lr, lr, ALU.mult)
    tt(t2[:], li, li, ALU.mult)
    tt(t1[:], t1[:], t2[:], ALU.add)
    fw.op("dve", lambda: V.reciprocal(t1[:], t1[:]), reads=[t1], writes=[t1])
    tt(fre[:], are[:], lr, ALU.mult)
    tt(t2[:], aim[:], li, ALU.mult)
    tt(fre[:], fre[:], t2[:], ALU.add)
    tt(fre[:], fre[:], t1[:], ALU.mult)
    tt(fim[:], aim[:], lr, ALU.mult)
    tt(t2[:], are[:], li, ALU.mult)
    tt(fim[:], fim[:], t2[:], ALU.subtract)
    tt(fim[:], fim[:], t1[:], ALU.mult)
    wbre = sb("wbre", [32, 128], BF16); wbim = sb("wbim", [32, 128], BF16)
    tt(t1[:], fre[:], bre[:], ALU.mult)
    tt(t2[:], fim[:], bim[:], ALU.mult)
    tt(wbre[:], t1[:], t2[:], ALU.subtract)
    tt(t1[:], fre[:], bim[:], ALU.mult)
    tt(t2[:], fim[:], bre[:], ALU.mult)
    tt(wbim[:], t1[:], t2[:], ALU.add)
    cre_bf = sb("cre_bf", [128, 32], BF16); ncim_bf = sb("ncim_bf", [128, 32], BF16)
    fw.op("dve", lambda: V.tensor_copy(cre_bf[:], cre[:]), reads=[cre], writes=[cre_bf])
    fw.op("dve", lambda: V.tensor_scalar(ncim_bf[:], cim[:], -1.0, None, op0=ALU.mult), reads=[cim], writes=[ncim_bf])
    ub = [sb("ub%d" % i, [32, TC]) for i in range(2)]
    ubf = [sb("ubf%d" % i, [32, TC], BF16) for i in range(2)]
    a1 = sb("a1", [128, TC]); a2 = sb("a2", [128, TC]); zir = sb("zir", [128, TC]); zii = sb("zii", [128, TC])
    zr = [sb("zr%d" % i, [128, TC]) for i in range(2)]
    zi = [sb("zi%d" % i, [128, TC]) for i in range(2)]
    b1 = sb("b1", [128, TC]); b2 = sb("b2", [128, TC])
    xr = [sb("xr%d" % i, [128, TC], BF16) for i in range(2)]
    xi = [sb("xi%d" % i, [128, TC], BF16) for i in range(2)]
    yo = [sb("yo%d" % i, [32, TC]) for i in range(2)]
    init = sb("init", [128, 2]); itmp = sb("itmp", [128, 2])
    nch = NT // TC
    cpb = LB // TC
    cT, sT = cosT[:, 0:TC], sinT[:, 0:TC]
    fw.dma("sp", ub[0][:], u[:, 0:TC], writes=[ub[0]])
    for ci in range(nch):
        ucur = ub[ci % 2]; ubc = ubf[ci % 2]
        if ci + 1 < nch:
            fw.dma("sp", ub[(ci + 1) % 2][:], u[:, (ci + 1) * TC:(ci + 2) * TC], writes=[ub[(ci + 1) % 2]])
        if ci % cpb == 0:
            fw.op("dve", lambda: V.memset(init[:], 0.0), writes=[init])
        fw.op("act", lambda: A.copy(out=ubc[:], in_=ucur[:]), reads=[ucur], writes=[ubc])
        pr = ps.get(); pi = ps.get()
        fw.op("pe", lambda: TE.matmul(pr[:, :TC], lhsT=wbre[:], rhs=ubc[:], start=True, stop=True), reads=[wbre, ubc], writes=[pr])
        fw.op("pe", lambda: TE.matmul(pi[:, :TC], lhsT=wbim[:], rhs=ubc[:], start=True, stop=True), reads=[wbim, ubc], writes=[pi])
        fw.op("dve", lambda: V.tensor_tensor(out=a1[:], in0=pr[:, :TC], in1=cT, op=ALU.mult), reads=[pr, cosT], writes=[a1])
        fw.op("dve", lambda: V.tensor_tensor(out=a2[:], in0=pi[:, :TC], in1=sT, op=ALU.mult), reads=[pi, sinT], writes=[a2])
        fw.op("dve", lambda: V.tensor_tensor(out=zir[:], in0=a1[:], in1=a2[:], op=ALU.add), reads=[a1, a2], writes=[zir])
        fw.op("dve", lambda: V.tensor_tensor(out=a1[:], in0=pi[:, :TC], in1=cT, op=ALU.mult), reads=[pi, cosT], writes=[a1])
        fw.op("dve", lambda: V.tensor_tensor(out=a2[:], in0=pr[:, :TC], in1=sT, op=ALU.mult), reads=[pr, sinT], writes=[a2])
        fw.op("dve", lambda: V.tensor_tensor(out=zii[:], in0=a1[:], in1=a2[:], op=ALU.subtract), reads=[a1, a2], writes=[zii])
        zrc = zr[ci % 2]; zic = zi[ci % 2]
        fw.op("dve", lambda: V.tensor_tensor_scan(out=zrc[:], data0=rhoT[:], data1=zir[:], initial=init[:, 0:1], op0=ALU.mult, op1=ALU.add),
              reads=[rhoT, zir, init], writes=[zrc])
        fw.op("dve", lambda: V.tensor_tensor_scan(out=zic[:], data0=rhoT[:], data1=zii[:], initial=init[:, 1:2], op0=ALU.mult, op1=ALU.add),
              reads=[rhoT, zii, init], writes=[zic])
        c5, s5 = cosT[:, TC:TC + 1], sinT[:, TC:TC + 1]
        fw.op("dve", lambda: V.tensor_scalar(itmp[:, 0:1], zic[:, TC - 1:TC], s5, None, op0=ALU.mult), reads=[zic, sinT], writes=[itmp])
        fw.op("dve", lambda: V.tensor_scalar(itmp[:, 1:2], zrc[:, TC - 1:TC], s5, None, op0=ALU.mult), reads=[zrc, sinT], writes=[itmp])
        fw.op("dve", lambda: V.scalar_tensor_tensor(out=init[:, 0:1], in0=zrc[:, TC - 1:TC], scalar=c5, in1=itmp[:, 0:1], op0=ALU.mult, op1=ALU.subtract),
              reads=[zrc, cosT, itmp], writes=[init])
        fw.op("dve", lambda: V.scalar_tensor_tensor(out=init[:, 1:2], in0=zic[:, TC - 1:TC], scalar=c5, in1=itmp[:, 1:2], op0=ALU.mult, op1=ALU.add),
              reads=[zic, cosT, itmp], writes=[init])
        xrc = xr[ci % 2]; xic = xi[ci % 2]
        fw.op("pool", lambda: P.tensor_tensor(out=b1[:], in0=zrc[:], in1=cT, op=ALU.mult), reads=[zrc, cosT], writes=[b1])
        fw.op("pool", lambda: P.tensor_tensor(out=b2[:], in0=zic[:], in1=sT, op=ALU.mult), reads=[zic, sinT], writes=[b2])
        fw.op("pool", lambda: P.tensor_tensor(out=xrc[:], in0=b1[:], in1=b2[:], op=ALU.subtract), reads=[b1, b2], writes=[xrc])
        fw.op("dve", lambda: V.tensor_tensor(out=a1[:], in0=zrc[:], in1=sT, op=ALU.mult), reads=[zrc, sinT], writes=[a1])
        fw.op("dve", lambda: V.tensor_tensor(out=a2[:], in0=zic[:], in1=cT, op=ALU.mult), reads=[zic, cosT], writes=[a2])
        fw.op("dve", lambda: V.tensor_tensor(out=xic[:], in0=a1[:], in1=a2[:], op=ALU.add), reads=[a1, a2], writes=[xic])
        py = ps.get()
        fw.op("pe", lambda: TE.matmul(py[0:32, :TC], lhsT=cre_bf[:], rhs=xrc[:], start=True, stop=False), reads=[cre_bf, xrc], writes=[py])
        fw.op("pe", lambda: TE.matmul(py[0:32, :TC], lhsT=ncim_bf[:], rhs=xic[:], start=False, stop=True), reads=[ncim_bf, xic], writes=[py])
        yoc = yo[ci % 2]
        fw.op("dve", lambda: V.scalar_tensor_tensor(out=yoc[:], in0=ucur[:], scalar=d_sb[:, 0:1], in1=py[0:32, :TC], op0=ALU.mult, op1=ALU.add),
              reads=[ucur, d_sb, py], writes=[yoc])
        fw.dma("act", yT[:, ci * TC:(ci + 1) * TC], yoc[:], reads=[yoc])
    fw.finish("sp")
    print("B: ninst", fw.ninst, "nwait", fw.nwait, "dsems", fw.ndsem)
    return nc


def host_B_inputs(d, c, uT_full):
    g2 = [2 * c, 2 * c + 1]
    lr = d['s5_lam_re'][0][g2].reshape(128); li = d['s5_lam_im'][0][g2].reshape(128)
    ld = np.repeat(d['s5_log_dt'][0][g2], 64)
    lamP = np.stack([lr, li, ld], 1).astype(np.float32)
    lamF = np.broadcast_to(np.stack([lr, li, ld], 0)[None], (32, 3, 128)).astype(np.float32).copy()
    BreT = np.zeros((32, 128), np.float32); BimT = np.zeros((32, 128), np.float32)
    CreT = np.zeros((128, 32), np.float32); CimT = np.zeros((128, 32), np.float32)
    for gl in range(2):
        g = g2[gl]
        BreT[gl * 16:(gl + 1) * 16, gl * 64:(gl + 1) * 64] = d['s5_b_re'][0][g].T
        BimT[gl * 16:(gl + 1) * 16, gl * 64:(gl + 1) * 64] = d['s5_b_im'][0][g].T
        CreT[gl * 64:(gl + 1) * 64, gl * 16:(gl + 1) * 16] = d['s5_c_re'][0][g].T
        CimT[gl * 64:(gl + 1) * 64, gl * 16:(gl + 1) * 16] = d['s5_c_im'][0][g].T
    dP = d['s5_d'][0][32 * c:32 * c + 32].reshape(32, 1).astype(np.float32)
    return {"u": np.ascontiguousarray(uT_full[32 * c:32 * c + 32]), "lamP": lamP, "lamF": lamF, "BreT": BreT, "BimT": BimT,
            "CreT": CreT, "CimT": CimT, "dP": dP}


T = 512
NCH = 8
NTOK = T * NCH


def build_CE(mode, NCH=NCH):
    NTOK = T * NCH
    nc = bass.Bass("TRN2", target_bir_lowering=False)
    V, A, P, TE = nc.vector, nc.scalar, nc.gpsimd, nc.tensor
    di = lambda n, s, dt=F32: nc.dram_tensor(n, s, dt, kind="ExternalInput").ap()
    xT = di("xT", [1024, NTOK])
    if mode == "C":
        ysT = di("ysT", [256, NTOK])
        ybT = di("ybT", [768, NTOK], BF16)
        w_glu = di("w_glu", [256, 256])
    else:
        oT = di("oT", [1024, NTOK], BF16)
    w_out = di("w_out", [1024, 1024])
    gm = di("gm", [128, 8])
    w_r = di("w_r", [1024, 20])
    b_r = di("b_r", [20, 1])
    w_gate = di("w_gate", [16, 1024, 256], BF16)
    w_up = di("w_up", [16, 1024, 256], BF16)
    w_down = di("w_down", [16, 256, 1024], BF16)
    outT = nc.dram_tensor("outT", [1024, NTOK], F32, kind="ExternalOutput").ap()
    fw = FW(nc)
    ps = PS(nc)

    def sb(name, shape, dt=F32):
        return nc.alloc_sbuf_tensor(name, shape, dt)
    eps_t = sb("eps", [128, 1])
    fw.op("pool", lambda: P.memset(eps_t[:], EPS), writes=["eps"])
    fw.eps_ap = eps_t[:, 0:1]
    ones_bf = sb("ones_bf", [128, 128], BF16)
    fw.op("pool", lambda: P.memset(ones_bf[:], 1.0), writes=[ones_bf])
    ident = make_ident(fw, nc, F32, "identf")
    esel = sb("esel", [16, 16, 128])
    fw.op("pool", lambda: P.memset(esel[:], 0.0), writes=[esel])
    fw.op("pool", lambda: P.affine_select(out=esel[:], in_=esel[:], pattern=[[-1, 16], [0, 128]],
                                          compare_op=ALU.not_equal, fill=1.0, base=0, channel_multiplier=1),
          reads=[esel], writes=[esel])
    gm_sb = sb("gm_sb", [128, 8]); br_sb = sb("br_sb", [20, 1])
    fw.dma("sp", gm_sb[:], gm, writes=[gm_sb])
    fw.dma("sp", br_sb[:], b_r, writes=[br_sb])
    wr_st = sb("wr_st", [128, 8, 20]); wr_sb = sb("wr_sb", [128, 8, 20])
    fw.dma("sp", wr_st[:], w_r.rearrange("(k p) e -> p k e", p=128), writes=[wr_st])
    for k in range(8):
        fw.op("pool", lambda: P.tensor_scalar(wr_sb[:, k, :], wr_st[:, k, :], gm_sb[:, k:k + 1], None, op0=ALU.mult),
              reads=[wr_st, gm_sb], writes=[wr_sb])
    stA = [sb("stA%d" % i, [128, 8, 256]) for i in range(1)]
    stD = [sb("stD%d" % i, [128, 2, 1024]) for i in range(2)]
    wo_bf = sb("wo_bf", [128, 8, 1024], BF16)
    for k in range(8):
        st = stD[k % 2]
        fw.dma("sp", st[:, 0, :], w_out[k * 128:(k + 1) * 128, :], writes=[st])
        fw.op("dve", lambda: V.tensor_copy(wo_bf[:, k, :], st[:, 0, :]), reads=[st], writes=[("wo_bf", k)])
    if mode == "C":
        wg_bf = sb("wglu_bf", [128, 2, 256], BF16)
        st = stA[0]
        fw.dma("sp", st[:, 0:2, :], w_glu.rearrange("(k p) f -> p k f", p=128), writes=[st])
        fw.op("pool", lambda: P.tensor_copy(wg_bf[:], st[:, 0:2, :]), reads=[st], writes=[wg_bf])
    hT = sb("hT", [128, 8, T]); hn = sb("hn", [128, 8, T], BF16)
    sq = sb("sq", [128, 8, T], BF16); s_sb = sb("s_sb", [128, T]); rstd = sb("rstd", [128, T])
    cat = sb("cat", [128, 8, T], BF16)
    actb = [sb("actb%d" % i, [128, 2, T], BF16) for i in range(2)]
    NWB = 4
    wdb = [sb("wdb%d" % i, [128, 2, 1024], BF16) for i in range(NWB)]
    wgb = [sb("wgb%d" % i, [128, 8, 256], BF16) for i in range(NWB)]
    wub = [sb("wub%d" % i, [128, 8, 256], BF16) for i in range(NWB)]
    lg = sb("lg", [20, T]); Lsb = sb("Lsb", [128, 4, 20]); gate = sb("gate", [128, 4, 16]); gT = sb("gT", [16, T])
    sm = sb("sm", [128, 16]); es = sb("es", [128, 4]); tmp4 = sb("tmp4", [128, 4]); ee = sb("ee", [128, 4])
    ohg = sb("ohg", [128, 4]); sel = sb("sel", [128, 4]); ge = sb("ge", [128, 4])
    sl = [sb("sl%d" % i, [128, T]) for i in range(2)]
    am = [sb("am%d" % i, [128, T]) for i in range(2)]
    if mode == "C":
        ys = sb("ys", [128, 2, T]); x2 = sb("x2", [128, 2, T]); yg = sb("yg", [128, 2, T]); ygb = sb("ygb", [128, 2, T], BF16)
        sgl = sb("sgl", [128, T])
    xv = xT.rearrange("(k p) t -> p k t", p=128)
    ov = outT.rearrange("(k p) t -> p k t", p=128)

    for ci in range(NCH):
        c0 = ci * T
        fw.dma("sp", hT[:], xv[:, :, c0:c0 + T], writes=[hT])
        if mode == "C":
            fw.dma("sp", ys[:], ysT.rearrange("(k p) t -> p k t", p=128)[:, :, c0:c0 + T], writes=[ys])
            fw.dma("sp", cat[:, 2:8, :], ybT.rearrange("(k p) t -> p k t", p=128)[:, :, c0:c0 + T], writes=[("cat", "b")])
            fw.op("dve", lambda: V.tensor_tensor(out=x2[:], in0=ys[:], in1=ys[:], op=ALU.mult), reads=[ys], writes=[x2])
            fw.op("dve", lambda: V.tensor_scalar(x2[:], x2[:], 0.044715, 1.0, op0=ALU.mult, op1=ALU.add), reads=[x2], writes=[x2])
            fw.op("dve", lambda: V.tensor_tensor(out=x2[:], in0=x2[:], in1=ys[:], op=ALU.mult), reads=[x2, ys], writes=[x2])
            fw.op("act", lambda: A.activation(out=x2[:], in_=x2[:], func=AF.Sigmoid, scale=1.5957691216057308), reads=[x2], writes=[x2])
            fw.op("dve", lambda: V.tensor_tensor(out=yg[:], in0=ys[:], in1=x2[:], op=ALU.mult), reads=[ys, x2], writes=[yg])
            fw.op("act", lambda: A.copy(out=ygb[:], in_=yg[:]), reads=[yg], writes=[ygb])
            for j in range(2):
                pb = ps.get()
                for i in range(2):
                    fw.op("pe", lambda: TE.matmul(pb[:, :T], lhsT=wg_bf[:, i, j * 128:(j + 1) * 128], rhs=ygb[:, i, :],
                                                  start=(i == 0), stop=(i == 1)), reads=[wg_bf, ygb], writes=[pb], sig=(i == 1))
                fw.op("act", lambda: A.activation(out=sgl[:], in_=pb[:, :T], func=AF.Sigmoid), reads=[pb], writes=[sgl])
                fw.op("dve", lambda: V.tensor_tensor(out=cat[:, j, :], in0=yg[:, j, :], in1=sgl[:], op=ALU.mult),
                      reads=[yg, sgl], writes=[("cat", "a%d" % j)])
            catres = [("cat", "a0"), ("cat", "a1")] + [("cat", "b")] * 6
        else:
            fw.dma("sp", cat[:], oT.rearrange("(k p) t -> p k t", p=128)[:, :, c0:c0 + T], writes=[("cat", "b")])
            catres = [("cat", "b")] * 8
        for dd in range(8):
            pb = ps.get()
            for k in range(8):
                fw.op("pe", lambda: TE.matmul(pb[:, :T], lhsT=wo_bf[:, k, dd * 128:(dd + 1) * 128], rhs=cat[:, k, :],
                                              start=(k == 0), stop=(k == 7)), reads=[("wo_bf", k), catres[k]], writes=[pb], sig=(k == 7))
            fw.op("dve", lambda: V.tensor_tensor(out=hT[:, dd, :], in0=hT[:, dd, :], in1=pb[:, :T], op=ALU.add),
                  reads=[hT, pb], writes=[hT])
        norm_chunk(fw, nc, ps, hT, hn, T, ones_bf, sq, s_sb, rstd, g=gm_sb)
        hnres = [(hn.name, k) for k in range(8)]
        pl = ps.get()
        for k in range(8):
            fw.op("pe", lambda: TE.matmul(pl[0:20, :T], lhsT=wr_sb[:, k, :], rhs=hT[:, k, :], start=(k == 0), stop=(k == 7)),
                  reads=[wr_sb, hT], writes=[pl], sig=(k == 7))
        fw.op("dve", lambda: V.tensor_tensor(out=lg[:], in0=pl[0:20, :T], in1=rstd[0:20, :], op=ALU.mult), reads=[pl, rstd], writes=[lg])
        fw.op("dve", lambda: V.tensor_scalar(lg[:], lg[:], br_sb[:, 0:1], None, op0=ALU.add), reads=[lg, br_sb], writes=[lg])
        pt = ps.get()
        for j in range(4):
            fw.op("pe", lambda: TE.transpose(pt[:, j * 20:(j + 1) * 20], lg[:, j * 128:(j + 1) * 128], ident[0:20, 0:20]),
                  reads=[lg, ident], writes=[pt])
        fw.op("dve", lambda: V.tensor_copy(Lsb[:].rearrange("p a b -> p (a b)"), pt[:, 0:80]), reads=[pt], writes=[Lsb])
        for j in range(4):
            L = Lsb[:, j, :]
            gl = L[:, 0:4]; el = L[:, 4:20]
            gmax, ngmax, gsum, gw = sm[:, 0:1], sm[:, 1:2], sm[:, 2:3], sm[:, 3:4]
            m1, nm1, m2, e2, wf = sm[:, 4:5], sm[:, 5:6], sm[:, 6:7], sm[:, 7:8], sm[:, 8:9]

            def vo(fn, r=(), w=()):
                fw.op("dve", fn, reads=list(r) + [Lsb, sm], writes=list(w))
            vo(lambda: V.tensor_reduce(out=gmax, in_=gl, axis=AX.X, op=ALU.max), w=[sm])
            vo(lambda: V.tensor_scalar(ngmax, gmax, -1.0, None, op0=ALU.mult), w=[sm])
            fw.op("act", lambda: A.activation(out=ge[:], in_=gl, func=AF.Exp, bias=ngmax, scale=1.0, accum_out=gsum),
                  reads=[Lsb, sm], writes=[ge, sm])
            vo(lambda: V.reciprocal(gw, gsum), w=[sm])
            vo(lambda: V.tensor_scalar(ohg[:], gl, gmax, None, op0=ALU.is_ge), w=[ohg])
            vo(lambda: V.tensor_scalar(es[:], el[:, 0:4], ohg[:, 0:1], None, op0=ALU.mult), r=[ohg], w=[es])
            for g in range(1, 4):
                vo(lambda: V.scalar_tensor_tensor(out=es[:], in0=el[:, 4 * g:4 * g + 4], scalar=ohg[:, g:g + 1], in1=es[:],
                                                  op0=ALU.mult, op1=ALU.add), r=[ohg, es], w=[es])
            vo(lambda: V.tensor_reduce(out=m1, in_=es[:], axis=AX.X, op=ALU.max), r=[es], w=[sm])
            vo(lambda: V.tensor_scalar(tmp4[:], es[:], m1, -1e30, op0=ALU.is_ge, op1=ALU.mult), r=[es], w=[tmp4])
            vo(lambda: V.tensor_tensor(out=tmp4[:], in0=tmp4[:], in1=es[:], op=ALU.add), r=[es, tmp4], w=[tmp4])
            vo(lambda: V.tensor_reduce(out=m2, in_=tmp4[:], axis=AX.X, op=ALU.max), r=[tmp4], w=[sm])
            vo(lambda: V.tensor_scalar(sel[:], es[:], m2, None, op0=ALU.is_ge), r=[es], w=[sel])
            vo(lambda: V.tensor_scalar(nm1, m1, -1.0, None, op0=ALU.mult), w=[sm])
            fw.op("act", lambda: A.activation(out=ee[:], in_=es[:], func=AF.Exp, bias=nm1, scale=1.0), reads=[es, sm], writes=[ee])
            fw.op("act", lambda: A.activation(out=e2, in_=m2, func=AF.Exp, bias=nm1, scale=1.0), reads=[sm], writes=[sm])
            vo(lambda: V.tensor_scalar(e2, e2, 1.0, None, op0=ALU.add), w=[sm])
            vo(lambda: V.reciprocal(e2, e2), w=[sm])
            vo(lambda: V.tensor_tensor(out=wf, in0=e2, in1=gw, op=ALU.mult), w=[sm])
            vo(lambda: V.tensor_tensor(out=ee[:], in0=ee[:], in1=sel[:], op=ALU.mult), r=[ee, sel], w=[ee])
            vo(lambda: V.tensor_scalar(ee[:], ee[:], wf, None, op0=ALU.mult), r=[ee], w=[ee])
            for g in range(4):
                vo(lambda: V.tensor_scalar(gate[:, j, 4 * g:4 * g + 4], ee[:], ohg[:, g:g + 1], None, op0=ALU.mult),
                   r=[ee, ohg], w=[gate])
        pg = ps.get()
        for j in range(4):
            fw.op("pe", lambda: TE.transpose(pg[0:16, j * 128:(j + 1) * 128], gate[:, j, :], ident[:]),
                  reads=[gate, ident], writes=[pg])
        fw.op("dve", lambda: V.tensor_copy(gT[:], pg[0:16, :T]), reads=[pg], writes=[gT])
        for e in range(16):
            wg, wu, wd = wgb[e % NWB], wub[e % NWB], wdb[e % NWB]
            ab = actb[e % 2]
            fw.dma("sp", wg[:], w_gate[e].rearrange("(k p) f -> p k f", p=128), writes=[wg])
            fw.dma("sp", wu[:], w_up[e].rearrange("(k p) f -> p k f", p=128), writes=[wu])
            fw.dma("sp", wd[:], w_down[e].rearrange("(k p) f -> p k f", p=128), writes=[wd])
            pgb = ps.get()
            fw.op("pe", lambda: TE.matmul(pgb[:, :T], lhsT=esel[:, e, :], rhs=gT[:], start=True, stop=True),
                  reads=[esel, gT], writes=[pgb])
            for f in range(2):
                p1 = ps.get(); p3 = ps.get()
                for k in range(8):
                    fw.op("pe", lambda: TE.matmul(p1[:, :T], lhsT=wg[:, k, f * 128:(f + 1) * 128], rhs=hn[:, k, :],
                                                  start=(k == 0), stop=(k == 7)), reads=[wg, hnres[k]], writes=[p1], sig=(k == 7))
                for k in range(8):
                    fw.op("pe", lambda: TE.matmul(p3[:, :T], lhsT=wu[:, k, f * 128:(f + 1) * 128], rhs=hn[:, k, :],
                                                  start=(k == 0), stop=(k == 7)), reads=[wu, hnres[k]], writes=[p3], sig=(k == 7))
                s_ = sl[f]; a_ = am[f]
                fw.op("act", lambda: A.activation(out=s_[:], in_=p1[:, :T], func=AF.Silu), reads=[p1], writes=[s_])
                fw.op("dve", lambda: V.tensor_tensor(out=a_[:], in0=s_[:], in1=p3[:, :T], op=ALU.mult), reads=[s_, p3], writes=[a_])
                fw.op("dve", lambda: V.tensor_tensor(out=ab[:, f, :], in0=a_[:], in1=pgb[:, :T], op=ALU.mult),
                      reads=[a_, pgb], writes=[ab])
            for dd in range(8):
                pb = ps.get()
                for f in range(2):
                    fw.op("pe", lambda: TE.matmul(pb[:, :T], lhsT=wd[:, f, dd * 128:(dd + 1) * 128], rhs=ab[:, f, :],
                                                  start=(f == 0), stop=(f == 1)), reads=[wd, ab], writes=[pb], sig=(f == 1))
                fw.op("dve", lambda: V.tensor_tensor(out=hT[:, dd, :], in0=hT[:, dd, :], in1=pb[:, :T], op=ALU.add),
                      reads=[hT, pb], writes=[hT])
        fw.dma("sp", ov[:, :, c0:c0 + T], hT[:], reads=[hT])
    fw.finish("sp")
    print(mode, ": ninst", fw.ninst, "nwait", fw.nwait, "dsems", fw.ndsem)
    return nc


def host_CE_weights(d, layer, wb=None):
    if wb is not None:
        return {"gm": np.ascontiguousarray(d['moe_norm'][layer].reshape(8, 128).T),
                "w_r": np.ascontiguousarray(np.concatenate([d['moe_w_group'][layer], d['moe_w_expert'][layer]], 1)),
                "b_r": np.concatenate([d['moe_b_group'][layer], d['moe_b_expert'][layer]]).reshape(20, 1).astype(np.float32),
                "w_gate": np.ascontiguousarray(wb[0][layer]), "w_up": np.ascontiguousarray(wb[1][layer]),
                "w_down": np.ascontiguousarray(wb[2][layer])}
    return {"gm": np.ascontiguousarray(d['moe_norm'][layer].reshape(8, 128).T),
            "w_r": np.ascontiguousarray(np.concatenate([d['moe_w_group'][layer], d['moe_w_expert'][layer]], 1)),
            "b_r": np.concatenate([d['moe_b_group'][layer], d['moe_b_expert'][layer]]).reshape(20, 1).astype(np.float32),
            "w_gate": d['moe_w_gate'][layer], "w_up": d['moe_w_up'][layer], "w_down": d['moe_w_down'][layer]}


T = 512
NFM = 14
NORMED = [True] * 10 + [False, False, True, True]


def build_P(NCH=8):
    NT = T * NCH
    nc = bass.Bass("TRN2", target_bir_lowering=False)
    V, A, P, TE = nc.vector, nc.scalar, nc.gpsimd, nc.tensor
    di = lambda n, s, dt=F32: nc.dram_tensor(n, s, dt, kind="ExternalInput").ap()
    hT_in = di("hT", [1024, NT])
    w_fm = di("w_fm", [1024, NFM * 128])
    w_tm = di("w_tm", [1024, 548])
    g1 = di("g1", [128, 8])
    G = di("G", [128, NFM])
    FM = nc.dram_tensor("FM", [NFM * 128, NT], BF16, kind="ExternalOutput").ap()
    TM = nc.dram_tensor("TM", [NT, 512], BF16, kind="ExternalOutput").ap()
    GT = nc.dram_tensor("GT", [NT, 36], F32, kind="ExternalOutput").ap()
    fw = FW(nc)
    ps = PS(nc)

    def sb(name, shape, dt=F32):
        return nc.alloc_sbuf_tensor(name, shape, dt)
    eps_t = sb("eps", [128, 1])
    fw.op("pool", lambda: P.memset(eps_t[:], EPS), writes=["eps"])
    fw.eps_ap = eps_t[:, 0:1]
    ones_bf = sb("ones_bf", [128, 128], BF16)
    fw.op("pool", lambda: P.memset(ones_bf[:], 1.0), writes=[ones_bf])
    blk = sb("blk", [128, 128], BF16)
    fw.op("pool", lambda: P.memset(blk[:], 1.0), writes=[blk])
    fw.op("pool", lambda: P.memset(blk[0:64, 64:128], 0.0), reads=[blk], writes=[blk])
    fw.op("pool", lambda: P.memset(blk[64:128, 0:64], 0.0), reads=[blk], writes=[blk])
    g_sb = sb("g_sb", [128, 8]); G_sb = sb("G_sb", [128, NFM])
    fw.dma("sp", g_sb[:], g1, writes=[g_sb])
    fw.dma("sp", G_sb[:], G, writes=[G_sb])
    wf_bf = sb("wf_bf", [128, 8, NFM * 128], BF16)
    wt_bf = sb("wt_bf", [128, 8, 548], BF16)
    wst = [sb("wst%d" % i, [128, NFM * 128]) for i in range(2)]
    for k in range(8):
        st = wst[k % 2]
        fw.dma("sp", st[:], w_fm[k * 128:(k + 1) * 128, :], writes=[st])
        fw.op("dve", lambda: V.tensor_scalar(wf_bf[:, k, :], st[:], g_sb[:, k:k + 1], None, op0=ALU.mult),
              reads=[st, g_sb], writes=[("wf", k)])
    for k in range(8):
        st = wst[k % 2]
        fw.dma("sp", st[:, 0:548], w_tm[k * 128:(k + 1) * 128, :], writes=[st])
        fw.op("dve", lambda: V.tensor_scalar(wt_bf[:, k, :], st[:, 0:548], g_sb[:, k:k + 1], None, op0=ALU.mult),
              reads=[st, g_sb], writes=[("wt", k)])
    hT = [sb("hT%d" % i, [128, 8, T]) for i in range(2)]
    hn = sb("hn", [128, 8, T], BF16)
    sq = sb("sq", [128, 8, T], BF16); s_sb = sb("s_sb", [128, T]); rstd = sb("rstd", [128, T])
    sq2 = [sb("sq2_%d" % i, [128, T], BF16) for i in range(2)]
    s2 = [sb("s2_%d" % i, [128, T]) for i in range(2)]
    fo = [sb("fo%d" % i, [128, T], BF16) for i in range(3)]
    to = [sb("to%d" % i, [128, 512], BF16) for i in range(2)]
    go = [sb("go%d" % i, [128, 36]) for i in range(2)]
    hv = hT_in.rearrange("(k p) t -> p k t", p=128)
    fw.dma("sp", hT[0][:], hv[:, :, 0:T], writes=[hT[0]])
    cnt = 0
    for ci in range(NCH):
        c0 = ci * T
        h = hT[ci % 2]
        if ci + 1 < NCH:
            fw.dma("sp", hT[(ci + 1) % 2][:], hv[:, :, c0 + T:c0 + 2 * T], writes=[hT[(ci + 1) % 2]])
        norm_chunk(fw, nc, ps, h, hn, T, ones_bf, sq, s_sb, rstd)
        for i in range(NFM):
            pb = ps.get()
            for k in range(8):
                fw.op("pe", lambda: TE.matmul(pb[:, :T], lhsT=wf_bf[:, k, i * 128:(i + 1) * 128], rhs=hn[:, k, :],
                                              start=(k == 0), stop=(k == 7)), reads=[("wf", k), (hn.name, k)], writes=[pb], sig=(k == 7))
            o = fo[cnt % 3]
            if NORMED[i]:
                q2 = sq2[cnt % 2]; s_ = s2[cnt % 2]
                fw.op("act", lambda: A.activation(out=q2[:], in_=pb[:, :T], func=AF.Square), reads=[pb], writes=[q2])
                p2 = ps.get()
                fw.op("pe", lambda: TE.matmul(p2[:, :T], lhsT=blk[:], rhs=q2[:], start=True, stop=True), reads=[blk, q2], writes=[p2])
                fw.op("act", lambda: A.activation(out=s_[:], in_=p2[:, :T], func=AF.Sqrt, scale=1.0 / 64, bias=fw.eps_ap),
                      reads=[p2, "eps"], writes=[s_])
                fw.op("dve", lambda: V.reciprocal(s_[:], s_[:]), reads=[s_], writes=[s_])
                fw.op("dve", lambda: V.scalar_tensor_tensor(out=o[:], in0=pb[:, :T], scalar=G_sb[:, i:i + 1], in1=s_[:],
                                                            op0=ALU.mult, op1=ALU.mult), reads=[pb, G_sb, s_], writes=[o])
            else:
                fw.op("act", lambda: A.copy(out=o[:], in_=pb[:, :T]), reads=[pb], writes=[o])
            fw.dma("act", FM[i * 128:(i + 1) * 128, c0:c0 + T], o[:], reads=[o])
            cnt += 1
        for j in range(4):
            pb = ps.get()
            for k in range(8):
                fw.op("pe", lambda: TE.matmul(pb[:, 0:512], lhsT=hn[:, k, j * 128:(j + 1) * 128], rhs=wt_bf[:, k, 0:512],
                                              start=(k == 0), stop=(k == 7)), reads=[("wt", k), (hn.name, k)], writes=[pb], sig=(k == 7))
            t_ = to[j % 2]
            fw.op("act", lambda: A.copy(out=t_[:], in_=pb[:, 0:512]), reads=[pb], writes=[t_])
            fw.dma("act", TM[c0 + j * 128:c0 + (j + 1) * 128, :], t_[:], reads=[t_])
            pg = ps.get()
            for k in range(8):
                fw.op("pe", lambda: TE.matmul(pg[:, 0:36], lhsT=hn[:, k, j * 128:(j + 1) * 128], rhs=wt_bf[:, k, 512:548],
                                              start=(k == 0), stop=(k == 7)), reads=[("wt", k), (hn.name, k)], writes=[pg], sig=(k == 7))
            g_ = go[j % 2]
            fw.op("act", lambda: A.activation(out=g_[:], in_=pg[:, 0:36], func=AF.Sigmoid), reads=[pg], writes=[g_])
            fw.dma("act", GT[c0 + j * 128:c0 + (j + 1) * 128, :], g_[:], reads=[g_])
    fw.finish("sp")
    print("P: ninst", fw.ninst, "nwait", fw.nwait, "dsems", fw.ndsem)
    return nc


FM_COLS = np.concatenate([np.arange(0, 512), np.arange(768, 1536), np.arange(1536, 1792), np.arange(1792, 1920), np.arange(2048, 2176)])
TM_COLS = np.concatenate([np.arange(512, 768), np.arange(1920, 2048), np.arange(2176, 2304), np.arange(2304, 2340)])


def host_P_weights(d):
    w = d['od_w_in'][0]
    t2 = lambda v: np.tile(v, 2)
    one = np.ones(128, np.float32)
    cols = [t2(d['moba_q_norm'][0])] * 2 + [t2(d['moba_k_norm'][0])] * 2 + [t2(d['nsa_q_norm'][0])] * 6 + [one, one] + \
           [t2(d['nsa_ksel_norm'][0]), t2(d['nsa_kwin_norm'][0])]
    return {"w_fm": np.ascontiguousarray(w[:, FM_COLS]), "w_tm": np.ascontiguousarray(w[:, TM_COLS]),
            "g1": np.ascontiguousarray(d['od_norm_mix'][0].reshape(8, 128).T),
            "G": np.ascontiguousarray(np.stack(cols, 1).astype(np.float32))}


L = 16384
NKT = 128
R_KM, R_QM, R_QD, R_KC, R_VC, R_KS, R_KW = 0, 64, 128, 512, 576, 640, 704
C_VM, C_VS, C_VW = 0, 64, 128
NFR = 768
GELU_S = 1.5957691216057308


def asel(fw, nc, ap, pattern, op, fill, base, cm, reads, writes):
    P = nc.gpsimd
    npart = ap.shape[0]
    lo = base + min(0, cm * (npart - 1)) + sum(min(0, st * (n - 1)) for st, n in pattern)
    hi = base + max(0, cm * (npart - 1)) + sum(max(0, st * (n - 1)) for st, n in pattern)
    if op == ALU.is_ge:
        all_t, all_f = lo >= 0, hi < 0
    elif op == ALU.is_gt:
        all_t, all_f = lo > 0, hi <= 0
    else:
        all_t, all_f = False, False
    if all_t:
        return
    if all_f:
        fw.op("pool", lambda: P.memset(ap, fill), reads=reads, writes=writes)
        return
    regs = fw.__dict__.setdefault("fill_regs", {})
    if fill not in regs:
        regs[fill] = P.to_reg(float(fill))
    fr = regs[fill]
    fw.op("pool", lambda: P.affine_select(out=ap, in_=ap, pattern=pattern, compare_op=op, fill=fr, base=base,
                                          channel_multiplier=cm), reads=reads, writes=writes)


def build_D(groups=tuple(range(32)), do_moba=True, do_nsa=True):
    nc = bass.Bass("TRN2", target_bir_lowering=False)
    V, A, P, TE = nc.vector, nc.scalar, nc.gpsimd, nc.tensor
    di = lambda n, s, dt=F32: nc.dram_tensor(n, s, dt, kind="ExternalInput").ap()
    FM = di("FM", [NFR, L], BF16)
    TM = di("TM", [L, 192], BF16)
    GT = di("GT", [L, 9])
    peT = di("peT", [2, 64, 32])
    w1 = di("w1", [2, 2048, 256])
    w2 = di("w2", [2, 256, 64])
    gk = di("gk", [64, 1])
    NS = len(groups)
    Oout = nc.dram_tensor("Oout", [NS * 512, 256], BF16, kind="ExternalOutput").ap()
    fw = FW(nc)

    def sb(name, shape, dt=F32):
        return nc.alloc_sbuf_tensor(name, shape, dt)
    psS = [nc.alloc_psum_tensor("psS%d" % i, [128, 512], F32) for i in range(3)]
    psA = [nc.alloc_psum_tensor("psA%d" % i, [128, 512], F32) for i in range(3)]
    psM = [nc.alloc_psum_tensor("psM%d" % i, [128, 512], F32) for i in range(1)]
    psB = nc.alloc_psum_tensor("psB", [128, 1024], BF16)
    cS = [0]; cM = [0]

    def getS():
        cS[0] += 1
        return psS[cS[0] % 3]

    def getM():
        cM[0] += 1
        return psM[cM[0] % len(psM)]
    eps_t = sb("eps", [128, 1])
    fw.op("pool", lambda: P.memset(eps_t[:], EPS), writes=["eps"])
    identf = make_ident(fw, nc, F32, "identf")
    identb = sb("identb", [128, 128], BF16)
    fw.op("dve", lambda: V.tensor_copy(identb[:], identf[:]), reads=[identf], writes=[identb])
    ones_bf = sb("ones_bf", [128, 128], BF16)
    fw.op("pool", lambda: P.memset(ones_bf[:], 1.0), writes=[ones_bf])
    sel64 = sb("sel64", [65, 128])
    fw.op("pool", lambda: P.memset(sel64[:], 0.0), writes=[sel64])
    fw.op("pool", lambda: P.memset(sel64[64:65, :], 1.0), reads=[sel64], writes=[sel64])
    tri = sb("tri", [128, 4, 512], BF16)
    wmask = sb("wmask", [128, 4, 512], BF16)
    fw.op("pool", lambda: P.memset(tri[:], 1.0), writes=[tri])
    fw.op("pool", lambda: P.memset(wmask[:], 1.0), writes=[wmask])
    for rel in range(4):
        fw.op("pool", lambda: P.affine_select(out=tri[:, rel, :], in_=tri[:, rel, :], pattern=[[1, 512]], compare_op=ALU.is_ge,
                                              fill=0.0, base=-128 * rel, channel_multiplier=-1), reads=[tri], writes=[tri])
        rr = rel - 4
        fw.op("pool", lambda: P.affine_select(out=wmask[:, rel, :], in_=wmask[:, rel, :], pattern=[[-1, 512]], compare_op=ALU.is_gt,
                                              fill=0.0, base=512 + 128 * rr, channel_multiplier=1), reads=[wmask], writes=[wmask])
    Kaug = sb("Kaug", [128, L], BF16)
    Vext = sb("Vext", [128, NKT, 65], BF16)
    fw.op("pool", lambda: P.memset(Vext[:, :, 64:65], 1.0), writes=[Vext])
    Rw = [sb("R%d" % w, [128, 6, 512], BF16) for w in range(4)]
    Pb = [sb("Pb%d" % i, [128, 512], BF16) for i in range(3)]
    cP = [0]
    OT_sb = sb("OT_sb", [65, 512])
    rl = sb("rl", [128, 4]); rg = sb("rg", [128, 4])
    o_acc = sb("o_acc", [128, 4, 192])
    o_bf = sb("o_bf", [128, 4, 192], BF16)
    MT = sb("MT", [128, 320], BF16)
    fw.op("pool", lambda: P.memset(MT[:], 0.0), writes=[MT])
    sc = sb("sc", [128, 256]); top8 = sb("top8", [128, 8])
    gt = sb("gt", [128, 4, 9])

    def exp_to(S, n=512):
        cP[0] += 1
        pb = Pb[cP[0] % 3]
        fw.op("act", lambda: A.activation(out=pb[:, :n], in_=S[:, :n], func=AF.Exp, scale=0.125), reads=[S], writes=[pb])
        return pb

    def finalize(acc, dst_cols, first, gate_col=None, dst=None):
        dst = o_acc if dst is None else dst
        fw.op("dve", lambda: V.tensor_copy(OT_sb[:], acc[0:65, :]), reads=[acc], writes=[OT_sb])
        pm = getM()
        for j in range(4):
            fw.op("pe", lambda: TE.transpose(pm[:, j * 65:(j + 1) * 65], OT_sb[0:65, j * 128:(j + 1) * 128], identf[0:65, 0:65]),
                  reads=[OT_sb, identf], writes=[pm])
        fw.op("dve", lambda: V.tensor_scalar(rl[:], pm[:, 64:260:65], 1e-30, None, op0=ALU.max), reads=[pm], writes=[rl])
        fw.op("dve", lambda: V.reciprocal(rl[:], rl[:]), reads=[rl], writes=[rl])
        if gate_col is not None:
            fw.op("dve", lambda: V.tensor_tensor(out=rg[:], in0=rl[:], in1=gt[:, :, gate_col], op=ALU.mult), reads=[rl, gt], writes=[rg])
            sc_ = rg
        else:
            sc_ = rl
        c0, c1 = dst_cols
        for j in range(4):
            if first:
                fw.op("dve", lambda: V.tensor_scalar(dst[:, j, c0:c1], pm[:, j * 65:j * 65 + 64], sc_[:, j:j + 1], None, op0=ALU.mult),
                      reads=[pm, sc_], writes=[dst])
            else:
                fw.op("dve", lambda: V.scalar_tensor_tensor(out=dst[:, j, c0:c1], in0=pm[:, j * 65:j * 65 + 64], scalar=sc_[:, j:j + 1],
                                                            in1=dst[:, j, c0:c1], op0=ALU.mult, op1=ALU.add),
                      reads=[pm, sc_, dst], writes=[dst])


    def pipe(steps, look=2):
        Ss = {}
        n = len(steps)
        for i in range(n + look):
            if i < n:
                S = getS()
                steps[i][0](S)
                Ss[i] = S
            j = i - look
            if j >= 0:
                steps[j][1](Ss.pop(j))

    if do_nsa:
        KcT = sb("KcT", [64, 1, 1024], BF16)
        Vc_ext = sb("Vc_ext", [128, 1, 8, 65], BF16)
        fw.op("pool", lambda: P.memset(Vc_ext[:], 0.0), writes=[Vc_ext])
        fw.op("pool", lambda: P.memset(Vc_ext[:, :, :, 64:65], 1.0), reads=[Vc_ext], writes=[Vc_ext])
        fw.op("pool", lambda: P.memset(KcT[:], 0.0), writes=[KcT])
        Amat = sb("Amat", [128, 8, 256], BF16)
        fw.op("pool", lambda: P.memset(Amat[:], 1.0), writes=[Amat])
        fw.op("pool", lambda: P.affine_select(out=Amat[:], in_=Amat[:], pattern=[[128, 8], [-4, 256]], compare_op=ALU.is_ge, fill=0.0,
                                              base=1, channel_multiplier=1), reads=[Amat], writes=[Amat])
        fw.op("pool", lambda: P.affine_select(out=Amat[:], in_=Amat[:], pattern=[[-128, 8], [4, 256]], compare_op=ALU.is_ge, fill=0.0,
                                              base=3, channel_multiplier=-1), reads=[Amat], writes=[Amat])
        gk_sb = sb("gk_sb", [64, 1])
        fw.dma("sp", gk_sb[:], gk, writes=[gk_sb])
        w1st = sb("w1st", [64, 8, 256]); w1b = sb("w1b", [64, 32, 256], BF16)
        w2st = sb("w2st", [128, 2, 64]); w2b = sb("w2b", [128, 2, 64], BF16)
        pest = sb("pest", [64, 32]); peb = sb("peb", [64, 32], BF16)
        hb = sb("hb", [128, 2])
        hid = sb("hid", [128, 2, 1024], BF16)
        hx = sb("hx", [128, 512]); hx2 = sb("hx2", [128, 512])
        ksq = sb("ksq", [64, 512], BF16); ks_ = sb("ks_", [64, 512])
        fw.op("pool", lambda: P.memset(hid[:], 0.0), writes=[hid])
        for kv in range(2):
            for half in range(4):
                fw.dma("sp", w1st[:], w1[kv].rearrange("(r d) f -> d r f", d=64)[:, half * 8:(half + 1) * 8, :], writes=[w1st])
                fw.op("dve", lambda: V.tensor_copy(w1b[:, half * 8:(half + 1) * 8, :], w1st[:]), reads=[w1st], writes=[w1b])
            fw.dma("sp", w2st[:], w2[kv].rearrange("(k p) f -> p k f", p=128), writes=[w2st])
            fw.op("dve", lambda: V.tensor_copy(w2b[:], w2st[:]), reads=[w2st], writes=[w2b])
            fw.dma("sp", pest[:], peT[kv], writes=[pest])
            fw.op("dve", lambda: V.tensor_copy(peb[:], pest[:]), reads=[pest], writes=[peb])
            pm = getM()
            for hh in range(2):
                for r_ in range(32):
                    fw.op("pe", lambda: TE.matmul(pm[:, hh:hh + 1], lhsT=w1b[:, r_, hh * 128:(hh + 1) * 128], rhs=peb[:, r_:r_ + 1],
                                                  start=(r_ == 0), stop=(r_ == 31)), reads=[w1b, peb], writes=[pm], sig=(r_ == 31))
            fw.op("dve", lambda: V.tensor_copy(hb[:], pm[:, 0:2]), reads=[pm], writes=[hb])
            for kvh in range(1):
                row0 = (R_KC if kv == 0 else R_VC)
                fw.dma("sp", Kaug[0:64, :], FM[row0:row0 + 64, :], writes=[Kaug])
                for (n0, cnt) in ((0, 512), (512, 511)):
                    for hh in range(2):
                        S = getS()
                        for r_ in range(32):
                            st0 = r_ + 16 * n0
                            fw.op("pe", lambda: TE.matmul(S[:, :cnt], lhsT=w1b[:, r_, hh * 128:(hh + 1) * 128],
                                                          rhs=Kaug[0:64, st0:st0 + 16 * (cnt - 1) + 1:16],
                                                          start=(r_ == 0), stop=(r_ == 31)), reads=[w1b, Kaug], writes=[S], sig=(r_ == 31))
                        fw.op("act", lambda: A.activation(out=hx[:, :cnt], in_=S[:, :cnt], func=AF.Identity, bias=hb[:, hh:hh + 1], scale=1.0),
                              reads=[S, hb], writes=[hx])
                        fw.op("dve", lambda: V.tensor_tensor(out=hx2[:, :cnt], in0=hx[:, :cnt], in1=hx[:, :cnt], op=ALU.mult), reads=[hx], writes=[hx2])
                        fw.op("dve", lambda: V.tensor_scalar(hx2[:, :cnt], hx2[:, :cnt], 0.044715, 1.0, op0=ALU.mult, op1=ALU.add), reads=[hx2], writes=[hx2])
                        fw.op("dve", lambda: V.tensor_tensor(out=hx2[:, :cnt], in0=hx2[:, :cnt], in1=hx[:, :cnt], op=ALU.mult), reads=[hx2, hx], writes=[hx2])
                        fw.op("act", lambda: A.activation(out=hx2[:, :cnt], in_=hx2[:, :cnt], func=AF.Sigmoid, scale=GELU_S), reads=[hx2], writes=[hx2])
                        fw.op("dve", lambda: V.tensor_tensor(out=hid[:, hh, n0:n0 + cnt], in0=hx[:, :cnt], in1=hx2[:, :cnt], op=ALU.mult),
                              reads=[hx, hx2], writes=[hid])
                if kv == 0:
                    for (n0, cnt) in ((0, 512), (512, 511)):
                        S = getS()
                        for hh in range(2):
                            fw.op("pe", lambda: TE.matmul(S[0:64, :cnt], lhsT=w2b[:, hh, :], rhs=hid[:, hh, n0:n0 + cnt],
                                                          start=(hh == 0), stop=(hh == 1)), reads=[w2b, hid], writes=[S], sig=(hh == 1))
                        fw.op("act", lambda: A.activation(out=ksq[:, :cnt], in_=S[0:64, :cnt], func=AF.Square), reads=[S], writes=[ksq])
                        S2 = getS()
                        fw.op("pe", lambda: TE.matmul(S2[0:64, :cnt], lhsT=ones_bf[0:64, 0:64], rhs=ksq[:, :cnt], start=True, stop=True),
                              reads=[ones_bf, ksq], writes=[S2])
                        fw.op("act", lambda: A.activation(out=ks_[:, :cnt], in_=S2[0:64, :cnt], func=AF.Sqrt, scale=1.0 / 64, bias=eps_t[0:64, 0:1]),
                              reads=[S2, "eps"], writes=[ks_])
                        fw.op("dve", lambda: V.reciprocal(ks_[:, :cnt], ks_[:, :cnt]), reads=[ks_], writes=[ks_])
                        fw.op("dve", lambda: V.scalar_tensor_tensor(out=KcT[:, kvh, n0:n0 + cnt], in0=S[0:64, :cnt], scalar=gk_sb[:, 0:1],
                                                                    in1=ks_[:, :cnt], op0=ALU.mult, op1=ALU.mult),
                              reads=[S, gk_sb, ks_], writes=[KcT])
                else:
                    for nt_ in range(8):
                        cnt = 128 if nt_ < 7 else 127
                        pm = getM()
                        for hh in range(2):
                            fw.op("pe", lambda: TE.matmul(pm[0:cnt, 0:64], lhsT=hid[:, hh, nt_ * 128:nt_ * 128 + cnt], rhs=w2b[:, hh, :],
                                                          start=(hh == 0), stop=(hh == 1)), reads=[hid, w2b], writes=[pm], sig=(hh == 1))
                        fw.op("act", lambda: A.copy(out=Vc_ext[0:cnt, kvh, nt_, 0:64], in_=pm[0:cnt, 0:64]), reads=[pm], writes=[Vc_ext])

    if do_moba:
        fw.op("pool", lambda: P.memset(Kaug[64:128, :], 1.0), reads=[Kaug], writes=[("Kaug", "E")])
        fw.op("pool", lambda: P.affine_select(out=Kaug[64:128, :], in_=Kaug[64:128, :], pattern=[[1, L]], compare_op=ALU.is_ge, fill=0.0,
                                              base=0, channel_multiplier=-256), reads=[("Kaug", "E")], writes=[("Kaug", "E")])
        fw.op("pool", lambda: P.affine_select(out=Kaug[64:128, :], in_=Kaug[64:128, :], pattern=[[-1, L]], compare_op=ALU.is_ge, fill=0.0,
                                              base=255, channel_multiplier=256), reads=[("Kaug", "E")], writes=[("Kaug", "E")])
        kmf = sb("kmf", [64, 64]); kmb = sb("kmb", [64, 64], BF16)
        gsc = sb("gsc", [128, 64])
        om = sb("om", [128, 4, 64]); omb = sb("omb", [128, 4, 64], BF16)
        for h in range(1):
            fw.dma("sp", Kaug[0:64, :], FM[R_KM + 64 * h:R_KM + 64 * h + 64, :], writes=[Kaug])
            fw.dma("sp", Vext[:, :, 0:64], TM[:, C_VM + 64 * h:C_VM + 64 * h + 64].rearrange("(kt p) c -> p kt c", p=128), writes=[Vext])
            fw.op("dve", lambda: V.tensor_reduce(out=kmf[:], in_=Kaug[0:64, :].rearrange("p (n s) -> p n s", s=256), axis=AX.X, op=ALU.add),
                  reads=[Kaug], writes=[kmf])
            fw.op("dve", lambda: V.tensor_scalar(kmb[:], kmf[:], 1.0 / 256, None, op0=ALU.mult), reads=[kmf], writes=[kmb])
            for si, j_ in enumerate(groups):
                G = j_
                t0 = 512 * G
                R0 = Rw[0]
                fw.dma("sp", R0[0:64, 0, :], FM[R_QM + 64 * h:R_QM + 64 * h + 64, t0:t0 + 512], writes=[("R", 0, "q")])
                for j in range(4):
                    cur = 2 * G + j // 2
                    pm = getM()
                    fw.op("pe", lambda: TE.matmul(pm[:, 0:64], lhsT=R0[0:64, 0, j * 128:(j + 1) * 128], rhs=kmb[:], start=True, stop=True),
                          reads=[("R", 0, "q"), kmb], writes=[pm])
                    fw.op("act", lambda: A.copy(out=gsc[:], in_=pm[:, 0:64]), reads=[pm], writes=[gsc])
                    asel(fw, nc, gsc[:], [[-1, 64]], ALU.is_ge, -1e9, cur - 1, 0, [gsc], [gsc])
                    fw.op("dve", lambda: V.max(out=top8[:], in_=gsc[:]), reads=[gsc], writes=[top8])
                    asel(fw, nc, gsc[:], [[-1, 64]], ALU.is_ge, 1e9, cur - 1, 0, [gsc, top8], [gsc])
                    fw.op("dve", lambda: V.tensor_scalar(MT[:, 64:128], gsc[:], top8[:, 2:3], 30000.0, op0=ALU.is_ge, op1=ALU.mult),
                          reads=[gsc, top8], writes=[MT])
                    fw.op("dve", lambda: V.tensor_scalar(MT[:, 64:128], MT[:, 64:128], -30000.0, None, op0=ALU.add), reads=[MT], writes=[MT])
                    fw.op("pe", lambda: TE.transpose(psB[:, 0:128], MT[:, 0:128], identb[:]), reads=[MT, identb], writes=[psB])
                    fw.op("act", lambda: A.copy(out=R0[64:128, 0, j * 128:(j + 1) * 128], in_=psB[64:128, 0:128]), reads=[psB], writes=[("R", 0, "m")])
                acc = psA[0]
                nk = 4 * G + 4
                steps = []
                for kt in range(nk):
                    def qk(S, kt=kt):
                        fw.op("pe", lambda: TE.matmul(S[:, :], lhsT=Kaug[:, kt * 128:(kt + 1) * 128], rhs=R0[:, 0, :], start=True, stop=True),
                              reads=[Kaug, ("Kaug", "E"), ("R", 0, "q"), ("R", 0, "m")], writes=[S])

                    def post(S, kt=kt, G=G, nk=nk, acc=acc):
                        pb = exp_to(S)
                        if kt >= 4 * G:
                            fw.op("dve", lambda: V.tensor_tensor(out=pb[:], in0=pb[:], in1=tri[:, kt - 4 * G, :], op=ALU.mult), reads=[pb, tri], writes=[pb])
                        fw.op("pe", lambda: TE.matmul(acc[0:65, :], lhsT=Vext[:, kt, :], rhs=pb[:], start=(kt == 0), stop=(kt == nk - 1)),
                              reads=[Vext, pb], writes=[acc])
                    steps.append((qk, post))
                pipe(steps)
                finalize(acc, (0, 64), True, None, dst=om)
                fw.op("act", lambda: A.copy(out=omb[:], in_=om[:]), reads=[om], writes=[omb])
                fw.dma("act", Oout[si * 512:(si + 1) * 512, 64 * h:64 * h + 64].rearrange("(j p) c -> p j c", p=128), omb[:], reads=[omb])

    if do_nsa:
        fw.op("pool", lambda: P.memset(Kaug[64:128, :], 1.0), reads=[Kaug, ("Kaug", "E")], writes=[("Kaug", "E")])
        fw.op("pool", lambda: P.affine_select(out=Kaug[64:128, :].rearrange("p (w a x) -> p w a x", w=4, a=64),
                                              in_=Kaug[64:128, :].rearrange("p (w a x) -> p w a x", w=4, a=64),
                                              pattern=[[0, 4], [1, 64], [0, 64]], compare_op=ALU.is_equal, fill=0.0,
                                              base=0, channel_multiplier=-1), reads=[("Kaug", "E")], writes=[("Kaug", "E")])
        Pc_all = sb("Pc_all", [128, 8, 6, 512], BF16)
        keepb = [sb("keepb%d" % i, [128, 4, 256]) for i in range(2)]
        biasb = [sb("biasb%d" % i, [128, 4, 256]) for i in range(2)]

        def gen_masks(G, bi):
            kp, bs = keepb[bi], biasb[bi]
            fw.op("pool", lambda: P.memset(kp[:], 1.0), writes=[kp])
            fw.op("pool", lambda: P.memset(bs[:], -1e9), writes=[bs])
            for j in range(4):
                for hf in range(2):
                    cur = 8 * G + 2 * j + hf
                    rows = slice(64 * hf, 64 * hf + 64)
                    asel(fw, nc, kp[rows, j, :], [[-1, 256]], ALU.is_ge, 0.0, cur - 2, 0, [kp], [kp])
                    asel(fw, nc, bs[rows, j, :], [[1, 256]], ALU.is_ge, 1e4, -cur - 1, 0, [bs], [bs])
                    asel(fw, nc, bs[rows, j, :], [[1, 256]], ALU.is_ge, 0.0, -(cur - 1), 0, [bs], [bs])
            fw.op("pool", lambda: P.memset(kp[:, :, 0:1], 0.0), reads=[kp], writes=[kp])
            fw.op("pool", lambda: P.memset(bs[:, :, 0:1], 1e4), reads=[bs], writes=[bs])
        gen_masks(groups[0], 0)
        rlb = sb("rlb", [128, 512])
        Kw = sb("Kw", [64, 1024], BF16)
        Vw = sb("Vw", [128, 8, 65], BF16)
        fw.op("pool", lambda: P.memset(Vw[:, :, 64:65], 1.0), writes=[Vw])
        for kvh in range(1):
            fw.dma("sp", Kaug[0:64, :], FM[R_KS + 64 * kvh:R_KS + 64 * kvh + 64, :], writes=[Kaug])
            fw.dma("sp", Vext[:, :, 0:64], TM[:, C_VS + 64 * kvh:C_VS + 64 * kvh + 64].rearrange("(kt p) c -> p kt c", p=128), writes=[Vext])
            for si, j_ in enumerate(groups):
                G = j_
                t0 = 512 * G
                wmax = (8 * G + 7) // 64
                for w in range(wmax + 1):
                    ng = 6 if w == 0 else 3
                    fw.dma("sp", Rw[w][0:64, 0:ng, :], FM[R_QD:R_QD + 64 * ng, t0:t0 + 512].rearrange("(g d) t -> d g t", d=64), writes=[("R", w, "q")])
                fw.dma("sp", gt[:], GT[t0:t0 + 512, :].rearrange("(j p) c -> p j c", p=128), writes=[gt])
                R0 = Rw[0]
                nkc = (32 * G + 30) // 128 + 1
                for half in range(2):
                    steps = []
                    for kc in range(nkc):
                        for gi in range(3):
                            g = half * 3 + gi

                            def qk(S, kc=kc, g=g):
                                fw.op("pe", lambda: TE.matmul(S[:, :], lhsT=KcT[:, kvh, kc * 128:(kc + 1) * 128], rhs=R0[0:64, g, :], start=True, stop=True),
                                      reads=[KcT, ("R", 0, "q")], writes=[S])

                            def post(S, kc=kc, g=g, gi=gi, nkc=nkc, t0=t0):
                                pc = Pc_all[:, kc, g, :]
                                fw.op("act", lambda: A.activation(out=pc, in_=S[:, :], func=AF.Exp, scale=0.125), reads=[S], writes=[("Pc", kc, g)])
                                asel(fw, nc, pc, [[1, 512]], ALU.is_ge, 0.0, t0 - 31 - 2048 * kc, -16, [("Pc", kc, g)], [("Pc", kc, g)])
                                fw.op("pe", lambda: TE.matmul(psA[gi][0:65, :], lhsT=Vc_ext[:, kvh, kc, :], rhs=pc, start=(kc == 0), stop=(kc == nkc - 1)),
                                      reads=[Vc_ext, ("Pc", kc, g)], writes=[psA[gi]])
                            steps.append((qk, post))
                    pipe(steps)
                    for gi in range(3):
                        g = half * 3 + gi
                        if half == 0:
                            finalize(psA[gi], (g * 64, g * 64 + 64), True, gate_col=0 * 3 + g)
                        else:
                            fw.op("dve", lambda: V.tensor_copy(OT_sb[:], psA[gi][0:65, :]), reads=[psA[gi]], writes=[OT_sb])
                        pm = getM()
                        fw.op("pe", lambda: TE.matmul(pm[:, :], lhsT=sel64[:], rhs=OT_sb[:], start=True, stop=True), reads=[sel64, OT_sb], writes=[pm])
                        fw.op("dve", lambda: V.tensor_scalar(rlb[:], pm[:, :], 1e-30, None, op0=ALU.max), reads=[pm], writes=[rlb])
                        fw.op("dve", lambda: V.reciprocal(rlb[:], rlb[:]), reads=[rlb], writes=[rlb])
                        for kc in range(nkc):
                            pc = Pc_all[:, kc, g, :]
                            fw.op("dve", lambda: V.tensor_tensor(out=pc, in0=pc, in1=rlb[:], op=ALU.mult), reads=[("Pc", kc, g), rlb], writes=[("Pc", kc, g)])
                for j in range(4):
                    pm = getM()
                    n_mm = nkc * 6
                    i_mm = 0
                    for kc in range(nkc):
                        for g in range(6):
                            fw.op("pe", lambda: TE.matmul(pm[:, 0:256], lhsT=Pc_all[:, kc, g, j * 128:(j + 1) * 128], rhs=Amat[:, kc, :],
                                                          start=(i_mm == 0), stop=(i_mm == n_mm - 1)), reads=[("Pc", kc, g), Amat], writes=[pm], sig=(i_mm == n_mm - 1))
                            i_mm += 1
                    kp, bs = keepb[si % 2], biasb[si % 2]
                    fw.op("dve", lambda: V.tensor_tensor(out=sc[:], in0=pm[:, 0:256], in1=kp[:, j, :], op=ALU.mult), reads=[pm, kp], writes=[sc])
                    fw.op("dve", lambda: V.tensor_tensor(out=sc[:], in0=sc[:], in1=bs[:, j, :], op=ALU.add), reads=[sc, bs], writes=[sc])
                    fw.op("dve", lambda: V.max(out=top8[:], in_=sc[:]), reads=[sc], writes=[top8])
                    fw.op("dve", lambda: V.tensor_scalar(MT[:, 64:320], sc[:], top8[:, 7:8], -30000.0, op0=ALU.is_lt, op1=ALU.mult),
                          reads=[sc, top8], writes=[MT])
                    for w in range(wmax + 1):
                        fw.op("pe", lambda: TE.transpose(psB[:, 0:128], MT[:, 64 * w:64 * w + 128], identb[:]), reads=[MT, identb], writes=[psB])
                        for g in range(3):
                            eng = "act" if g % 2 == 0 else "dve"
                            if eng == "act":
                                fw.op("act", lambda: A.copy(out=Rw[w][64:128, g, j * 128:(j + 1) * 128], in_=psB[64:128, 0:128]),
                                      reads=[psB], writes=[("R", w, "m")])
                            else:
                                fw.op("dve", lambda: V.tensor_copy(Rw[w][64:128, g, j * 128:(j + 1) * 128], psB[64:128, 0:128]),
                                      reads=[psB], writes=[("R", w, "m")])
                if si + 1 < len(groups):
                    gen_masks(groups[si + 1], (si + 1) % 2)
                nk = 4 * G + 4
                for half in range(1):
                    steps = []
                    for kt in range(nk):
                        w = kt // 32
                        for gi in range(3):
                            g = half * 3 + gi

                            def qk(S, kt=kt, w=w, g=g):
                                fw.op("pe", lambda: TE.matmul(S[:, :], lhsT=Kaug[:, kt * 128:(kt + 1) * 128], rhs=Rw[w][:, g, :], start=True, stop=True),
                                      reads=[Kaug, ("Kaug", "E"), ("R", w, "q"), ("R", w, "m")], writes=[S])

                            def post(S, kt=kt, gi=gi, G=G, nk=nk):
                                pb = exp_to(S)
                                if kt >= 4 * G:
                                    fw.op("dve", lambda: V.tensor_tensor(out=pb[:], in0=pb[:], in1=tri[:, kt - 4 * G, :], op=ALU.mult), reads=[pb, tri], writes=[pb])
                                fw.op("pe", lambda: TE.matmul(psA[gi][0:65, :], lhsT=Vext[:, kt, :], rhs=pb[:], start=(kt == 0), stop=(kt == nk - 1)),
                                      reads=[Vext, pb], writes=[psA[gi]])
                            steps.append((qk, post))
                    pipe(steps)
                    for gi in range(3):
                        g = half * 3 + gi
                        finalize(psA[gi], (g * 64, g * 64 + 64), False, gate_col=1 * 3 + g)
                k0 = max(0, 4 * G - 4)
                nkw = 4 * G + 4 - k0
                fw.dma("sp", Kw[:, 0:nkw * 128], FM[R_KW + 64 * kvh:R_KW + 64 * kvh + 64, k0 * 128:(k0 + nkw) * 128], writes=[Kw])
                fw.dma("sp", Vw[:, 0:nkw, 0:64], TM[k0 * 128:(k0 + nkw) * 128, C_VW + 64 * kvh:C_VW + 64 * kvh + 64].rearrange("(kt p) c -> p kt c", p=128),
                       writes=[Vw])
                for half in range(1):
                    steps = []
                    for ki in range(nkw):
                        kt = k0 + ki
                        rel = kt - 4 * G
                        for gi in range(3):
                            g = half * 3 + gi

                            def qk(S, ki=ki, g=g):
                                fw.op("pe", lambda: TE.matmul(S[:, :], lhsT=Kw[:, ki * 128:(ki + 1) * 128], rhs=R0[0:64, g, :], start=True, stop=True),
                                      reads=[Kw, ("R", 0, "q")], writes=[S])

                            def post(S, ki=ki, gi=gi, rel=rel, nkw=nkw):
                                pb = exp_to(S)
                                mk = tri[:, rel, :] if rel >= 0 else wmask[:, rel + 4, :]
                                fw.op("dve", lambda: V.tensor_tensor(out=pb[:], in0=pb[:], in1=mk, op=ALU.mult), reads=[pb, tri, wmask], writes=[pb])
                                fw.op("pe", lambda: TE.matmul(psA[gi][0:65, :], lhsT=Vw[:, ki, :], rhs=pb[:], start=(ki == 0), stop=(ki == nkw - 1)),
                                      reads=[Vw, pb], writes=[psA[gi]])
                            steps.append((qk, post))
                    pipe(steps)
                    for gi in range(3):
                        g = half * 3 + gi
                        finalize(psA[gi], (g * 64, g * 64 + 64), False, gate_col=2 * 3 + g)
                fw.op("act", lambda: A.copy(out=o_bf[:], in_=o_acc[:]), reads=[o_acc], writes=[o_bf])
                fw.dma("act", Oout[si * 512:(si + 1) * 512, 64:256].rearrange("(j p) c -> p j c", p=128), o_bf[:], reads=[o_bf])
    fw.finish("sp")
    print("D: ninst", fw.ninst, "nwait", fw.nwait, "dsems", fw.ndsem)
    return nc


def host_D_weights(d):
    return {"peT": np.ascontiguousarray(np.stack([d['cmp_pe_k'][0].T, d['cmp_pe_v'][0].T], 0)),
            "w1": np.ascontiguousarray(np.stack([d['cmp_w1_k'][0], d['cmp_w1_v'][0]], 0)),
            "w2": np.ascontiguousarray(np.stack([d['cmp_w2_k'][0], d['cmp_w2_v'][0]], 0)),
            "gk": d['nsa_kcmp_norm'][0].reshape(64, 1).astype(np.float32)}


def core_inputs(FMb, TMb, GTb, rr):
    h = rr; kvh = rr // 2; half = rr % 2
    mine = [kvh * 6 + half * 3 + i for i in range(3)]
    other = [kvh * 6 + (1 - half) * 3 + i for i in range(3)]
    rows = [np.arange(256 + 64 * h, 256 + 64 * h + 64), np.arange(64 * h, 64 * h + 64)]
    for q in mine + other:
        rows.append(np.arange(512 + 64 * q, 512 + 64 * q + 64))
    rows += [np.arange(1280 + 64 * kvh, 1280 + 64 * kvh + 64), np.arange(1408 + 64 * kvh, 1408 + 64 * kvh + 64),
             np.arange(1536 + 64 * kvh, 1536 + 64 * kvh + 64), np.arange(1664 + 64 * kvh, 1664 + 64 * kvh + 64)]
    rows = np.concatenate(rows)
    cols = np.concatenate([np.arange(64 * h, 64 * h + 64), np.arange(256 + 64 * kvh, 256 + 64 * kvh + 64), np.arange(384 + 64 * kvh, 384 + 64 * kvh + 64)])
    gcols = np.array([br * 12 + q for br in range(3) for q in mine])
    ocols = np.concatenate([np.arange(64 * h, 64 * h + 64)] + [np.arange(256 + 64 * q, 256 + 64 * q + 64) for q in mine])
    return {"FM": np.ascontiguousarray(FMb[rows]), "TM": np.ascontiguousarray(TMb[:, cols]), "GT": np.ascontiguousarray(GTb[:, gcols])}, ocols


BF = ml_dtypes.bfloat16
_CACHE = {}


def _prog(name, fn):
    if name not in _CACHE:
        _CACHE[name] = fn()
    return _CACHE[name]


def _run(nc, in_maps, n=8):
    res = run_bass_kernel_spmd(nc, in_maps, core_ids=list(range(n)))
    return res.results


def build_W():
    nc = bass.Bass("TRN2", target_bir_lowering=False)
    V, A = nc.vector, nc.scalar
    gu = nc.dram_tensor("gu", [2, 4096, 256], F32, kind="ExternalInput").ap()
    dn = nc.dram_tensor("dn", [1024, 1024], F32, kind="ExternalInput").ap()
    gu_o = nc.dram_tensor("gu_o", [2, 4096, 256], BF16, kind="ExternalOutput").ap()
    dn_o = nc.dram_tensor("dn_o", [1024, 1024], BF16, kind="ExternalOutput").ap()
    fw = FW(nc)
    for i in range(2):
        st = nc.alloc_sbuf_tensor("st%d" % i, [128, 32, 256], F32)
        ob = nc.alloc_sbuf_tensor("ob%d" % i, [128, 32, 256], BF16)
        fw.dma("sp", st[:], gu[i].rearrange("(p k) f -> p k f", p=128), writes=[st])
        fw.op("dve", lambda: V.tensor_copy(ob[:, 0:16, :], st[:, 0:16, :]), reads=[st], writes=[(ob.name, 0)])
        fw.op("act", lambda: A.copy(out=ob[:, 16:32, :], in_=st[:, 16:32, :]), reads=[st], writes=[(ob.name, 1)])
        fw.dma("sp", gu_o[i].rearrange("(p k) f -> p k f", p=128), ob[:], reads=[(ob.name, 0), (ob.name, 1)])
    st = nc.alloc_sbuf_tensor("std", [128, 8, 1024], F32)
    ob = nc.alloc_sbuf_tensor("obd", [128, 8, 1024], BF16)
    fw.dma("sp", st[:], dn.rearrange("(p k) f -> p k f", p=128), writes=[st])
    fw.op("dve", lambda: V.tensor_copy(ob[:, 0:4, :], st[:, 0:4, :]), reads=[st], writes=[(ob.name, 0)])
    fw.op("act", lambda: A.copy(out=ob[:, 4:8, :], in_=st[:, 4:8, :]), reads=[st], writes=[(ob.name, 1)])
    fw.dma("sp", dn_o.rearrange("(p k) f -> p k f", p=128), ob[:], reads=[(ob.name, 0), (ob.name, 1)])
    fw.finish("sp")
    return nc


def run_W(d):
    g = d['moe_w_gate'].reshape(8, 4096, 256)
    u = d['moe_w_up'].reshape(8, 4096, 256)
    dn = d['moe_w_down'].reshape(8, 1024, 1024)
    in_maps = [{"gu": np.ascontiguousarray(np.stack([g[c], u[c]], 0)), "dn": np.ascontiguousarray(dn[c])} for c in range(8)]
    r = _run(build_W(), in_maps)
    gb = np.stack([r[c]["gu_o"][0] for c in range(8)], 0).reshape(2, 16, 1024, 256)
    ub = np.stack([r[c]["gu_o"][1] for c in range(8)], 0).reshape(2, 16, 1024, 256)
    db = np.stack([r[c]["dn_o"] for c in range(8)], 0).reshape(2, 16, 256, 1024)
    return gb, ub, db

def kernel(**inp):
    d = {k: np.asarray(v) for k, v in inp.items()}
    x = d['x'].astype(np.float32, copy=False)
    B, Lq, D = x.shape
    Q = 4096
    in_maps = []
    for c in range(8):
        b, q = c // 4, c % 4
        xs = x[b, q * Q:(q + 1) * Q]
        halo = x[b, q * Q - 128:q * Q] if q > 0 else np.zeros((128, D), np.float32)
        in_maps.append({"xT": np.ascontiguousarray(np.concatenate([halo, xs], 0).T), "w_in": d['ev_w_in'][0],
                        "g0": np.ascontiguousarray(d['ev_norm_mix'][0].reshape(8, 128).T),
                        "cw": np.ascontiguousarray(d['conv_w'][0].reshape(3, 6, 128).transpose(2, 1, 0)),
                        "cb": np.ascontiguousarray(d['conv_b'][0].reshape(6, 128).T)})
    rA = _run(build_A(), in_maps)
    uT_full = np.concatenate([rA[c]["uT"] for c in range(8)], axis=1)
    g_ = d['moe_w_gate'].reshape(8, 4096, 256)
    u_ = d['moe_w_up'].reshape(8, 4096, 256)
    dn_ = d['moe_w_down'].reshape(8, 1024, 1024)
    in_maps = []
    for c in range(8):
        m = host_B_inputs(d, c, uT_full)
        m["gu"] = np.ascontiguousarray(np.stack([g_[c], u_[c]], 0))
        m["dn"] = np.ascontiguousarray(dn_[c])
        in_maps.append(m)
    rB = _run(build_B(), in_maps)
    wb = (np.stack([rB[c]["gu_o"][0] for c in range(8)], 0).reshape(2, 16, 1024, 256),
          np.stack([rB[c]["gu_o"][1] for c in range(8)], 0).reshape(2, 16, 1024, 256),
          np.stack([rB[c]["dn_o"] for c in range(8)], 0).reshape(2, 16, 256, 1024))
    ys5 = np.concatenate([rB[c]["yT"] for c in range(8)], axis=0)
    W0 = host_CE_weights(d, 0, wb)
    in_maps = []
    for c in range(8):
        b, q = c // 4, c % 4
        m = {"xT": np.ascontiguousarray(x[b, q * Q:(q + 1) * Q].T), "ysT": np.ascontiguousarray(ys5[:, c * Q:(c + 1) * Q]),
             "ybT": rA[c]["ybT"], "w_glu": d['s5_w_glu'][0], "w_out": d['ev_w_out'][0]}
        m.update(W0)
        in_maps.append(m)
    rC = _run(build_CE("C"), in_maps)
    h2T = [rC[c]["outT"] for c in range(8)]
    WP = host_P_weights(d)
    in_maps = []
    for c in range(8):
        m = {"hT": h2T[c]}
        m.update(WP)
        in_maps.append(m)
    rP = _run(build_P(), in_maps)
    WD = host_D_weights(d)
    in_maps = []
    ocs = []
    for b in range(2):
        FMb = np.concatenate([rP[4 * b + q]["FM"] for q in range(4)], axis=1)
        TMb = np.concatenate([rP[4 * b + q]["TM"] for q in range(4)], axis=0)
        GTb = np.concatenate([rP[4 * b + q]["GT"] for q in range(4)], axis=0)
        for rr in range(4):
            m, ocols = core_inputs(FMb, TMb, GTb, rr)
            m.update(WD)
            in_maps.append(m)
            ocs.append(ocols)
    rD = _run(build_D(), in_maps)
    o = np.zeros((2, Lq, D), BF)
    for c in range(8):
        o[c // 4][:, ocs[c]] = rD[c]["Oout"]
    W1 = host_CE_weights(d, 1, wb)
    in_maps = []
    for c in range(8):
        b, q = c // 4, c % 4
        m = {"xT": h2T[c], "oT": np.ascontiguousarray(o[b, q * Q:(q + 1) * Q].T), "w_out": d['od_w_out'][0]}
        m.update(W1)
        in_maps.append(m)
    rE = _run(build_CE("E"), in_maps)
    out = np.empty((B, Lq, D), np.float32)
    for c in range(8):
        b, q = c // 4, c % 4
        out[b, q * Q:(q + 1) * Q] = rE[c]["outT"].T
    return out
```

```python
import math
import numpy as np
import ml_dtypes
import numpy as np
import concourse.bass as bass
import concourse.mybir as mybir
from concourse.bass_utils import run_bass_kernel_spmd

F32 = mybir.dt.float32
BF16 = mybir.dt.bfloat16
I32 = mybir.dt.int32
AF = mybir.ActivationFunctionType
ALU = mybir.AluOpType
AX = mybir.AxisListType


class Res:
    __slots__ = ("name", "w", "r", "dsem", "dval")

    def __init__(self, name):
        self.name = name
        self.w = None
        self.r = []
        self.dsem = None
        self.dval = 0


class FW:
    ENG = ("pe", "dve", "act", "pool", "sp")

    def __init__(self, nc):
        self.nc = nc
        self.e = {"pe": nc.tensor, "dve": nc.vector, "act": nc.scalar,
                  "pool": nc.gpsimd, "sp": nc.sync}
        self.sem = {k: nc.alloc_semaphore(name="s_" + k) for k in self.ENG}
        self.cnt = {k: 0 for k in self.ENG}
        self.clock = {k: {} for k in self.ENG}
        self.vc = {}
        self.res = {}
        self.semobj = {k: self.sem[k] for k in self.ENG}
        self.ndsem = 0
        self.nwait = 0
        self.ninst = 0

    def R(self, key):
        r = self.res.get(key)
        if r is None:
            r = Res(key)
            self.res[key] = r
        return r

    def _key(self, x):
        if isinstance(x, Res):
            return x
        if isinstance(x, (str, tuple)):
            return self.R(x)
        t = getattr(x, "tensor", x)
        return self.R(t.name)

    def _wait(self, eng, tok):
        if tok is None:
            return
        semkey, val = tok
        if eng == "pe" and semkey == "pe":
            return
        ck = self.clock[eng]
        if ck.get(semkey, 0) >= val:
            return
        self.e[eng].wait_ge(self.semobj[semkey], val)
        self.nwait += 1
        ck[semkey] = val
        snap = self.vc.get(tok)
        if snap:
            for k, v in snap.items():
                if ck.get(k, 0) < v:
                    ck[k] = v

    def _deps(self, eng, reads, writes):
        rr = [self._key(x) for x in reads]
        ww = [self._key(x) for x in writes]
        for r in rr:
            self._wait(eng, r.w)
        for w in ww:
            self._wait(eng, w.w)
            for t in w.r:
                self._wait(eng, t)
        return rr, ww

    def op(self, eng, fn, reads=(), writes=(), sig=True):
        rr, ww = self._deps(eng, reads, writes)
        inst = fn()
        self.ninst += 1
        pend = self.__dict__.setdefault("pend", {})
        if not sig:
            pend.setdefault(eng, []).append((rr, ww))
            return None
        self.cnt[eng] += 1
        tok = (eng, self.cnt[eng])
        inst.then_inc(self.sem[eng], 1)
        snap = dict(self.clock[eng])
        snap[eng] = self.cnt[eng]
        self.vc[tok] = snap
        for (prr, pww) in pend.pop(eng, []) + [(rr, ww)]:
            for r in prr:
                r.r.append(tok)
            for w in pww:
                w.w = tok
                w.r = []
        return tok

    def dma(self, q, out, in_, reads=(), writes=(), owner=None, **kw):
        rr, ww = self._deps(q, reads, writes)
        own = self._key(owner) if owner is not None else (ww[0] if ww else rr[0])
        if own.dsem is None:
            own.dsem = ("d", self.ndsem)
            self.semobj[own.dsem] = self.nc.alloc_semaphore(name="d%d" % self.ndsem)
            self.ndsem += 1
        own.dval += 16
        tok = (own.dsem, own.dval)
        self.e[q].dma_start(out=out, in_=in_, **kw).then_inc(self.semobj[own.dsem], 16)
        self.ninst += 1
        self.vc[tok] = dict(self.clock[q])
        for r in rr:
            r.r.append(tok)
        for w in ww:
            w.w = tok
            w.r = []
        return tok

    def finish(self, eng="sp"):
        for r in list(self.res.values()):
            self._wait(eng, r.w)
            for t in r.r:
                self._wait(eng, t)
        for k in self.ENG:
            if self.cnt[k]:
                self._wait(eng, (k, self.cnt[k]))


EPS = 1e-6


class PS:
    def __init__(self, nc, n=8):
        self.b = [nc.alloc_psum_tensor("ps%d" % i, [128, 512], F32) for i in range(n)]
        self.i = 0

    def get(self):
        b = self.b[self.i % len(self.b)]
        self.i += 1
        return b


def make_ident(fw, nc, dt, name="ident"):
    ident = nc.alloc_sbuf_tensor(name, [128, 128], dt)
    P = nc.gpsimd
    fw.op("pool", lambda: P.memset(ident[:], 0.0), writes=[ident])
    fw.op("pool", lambda: P.affine_select(out=ident[:], in_=ident[:], pattern=[[-1, 128]],
                                          compare_op=ALU.not_equal, fill=1.0, base=0,
                                          channel_multiplier=1), reads=[ident], writes=[ident])
    return ident


def norm_chunk(fw, nc, ps, x, hn, T, ones_bf, sq, s_sb, rstd, engs=("dve",), g=None):
    V, A, P, TE = nc.vector, nc.scalar, nc.gpsimd, nc.tensor
    fw.op("act", lambda: A.activation(out=sq[:, :, :T], in_=x[:, :, :T], func=AF.Square),
          reads=[x], writes=[sq])
    pb = ps.get()
    for k in range(8):
        fw.op("pe", lambda: TE.matmul(pb[:, :T], lhsT=ones_bf[:], rhs=sq[:, k, :T],
                                      start=(k == 0), stop=(k == 7)),
              reads=[sq, ones_bf], writes=[pb], sig=(k == 7))
    fw.op("act", lambda: A.activation(out=s_sb[:, :T], in_=pb[:, :T], func=AF.Sqrt,
                                      scale=1.0 / 1024, bias=fw.eps_ap),
          reads=[pb, "eps"], writes=[s_sb])
    fw.op("dve", lambda: V.reciprocal(rstd[:, :T], s_sb[:, :T]), reads=[s_sb], writes=[rstd])
    for k in range(8):
        e = engs[k % len(engs)]
        E = fw.e[e]
        if g is None:
            fw.op(e, lambda: E.tensor_tensor(out=hn[:, k, :T], in0=x[:, k, :T], in1=rstd[:, :T],
                                             op=ALU.mult),
                  reads=[x, rstd], writes=[(hn.name, k)])
        else:
            fw.op(e, lambda: E.scalar_tensor_tensor(out=hn[:, k, :T], in0=x[:, k, :T], scalar=g[:, k:k + 1], in1=rstd[:, :T],
                                                    op0=ALU.mult, op1=ALU.mult),
                  reads=[x, rstd, g], writes=[(hn.name, k)])


def build_A(NTOK=4096, HALO=128):
    nc = bass.Bass("TRN2", target_bir_lowering=False)
    V, A, P, TE = nc.vector, nc.scalar, nc.gpsimd, nc.tensor
    TT = NTOK + HALO
    xT = nc.dram_tensor("xT", [1024, TT], F32, kind="ExternalInput").ap()
    w_in = nc.dram_tensor("w_in", [1024, 2560], F32, kind="ExternalInput").ap()
    g0 = nc.dram_tensor("g0", [128, 8], F32, kind="ExternalInput").ap()
    cw = nc.dram_tensor("cw", [128, 6, 3], F32, kind="ExternalInput").ap()
    cb = nc.dram_tensor("cb", [128, 6], F32, kind="ExternalInput").ap()
    uT = nc.dram_tensor("uT", [256, NTOK], F32, kind="ExternalOutput").ap()
    ybT = nc.dram_tensor("ybT", [768, NTOK], BF16, kind="ExternalOutput").ap()
    fw = FW(nc)
    ps = PS(nc)
    eps_t = nc.alloc_sbuf_tensor("eps", [128, 1], F32)
    fw.op("pool", lambda: P.memset(eps_t[:], EPS), writes=["eps"])
    fw.eps_ap = eps_t[:, 0:1]
    ones_bf = nc.alloc_sbuf_tensor("ones_bf", [128, 128], BF16)
    fw.op("pool", lambda: P.memset(ones_bf[:], 1.0), writes=[ones_bf])
    g_sb = nc.alloc_sbuf_tensor("g_sb", [128, 8], F32)
    cw_sb = nc.alloc_sbuf_tensor("cw_sb", [128, 6, 3], F32)
    cb_sb = nc.alloc_sbuf_tensor("cb_sb", [128, 6], F32)
    fw.dma("sp", g_sb[:], g0, writes=[g_sb])
    fw.dma("sp", cw_sb[:], cw, writes=[cw_sb])
    fw.dma("sp", cb_sb[:], cb, writes=[cb_sb])
    w_bf = nc.alloc_sbuf_tensor("w_bf", [128, 8, 2560], BF16)
    wst = [nc.alloc_sbuf_tensor("wst%d" % i, [128, 2560], F32) for i in range(2)]
    for k in range(8):
        st = wst[k % 2]
        fw.dma("sp", st[:], w_in[k * 128:(k + 1) * 128, :], writes=[st])
        fw.op("dve", lambda: V.tensor_scalar(w_bf[:, k, :], st[:], g_sb[:, k:k + 1], None, op0=ALU.mult),
              reads=[st, g_sb], writes=[("w_bf", k)])
    wres = [("w_bf", k) for k in range(8)]
    xv = xT.rearrange("(k p) t -> p k t", p=128)
    xb = [nc.alloc_sbuf_tensor("xb%d" % i, [128, 8, 512], F32) for i in range(2)]
    hnb = [nc.alloc_sbuf_tensor("hn%d" % i, [128, 8, 512], BF16) for i in range(2)]
    sq = nc.alloc_sbuf_tensor("sq", [128, 8, 512], BF16)
    s_sb = nc.alloc_sbuf_tensor("s_sb", [128, 512], F32)
    rstd = nc.alloc_sbuf_tensor("rstd", [128, 512], F32)
    zb = [nc.alloc_sbuf_tensor("z%d" % c, [128, 514], F32) for c in range(6)]
    xcs = [nc.alloc_sbuf_tensor("xcs%d" % i, [128, 512], F32) for i in range(2)]
    acc = [nc.alloc_sbuf_tensor("acc%d" % i, [128, 512], F32) for i in range(2)]
    ybo = [nc.alloc_sbuf_tensor("ybo%d" % i, [128, 512], BF16) for i in range(2)]
    uo = [nc.alloc_sbuf_tensor("uo%d" % i, [128, 512], F32) for i in range(2)]
    for c in range(6):
        fw.op("pool", lambda: P.memset(zb[c][:], 0.0), writes=[zb[c]])

    def proj_tile(hn, f0, T):
        pb = ps.get()
        for k in range(8):
            fw.op("pe", lambda: TE.matmul(pb[:, :T], lhsT=w_bf[:, k, f0:f0 + 128], rhs=hn[:, k, :T],
                                          start=(k == 0), stop=(k == 7)),
                  reads=[("w_bf", k), (hn.name, k)], writes=[pb], sig=(k == 7))
        return pb

    chunks = [(0, HALO, True)] + [(HALO + 512 * i, 512, False) for i in range(NTOK // 512)]
    ci = 0
    for (c0, T, halo) in chunks:
        x = xb[ci % 2]
        hn = hnb[ci % 2]
        if ci == 0:
            fw.dma("sp", x[:, :, :T], xv[:, :, c0:c0 + T], writes=[x])
        if ci + 1 < len(chunks):
            c1, T1, _ = chunks[ci + 1]
            xn = xb[(ci + 1) % 2]
            fw.dma("sp", xn[:, :, :T1], xv[:, :, c1:c1 + T1], writes=[xn])
        norm_chunk(fw, nc, ps, x, hn, T, ones_bf, sq, s_sb, rstd)
        o0 = c0 - HALO
        if not halo:
            for j in range(2):
                pb = proj_tile(hn, j * 128, T)
                u_sb = uo[j]
                fw.op("act", lambda: A.copy(out=u_sb[:, :T], in_=pb[:, :T]), reads=[pb], writes=[u_sb])
                fw.dma("pool", uT[j * 128:(j + 1) * 128, o0:o0 + T], u_sb[:, :T], reads=[u_sb])
        for c in range(6):
            z = zb[c]
            p_xc = proj_tile(hn, 256 + c * 128, T)
            p_gc = proj_tile(hn, 256 + 1536 + c * 128, T)
            xs = xcs[c % 2]
            fw.op("act", lambda: A.copy(out=xs[:, :T], in_=p_xc[:, :T]), reads=[p_xc], writes=[xs])
            fw.op("dve", lambda: V.tensor_tensor(out=z[:, 2:2 + T], in0=p_gc[:, :T], in1=xs[:, :T], op=ALU.mult),
                  reads=[p_gc, xs], writes=[z])
            if not halo:
                p_gb = proj_tile(hn, 256 + 768 + c * 128, T)
                a = acc[c % 2]
                fw.op("dve", lambda: V.tensor_scalar(a[:, :T], z[:, 0:T], cw_sb[:, c, 0:1], cb_sb[:, c:c + 1],
                                                     op0=ALU.mult, op1=ALU.add),
                      reads=[z, cw_sb, cb_sb], writes=[a])
                fw.op("dve", lambda: V.scalar_tensor_tensor(out=a[:, :T], in0=z[:, 1:1 + T], scalar=cw_sb[:, c, 1:2],
                                                            in1=a[:, :T], op0=ALU.mult, op1=ALU.add),
                      reads=[z, cw_sb, a], writes=[a])
                fw.op("dve", lambda: V.scalar_tensor_tensor(out=a[:, :T], in0=z[:, 2:2 + T], scalar=cw_sb[:, c, 2:3],
                                                            in1=a[:, :T], op0=ALU.mult, op1=ALU.add),
                      reads=[z, cw_sb, a], writes=[a])
                yo = ybo[c % 2]
                fw.op("dve", lambda: V.tensor_tensor(out=yo[:, :T], in0=p_gb[:, :T], in1=a[:, :T], op=ALU.mult),
                      reads=[p_gb, a], writes=[yo])
                fw.dma("pool", ybT[c * 128:(c + 1) * 128, o0:o0 + T], yo[:, :T], reads=[yo])
            fw.op("act", lambda: A.copy(out=z[:, 0:2], in_=z[:, T:T + 2]), reads=[z], writes=[z])
        ci += 1
    fw.finish("sp")
    print("A: ninst", fw.ninst, "nwait", fw.nwait, "dsems", fw.ndsem)
    return nc


import math

TWO_PI = 2.0 * math.pi
C1 = 6.28125
C2 = TWO_PI - C1
BND = 3.1415925


def sincos(fw, nc, ph, sin_o, cos_o, tmp_f, tmp_i, tmp_m, eng="dve"):
    V, A = nc.vector, nc.scalar
    rd = [ph.tensor, tmp_f.tensor, tmp_i.tensor, tmp_m.tensor]

    def vop(fn, reads, writes):
        fw.op("dve", fn, reads=reads, writes=writes)
    vop(lambda: V.tensor_scalar(tmp_f, ph, 1.0 / TWO_PI, None, op0=ALU.mult), [ph.tensor], [tmp_f.tensor])
    vop(lambda: V.tensor_copy(tmp_i, tmp_f), [tmp_f.tensor], [tmp_i.tensor])
    vop(lambda: V.tensor_copy(tmp_f, tmp_i), [tmp_i.tensor], [tmp_f.tensor])
    vop(lambda: V.scalar_tensor_tensor(out=tmp_m, in0=tmp_f, scalar=-C1, in1=ph, op0=ALU.mult, op1=ALU.add),
        [tmp_f.tensor, ph.tensor], [tmp_m.tensor])
    vop(lambda: V.scalar_tensor_tensor(out=tmp_m, in0=tmp_f, scalar=-C2, in1=tmp_m, op0=ALU.mult, op1=ALU.add),
        [tmp_f.tensor, tmp_m.tensor], [tmp_m.tensor])

    def wrap(r):
        vop(lambda: V.tensor_scalar(tmp_f, r, BND, -TWO_PI, op0=ALU.is_gt, op1=ALU.mult), [r.tensor], [tmp_f.tensor])
        vop(lambda: V.tensor_tensor(out=r, in0=r, in1=tmp_f, op=ALU.add), [r.tensor, tmp_f.tensor], [r.tensor])
        vop(lambda: V.tensor_scalar(tmp_f, r, -BND, TWO_PI, op0=ALU.is_lt, op1=ALU.mult), [r.tensor], [tmp_f.tensor])
        vop(lambda: V.tensor_tensor(out=r, in0=r, in1=tmp_f, op=ALU.add), [r.tensor, tmp_f.tensor], [r.tensor])
    wrap(tmp_m)
    fw.op("act", lambda: A.activation(out=sin_o, in_=tmp_m, func=AF.Sin), reads=[tmp_m.tensor], writes=[sin_o.tensor])
    vop(lambda: V.tensor_scalar(tmp_m, tmp_m, math.pi / 2, None, op0=ALU.add), [tmp_m.tensor], [tmp_m.tensor])
    wrap(tmp_m)
    fw.op("act", lambda: A.activation(out=cos_o, in_=tmp_m, func=AF.Sin), reads=[tmp_m.tensor], writes=[cos_o.tensor])


def build_B(NT=32768, LB=16384, TC=512):
    nc = bass.Bass("TRN2", target_bir_lowering=False)
    V, A, P, TE = nc.vector, nc.scalar, nc.gpsimd, nc.tensor
    u = nc.dram_tensor("u", [32, NT], F32, kind="ExternalInput").ap()
    lamP = nc.dram_tensor("lamP", [128, 3], F32, kind="ExternalInput").ap()
    lamF = nc.dram_tensor("lamF", [32, 3, 128], F32, kind="ExternalInput").ap()
    BreT = nc.dram_tensor("BreT", [32, 128], F32, kind="ExternalInput").ap()
    BimT = nc.dram_tensor("BimT", [32, 128], F32, kind="ExternalInput").ap()
    CreT = nc.dram_tensor("CreT", [128, 32], F32, kind="ExternalInput").ap()
    CimT = nc.dram_tensor("CimT", [128, 32], F32, kind="ExternalInput").ap()
    dP = nc.dram_tensor("dP", [32, 1], F32, kind="ExternalInput").ap()
    yT = nc.dram_tensor("yT", [32, NT], F32, kind="ExternalOutput").ap()
    fw = FW(nc)
    ps = PS(nc)

    def sb(name, shape, dt=F32):
        return nc.alloc_sbuf_tensor(name, shape, dt)
    lp = sb("lp", [128, 3]); lf = sb("lf", [32, 3, 128])
    bre = sb("bre", [32, 128]); bim = sb("bim", [32, 128]); cre = sb("cre", [128, 32]); cim = sb("cim", [128, 32])
    d_sb = sb("d_sb", [32, 1])
    for t, src in ((lp, lamP), (lf, lamF), (bre, BreT), (bim, BimT), (cre, CreT), (cim, CimT), (d_sb, dP)):
        fw.dma("sp", t[:], src, writes=[t])
    gu = nc.dram_tensor("gu", [2, 4096, 256], F32, kind="ExternalInput").ap()
    dn = nc.dram_tensor("dn", [1024, 1024], F32, kind="ExternalInput").ap()
    gu_o = nc.dram_tensor("gu_o", [2, 4096, 256], BF16, kind="ExternalOutput").ap()
    dn_o = nc.dram_tensor("dn_o", [1024, 1024], BF16, kind="ExternalOutput").ap()
    for i in range(3):
        wst_ = sb("Wst%d" % i, [128, 32, 256]); wob_ = sb("Wob%d" % i, [128, 32, 256], BF16)
        if i < 2:
            src_ap = gu[i].rearrange("(p k) f -> p k f", p=128)
            dst_ap = gu_o[i].rearrange("(p k) f -> p k f", p=128)
        else:
            src_ap = dn.rearrange("(p k) (a f) -> p (k a) f", p=128, f=256)
            dst_ap = dn_o.rearrange("(p k) (a f) -> p (k a) f", p=128, f=256)
        fw.dma("sp", wst_[:], src_ap, writes=[wst_])
        fw.op("act", lambda: A.copy(out=wob_[:], in_=wst_[:]), reads=[wst_], writes=[wob_])
        fw.dma("act", dst_ap, wob_[:], reads=[wob_])
    pp = sb("pp", [128, 4])
    fw.op("act", lambda: A.activation(out=pp[:, 0:1], in_=lp[:, 2:3], func=AF.Exp), reads=[lp], writes=[pp])
    fw.op("dve", lambda: V.tensor_tensor(out=pp[:, 3:4], in0=lp[:, 0:1], in1=pp[:, 0:1], op=ALU.mult), reads=[lp, pp], writes=[pp])
    fw.op("act", lambda: A.activation(out=pp[:, 1:2], in_=pp[:, 3:4], func=AF.Exp), reads=[pp], writes=[pp])
    fw.op("dve", lambda: V.tensor_tensor(out=pp[:, 2:3], in0=lp[:, 1:2], in1=pp[:, 0:1], op=ALU.mult), reads=[lp, pp], writes=[pp])
    NTB = TC + 1
    tau_i = sb("tau_i", [128, NTB], I32)
    fw.op("pool", lambda: P.iota(tau_i[:], pattern=[[1, NTB]], base=0, channel_multiplier=0), writes=[tau_i])
    ph = sb("ph", [128, NTB]); tf = sb("tf", [128, NTB]); ti = sb("ti", [128, NTB], I32); tm = sb("tm", [128, NTB])
    cosT = sb("cosT", [128, NTB]); sinT = sb("sinT", [128, NTB]); rhoT = sb("rhoT", [128, TC])
    fw.op("dve", lambda: V.tensor_copy(ph[:], tau_i[:]), reads=[tau_i], writes=[ph])
    fw.op("dve", lambda: V.tensor_scalar(ph[:], ph[:], pp[:, 2:3], None, op0=ALU.mult), reads=[ph, pp], writes=[ph])
    sincos(fw, nc, ph[:], sinT[:], cosT[:], tf[:], ti[:], tm[:])
    fw.op("pool", lambda: P.memset(rhoT[:], 1.0), writes=[rhoT])
    fw.op("dve", lambda: V.tensor_scalar(rhoT[:], rhoT[:], pp[:, 1:2], None, op0=ALU.mult), reads=[rhoT, pp], writes=[rhoT])
    dtF = sb("dtF", [32, 128]); magF = sb("magF", [32, 128]); thF = sb("thF", [32, 128])
    f1 = sb("f1", [32, 128]); f2 = sb("f2", [32, 128]); fi = sb("fi", [32, 128], I32)
    sF = sb("sF", [32, 128]); cF = sb("cF", [32, 128])
    are = sb("are", [32, 128]); aim = sb("aim", [32, 128]); fre = sb("fre", [32, 128]); fim = sb("fim", [32, 128])
    t1 = sb("t1", [32, 128]); t2 = sb("t2", [32, 128])
    lr, li, ld = lf[:, 0, :], lf[:, 1, :], lf[:, 2, :]
    fw.op("act", lambda: A.activation(out=dtF[:], in_=ld, func=AF.Exp), reads=[lf], writes=[dtF])
    fw.op("dve", lambda: V.tensor_tensor(out=magF[:], in0=lr, in1=dtF[:], op=ALU.mult), reads=[lf, dtF], writes=[magF])
    fw.op("act", lambda: A.activation(out=magF[:], in_=magF[:], func=AF.Exp), reads=[magF], writes=[magF])
    fw.op("dve", lambda: V.tensor_tensor(out=thF[:], in0=li, in1=dtF[:], op=ALU.mult), reads=[lf, dtF], writes=[thF])
    sincos(fw, nc, thF[:], sF[:], cF[:], f1[:], fi[:], f2[:])

    def tt(o, a, b, op):
        fw.op("dve", lambda: V.tensor_tensor(out=o, in0=a, in1=b, op=op), reads=[a, b], writes=[o])
    tt(are[:], magF[:], cF[:], ALU.mult)
    tt(aim[:], magF[:], sF[:], ALU.mult)
    fw.op("dve", lambda: V.tensor_scalar(are[:], are[:], -1.0, None, op0=ALU.add), reads=[are], writes=[are])
    tt(t1[:], lr, lr, ALU.mult)
    tt(t2[:], li, li, ALU.mult)
    tt(t1[:], t1[:], t2[:], ALU.add)
    fw.op("dve", lambda: V.reciprocal(t1[:], t1[:]), reads=[t1], writes=[t1])
    tt(fre[:], are[:], lr, ALU.mult)
    tt(t2[:], aim[:], li, ALU.mult)
    tt(fre[:], fre[:], t2[:], ALU.add)
    tt(fre[:], fre[:], t1[:], ALU.mult)
    tt(fim[:], aim[:], lr, ALU.mult)
    tt(t2[:], are[:], li, ALU.mult)
    tt(fim[:], fim[:], t2[:], ALU.subtract)
    tt(fim[:], fim[:], t1[:], ALU.mult)
    wbre = sb("wbre", [32, 128], BF16); wbim = sb("wbim", [32, 128], BF16)
    tt(t1[:], fre[:], bre[:], ALU.mult)
    tt(t2[:], fim[:], bim[:], ALU.mult)
    tt(wbre[:], t1[:], t2[:], ALU.subtract)
    tt(t1[:], fre[:], bim[:], ALU.mult)
    tt(t2[:], fim[:], bre[:], ALU.mult)
    tt(wbim[:], t1[:], t2[:], ALU.add)
    cre_bf = sb("cre_bf", [128, 32], BF16); ncim_bf = sb("ncim_bf", [128, 32], BF16)
    fw.op("dve", lambda: V.tensor_copy(cre_bf[:], cre[:]), reads=[cre], writes=[cre_bf])
    fw.op("dve", lambda: V.tensor_scalar(ncim_bf[:], cim[:], -1.0, None, op0=ALU.mult), reads=[cim], writes=[ncim_bf])
    ub = [sb("ub%d" % i, [32, TC]) for i in range(2)]
    ubf = [sb("ubf%d" % i, [32, TC], BF16) for i in range(2)]
    a1 = sb("a1", [128, TC]); a2 = sb("a2", [128, TC]); zir = sb("zir", [128, TC]); zii = sb("zii", [128, TC])
    zr = [sb("zr%d" % i, [128, TC]) for i in range(2)]
    zi = [sb("zi%d" % i, [128, TC]) for i in range(2)]
    b1 = sb("b1", [128, TC]); b2 = sb("b2", [128, TC])
    xr = [sb("xr%d" % i, [128, TC], BF16) for i in range(2)]
    xi = [sb("xi%d" % i, [128, TC], BF16) for i in range(2)]
    yo = [sb("yo%d" % i, [32, TC]) for i in range(2)]
    init = sb("init", [128, 2]); itmp = sb("itmp", [128, 2])
    nch = NT // TC
    cpb = LB // TC
    cT, sT = cosT[:, 0:TC], sinT[:, 0:TC]
    fw.dma("sp", ub[0][:], u[:, 0:TC], writes=[ub[0]])
    for ci in range(nch):
        ucur = ub[ci % 2]; ubc = ubf[ci % 2]
        if ci + 1 < nch:
            fw.dma("sp", ub[(ci + 1) % 2][:], u[:, (ci + 1) * TC:(ci + 2) * TC], writes=[ub[(ci + 1) % 2]])
        if ci % cpb == 0:
            fw.op("dve", lambda: V.memset(init[:], 0.0), writes=[init])
        fw.op("act", lambda: A.copy(out=ubc[:], in_=ucur[:]), reads=[ucur], writes=[ubc])
        pr = ps.get(); pi = ps.get()
        fw.op("pe", lambda: TE.matmul(pr[:, :TC], lhsT=wbre[:], rhs=ubc[:], start=True, stop=True), reads=[wbre, ubc], writes=[pr])
        fw.op("pe", lambda: TE.matmul(pi[:, :TC], lhsT=wbim[:], rhs=ubc[:], start=True, stop=True), reads=[wbim, ubc], writes=[pi])
        fw.op("dve", lambda: V.tensor_tensor(out=a1[:], in0=pr[:, :TC], in1=cT, op=ALU.mult), reads=[pr, cosT], writes=[a1])
        fw.op("dve", lambda: V.tensor_tensor(out=a2[:], in0=pi[:, :TC], in1=sT, op=ALU.mult), reads=[pi, sinT], writes=[a2])
        fw.op("dve", lambda: V.tensor_tensor(out=zir[:], in0=a1[:], in1=a2[:], op=ALU.add), reads=[a1, a2], writes=[zir])
        fw.op("dve", lambda: V.tensor_tensor(out=a1[:], in0=pi[:, :TC], in1=cT, op=ALU.mult), reads=[pi, cosT], writes=[a1])
        fw.op("dve", lambda: V.tensor_tensor(out=a2[:], in0=pr[:, :TC], in1=sT, op=ALU.mult), reads=[pr, sinT], writes=[a2])
        fw.op("dve", lambda: V.tensor_tensor(out=zii[:], in0=a1[:], in1=a2[:], op=ALU.subtract), reads=[a1, a2], writes=[zii])
        zrc = zr[ci % 2]; zic = zi[ci % 2]
        fw.op("dve", lambda: V.tensor_tensor_scan(out=zrc[:], data0=rhoT[:], data1=zir[:], initial=init[:, 0:1], op0=ALU.mult, op1=ALU.add),
              reads=[rhoT, zir, init], writes=[zrc])
        fw.op("dve", lambda: V.tensor_tensor_scan(out=zic[:], data0=rhoT[:], data1=zii[:], initial=init[:, 1:2], op0=ALU.mult, op1=ALU.add),
              reads=[rhoT, zii, init], writes=[zic])
        c5, s5 = cosT[:, TC:TC + 1], sinT[:, TC:TC + 1]
        fw.op("dve", lambda: V.tensor_scalar(itmp[:, 0:1], zic[:, TC - 1:TC], s5, None, op0=ALU.mult), reads=[zic, sinT], writes=[itmp])
        fw.op("dve", lambda: V.tensor_scalar(itmp[:, 1:2], zrc[:, TC - 1:TC], s5, None, op0=ALU.mult), reads=[zrc, sinT], writes=[itmp])
        fw.op("dve", lambda: V.scalar_tensor_tensor(out=init[:, 0:1], in0=zrc[:, TC - 1:TC], scalar=c5, in1=itmp[:, 0:1], op0=ALU.mult, op1=ALU.subtract),
              reads=[zrc, cosT, itmp], writes=[init])
        fw.op("dve", lambda: V.scalar_tensor_tensor(out=init[:, 1:2], in0=zic[:, TC - 1:TC], scalar=c5, in1=itmp[:, 1:2], op0=ALU.mult, op1=ALU.add),
              reads=[zic, cosT, itmp], writes=[init])
        xrc = xr[ci % 2]; xic = xi[ci % 2]
        fw.op("pool", lambda: P.tensor_tensor(out=b1[:], in0=zrc[:], in1=cT, op=ALU.mult), reads=[zrc, cosT], writes=[b1])
        fw.op("pool", lambda: P.tensor_tensor(out=b2[:], in0=zic[:], in1=sT, op=ALU.mult), reads=[zic, sinT], writes=[b2])
        fw.op("pool", lambda: P.tensor_tensor(out=xrc[:], in0=b1[:], in1=b2[:], op=ALU.subtract), reads=[b1, b2], writes=[xrc])
        fw.op("dve", lambda: V.tensor_tensor(out=a1[:], in0=zrc[:], in1=sT, op=ALU.mult), reads=[zrc, sinT], writes=[a1])
        fw.op("dve", lambda: V.tensor_tensor(out=a2[:], in0=zic[:], in1=cT, op=ALU.mult), reads=[zic, cosT], writes=[a2])
        fw.op("dve", lambda: V.tensor_tensor(out=xic[:], in0=a1[:], in1=a2[:], op=ALU.add), reads=[a1, a2], writes=[xic])
        py = ps.get()
        fw.op("pe", lambda: TE.matmul(py[0:32, :TC], lhsT=cre_bf[:], rhs=xrc[:], start=True, stop=False), reads=[cre_bf, xrc], writes=[py])
        fw.op("pe", lambda: TE.matmul(py[0:32, :TC], lhsT=ncim_bf[:], rhs=xic[:], start=False, stop=True), reads=[ncim_bf, xic], writes=[py])
        yoc = yo[ci % 2]
        fw.op("dve", lambda: V.scalar_tensor_tensor(out=yoc[:], in0=ucur[:], scalar=d_sb[:, 0:1], in1=py[0:32, :TC], op0=ALU.mult, op1=ALU.add),
              reads=[ucur, d_sb, py], writes=[yoc])
        fw.dma("act", yT[:, ci * TC:(ci + 1) * TC], yoc[:], reads=[yoc])
    fw.finish("sp")
    print("B: ninst", fw.ninst, "nwait", fw.nwait, "dsems", fw.ndsem)
    return nc


def host_B_inputs(d, c, uT_full):
    g2 = [2 * c, 2 * c + 1]
    lr = d['s5_lam_re'][0][g2].reshape(128); li = d['s5_lam_im'][0][g2].reshape(128)
    ld = np.repeat(d['s5_log_dt'][0][g2], 64)
    lamP = np.stack([lr, li, ld], 1).astype(np.float32)
    lamF = np.broadcast_to(np.stack([lr, li, ld], 0)[None], (32, 3, 128)).astype(np.float32).copy()
    BreT = np.zeros((32, 128), np.float32); BimT = np.zeros((32, 128), np.float32)
    CreT = np.zeros((128, 32), np.float32); CimT = np.zeros((128, 32), np.float32)
    for gl in range(2):
        g = g2[gl]
        BreT[gl * 16:(gl + 1) * 16, gl * 64:(gl + 1) * 64] = d['s5_b_re'][0][g].T
        BimT[gl * 16:(gl + 1) * 16, gl * 64:(gl + 1) * 64] = d['s5_b_im'][0][g].T
        CreT[gl * 64:(gl + 1) * 64, gl * 16:(gl + 1) * 16] = d['s5_c_re'][0][g].T
        CimT[gl * 64:(gl + 1) * 64, gl * 16:(gl + 1) * 16] = d['s5_c_im'][0][g].T
    dP = d['s5_d'][0][32 * c:32 * c + 32].reshape(32, 1).astype(np.float32)
    return {"u": np.ascontiguousarray(uT_full[32 * c:32 * c + 32]), "lamP": lamP, "lamF": lamF, "BreT": BreT, "BimT": BimT,
            "CreT": CreT, "CimT": CimT, "dP": dP}


T = 512
NCH = 8
NTOK = T * NCH


def build_CE(mode, NCH=NCH):
    NTOK = T * NCH
    nc = bass.Bass("TRN2", target_bir_lowering=False)
    V, A, P, TE = nc.vector, nc.scalar, nc.gpsimd, nc.tensor
    di = lambda n, s, dt=F32: nc.dram_tensor(n, s, dt, kind="ExternalInput").ap()
    xT = di("xT", [1024, NTOK])
    if mode == "C":
        ysT = di("ysT", [256, NTOK])
        ybT = di("ybT", [768, NTOK], BF16)
        w_glu = di("w_glu", [256, 256])
    else:
        oT = di("oT", [1024, NTOK], BF16)
    w_out = di("w_out", [1024, 1024])
    gm = di("gm", [128, 8])
    w_r = di("w_r", [1024, 20])
    b_r = di("b_r", [20, 1])
    w_gate = di("w_gate", [16, 1024, 256], BF16)
    w_up = di("w_up", [16, 1024, 256], BF16)
    w_down = di("w_down", [16, 256, 1024], BF16)
    outT = nc.dram_tensor("outT", [1024, NTOK], F32, kind="ExternalOutput").ap()
    fw = FW(nc)
    ps = PS(nc)

    def sb(name, shape, dt=F32):
        return nc.alloc_sbuf_tensor(name, shape, dt)
    eps_t = sb("eps", [128, 1])
    fw.op("pool", lambda: P.memset(eps_t[:], EPS), writes=["eps"])
    fw.eps_ap = eps_t[:, 0:1]
    ones_bf = sb("ones_bf", [128, 128], BF16)
    fw.op("pool", lambda: P.memset(ones_bf[:], 1.0), writes=[ones_bf])
    ident = make_ident(fw, nc, F32, "identf")
    esel = sb("esel", [16, 16, 128])
    fw.op("pool", lambda: P.memset(esel[:], 0.0), writes=[esel])
    fw.op("pool", lambda: P.affine_select(out=esel[:], in_=esel[:], pattern=[[-1, 16], [0, 128]],
                                          compare_op=ALU.not_equal, fill=1.0, base=0, channel_multiplier=1),
          reads=[esel], writes=[esel])
    gm_sb = sb("gm_sb", [128, 8]); br_sb = sb("br_sb", [20, 1])
    fw.dma("sp", gm_sb[:], gm, writes=[gm_sb])
    fw.dma("sp", br_sb[:], b_r, writes=[br_sb])
    wr_st = sb("wr_st", [128, 8, 20]); wr_sb = sb("wr_sb", [128, 8, 20])
    fw.dma("sp", wr_st[:], w_r.rearrange("(k p) e -> p k e", p=128), writes=[wr_st])
    for k in range(8):
        fw.op("pool", lambda: P.tensor_scalar(wr_sb[:, k, :], wr_st[:, k, :], gm_sb[:, k:k + 1], None, op0=ALU.mult),
              reads=[wr_st, gm_sb], writes=[wr_sb])
    stA = [sb("stA%d" % i, [128, 8, 256]) for i in range(1)]
    stD = [sb("stD%d" % i, [128, 2, 1024]) for i in range(2)]
    wo_bf = sb("wo_bf", [128, 8, 1024], BF16)
    for k in range(8):
        st = stD[k % 2]
        fw.dma("sp", st[:, 0, :], w_out[k * 128:(k + 1) * 128, :], writes=[st])
        fw.op("dve", lambda: V.tensor_copy(wo_bf[:, k, :], st[:, 0, :]), reads=[st], writes=[("wo_bf", k)])
    if mode == "C":
        wg_bf = sb("wglu_bf", [128, 2, 256], BF16)
        st = stA[0]
        fw.dma("sp", st[:, 0:2, :], w_glu.rearrange("(k p) f -> p k f", p=128), writes=[st])
        fw.op("pool", lambda: P.tensor_copy(wg_bf[:], st[:, 0:2, :]), reads=[st], writes=[wg_bf])
    hT = sb("hT", [128, 8, T]); hn = sb("hn", [128, 8, T], BF16)
    sq = sb("sq", [128, 8, T], BF16); s_sb = sb("s_sb", [128, T]); rstd = sb("rstd", [128, T])
    cat = sb("cat", [128, 8, T], BF16)
    actb = [sb("actb%d" % i, [128, 2, T], BF16) for i in range(2)]
    NWB = 4
    wdb = [sb("wdb%d" % i, [128, 2, 1024], BF16) for i in range(NWB)]
    wgb = [sb("wgb%d" % i, [128, 8, 256], BF16) for i in range(NWB)]
    wub = [sb("wub%d" % i, [128, 8, 256], BF16) for i in range(NWB)]
    lg = sb("lg", [20, T]); Lsb = sb("Lsb", [128, 4, 20]); gate = sb("gate", [128, 4, 16]); gT = sb("gT", [16, T])
    sm = sb("sm", [128, 16]); es = sb("es", [128, 4]); tmp4 = sb("tmp4", [128, 4]); ee = sb("ee", [128, 4])
    ohg = sb("ohg", [128, 4]); sel = sb("sel", [128, 4]); ge = sb("ge", [128, 4])
    sl = [sb("sl%d" % i, [128, T]) for i in range(2)]
    am = [sb("am%d" % i, [128, T]) for i in range(2)]
    if mode == "C":
        ys = sb("ys", [128, 2, T]); x2 = sb("x2", [128, 2, T]); yg = sb("yg", [128, 2, T]); ygb = sb("ygb", [128, 2, T], BF16)
        sgl = sb("sgl", [128, T])
    xv = xT.rearrange("(k p) t -> p k t", p=128)
    ov = outT.rearrange("(k p) t -> p k t", p=128)

    for ci in range(NCH):
        c0 = ci * T
        fw.dma("sp", hT[:], xv[:, :, c0:c0 + T], writes=[hT])
        if mode == "C":
            fw.dma("sp", ys[:], ysT.rearrange("(k p) t -> p k t", p=128)[:, :, c0:c0 + T], writes=[ys])
            fw.dma("sp", cat[:, 2:8, :], ybT.rearrange("(k p) t -> p k t", p=128)[:, :, c0:c0 + T], writes=[("cat", "b")])
            fw.op("dve", lambda: V.tensor_tensor(out=x2[:], in0=ys[:], in1=ys[:], op=ALU.mult), reads=[ys], writes=[x2])
            fw.op("dve", lambda: V.tensor_scalar(x2[:], x2[:], 0.044715, 1.0, op0=ALU.mult, op1=ALU.add), reads=[x2], writes=[x2])
            fw.op("dve", lambda: V.tensor_tensor(out=x2[:], in0=x2[:], in1=ys[:], op=ALU.mult), reads=[x2, ys], writes=[x2])
            fw.op("act", lambda: A.activation(out=x2[:], in_=x2[:], func=AF.Sigmoid, scale=1.5957691216057308), reads=[x2], writes=[x2])
            fw.op("dve", lambda: V.tensor_tensor(out=yg[:], in0=ys[:], in1=x2[:], op=ALU.mult), reads=[ys, x2], writes=[yg])
            fw.op("act", lambda: A.copy(out=ygb[:], in_=yg[:]), reads=[yg], writes=[ygb])
            for j in range(2):
                pb = ps.get()
                for i in range(2):
                    fw.op("pe", lambda: TE.matmul(pb[:, :T], lhsT=wg_bf[:, i, j * 128:(j + 1) * 128], rhs=ygb[:, i, :],
                                                  start=(i == 0), stop=(i == 1)), reads=[wg_bf, ygb], writes=[pb], sig=(i == 1))
                fw.op("act", lambda: A.activation(out=sgl[:], in_=pb[:, :T], func=AF.Sigmoid), reads=[pb], writes=[sgl])
                fw.op("dve", lambda: V.tensor_tensor(out=cat[:, j, :], in0=yg[:, j, :], in1=sgl[:], op=ALU.mult),
                      reads=[yg, sgl], writes=[("cat", "a%d" % j)])
            catres = [("cat", "a0"), ("cat", "a1")] + [("cat", "b")] * 6
        else:
            fw.dma("sp", cat[:], oT.rearrange("(k p) t -> p k t", p=128)[:, :, c0:c0 + T], writes=[("cat", "b")])
            catres = [("cat", "b")] * 8
        for dd in range(8):
            pb = ps.get()
            for k in range(8):
                fw.op("pe", lambda: TE.matmul(pb[:, :T], lhsT=wo_bf[:, k, dd * 128:(dd + 1) * 128], rhs=cat[:, k, :],
                                              start=(k == 0), stop=(k == 7)), reads=[("wo_bf", k), catres[k]], writes=[pb], sig=(k == 7))
            fw.op("dve", lambda: V.tensor_tensor(out=hT[:, dd, :], in0=hT[:, dd, :], in1=pb[:, :T], op=ALU.add),
                  reads=[hT, pb], writes=[hT])
        norm_chunk(fw, nc, ps, hT, hn, T, ones_bf, sq, s_sb, rstd, g=gm_sb)
        hnres = [(hn.name, k) for k in range(8)]
        pl = ps.get()
        for k in range(8):
            fw.op("pe", lambda: TE.matmul(pl[0:20, :T], lhsT=wr_sb[:, k, :], rhs=hT[:, k, :], start=(k == 0), stop=(k == 7)),
                  reads=[wr_sb, hT], writes=[pl], sig=(k == 7))
        fw.op("dve", lambda: V.tensor_tensor(out=lg[:], in0=pl[0:20, :T], in1=rstd[0:20, :], op=ALU.mult), reads=[pl, rstd], writes=[lg])
        fw.op("dve", lambda: V.tensor_scalar(lg[:], lg[:], br_sb[:, 0:1], None, op0=ALU.add), reads=[lg, br_sb], writes=[lg])
        pt = ps.get()
        for j in range(4):
            fw.op("pe", lambda: TE.transpose(pt[:, j * 20:(j + 1) * 20], lg[:, j * 128:(j + 1) * 128], ident[0:20, 0:20]),
                  reads=[lg, ident], writes=[pt])
        fw.op("dve", lambda: V.tensor_copy(Lsb[:].rearrange("p a b -> p (a b)"), pt[:, 0:80]), reads=[pt], writes=[Lsb])
        for j in range(4):
            L = Lsb[:, j, :]
            gl = L[:, 0:4]; el = L[:, 4:20]
            gmax, ngmax, gsum, gw = sm[:, 0:1], sm[:, 1:2], sm[:, 2:3], sm[:, 3:4]
            m1, nm1, m2, e2, wf = sm[:, 4:5], sm[:, 5:6], sm[:, 6:7], sm[:, 7:8], sm[:, 8:9]

            def vo(fn, r=(), w=()):
                fw.op("dve", fn, reads=list(r) + [Lsb, sm], writes=list(w))
            vo(lambda: V.tensor_reduce(out=gmax, in_=gl, axis=AX.X, op=ALU.max), w=[sm])
            vo(lambda: V.tensor_scalar(ngmax, gmax, -1.0, None, op0=ALU.mult), w=[sm])
            fw.op("act", lambda: A.activation(out=ge[:], in_=gl, func=AF.Exp, bias=ngmax, scale=1.0, accum_out=gsum),
                  reads=[Lsb, sm], writes=[ge, sm])
            vo(lambda: V.reciprocal(gw, gsum), w=[sm])
            vo(lambda: V.tensor_scalar(ohg[:], gl, gmax, None, op0=ALU.is_ge), w=[ohg])
            vo(lambda: V.tensor_scalar(es[:], el[:, 0:4], ohg[:, 0:1], None, op0=ALU.mult), r=[ohg], w=[es])
            for g in range(1, 4):
                vo(lambda: V.scalar_tensor_tensor(out=es[:], in0=el[:, 4 * g:4 * g + 4], scalar=ohg[:, g:g + 1], in1=es[:],
                                                  op0=ALU.mult, op1=ALU.add), r=[ohg, es], w=[es])
            vo(lambda: V.tensor_reduce(out=m1, in_=es[:], axis=AX.X, op=ALU.max), r=[es], w=[sm])
            vo(lambda: V.tensor_scalar(tmp4[:], es[:], m1, -1e30, op0=ALU.is_ge, op1=ALU.mult), r=[es], w=[tmp4])
            vo(lambda: V.tensor_tensor(out=tmp4[:], in0=tmp4[:], in1=es[:], op=ALU.add), r=[es, tmp4], w=[tmp4])
            vo(lambda: V.tensor_reduce(out=m2, in_=tmp4[:], axis=AX.X, op=ALU.max), r=[tmp4], w=[sm])
            vo(lambda: V.tensor_scalar(sel[:], es[:], m2, None, op0=ALU.is_ge), r=[es], w=[sel])
            vo(lambda: V.tensor_scalar(nm1, m1, -1.0, None, op0=ALU.mult), w=[sm])
            fw.op("act", lambda: A.activation(out=ee[:], in_=es[:], func=AF.Exp, bias=nm1, scale=1.0), reads=[es, sm], writes=[ee])
            fw.op("act", lambda: A.activation(out=e2, in_=m2, func=AF.Exp, bias=nm1, scale=1.0), reads=[sm], writes=[sm])
            vo(lambda: V.tensor_scalar(e2, e2, 1.0, None, op0=ALU.add), w=[sm])
            vo(lambda: V.reciprocal(e2, e2), w=[sm])
            vo(lambda: V.tensor_tensor(out=wf, in0=e2, in1=gw, op=ALU.mult), w=[sm])
            vo(lambda: V.tensor_tensor(out=ee[:], in0=ee[:], in1=sel[:], op=ALU.mult), r=[ee, sel], w=[ee])
            vo(lambda: V.tensor_scalar(ee[:], ee[:], wf, None, op0=ALU.mult), r=[ee], w=[ee])
            for g in range(4):
                vo(lambda: V.tensor_scalar(gate[:, j, 4 * g:4 * g + 4], ee[:], ohg[:, g:g + 1], None, op0=ALU.mult),
                   r=[ee, ohg], w=[gate])
        pg = ps.get()
        for j in range(4):
            fw.op("pe", lambda: TE.transpose(pg[0:16, j * 128:(j + 1) * 128], gate[:, j, :], ident[:]),
                  reads=[gate, ident], writes=[pg])
        fw.op("dve", lambda: V.tensor_copy(gT[:], pg[0:16, :T]), reads=[pg], writes=[gT])
        for e in range(16):
            wg, wu, wd = wgb[e % NWB], wub[e % NWB], wdb[e % NWB]
            ab = actb[e % 2]
            fw.dma("sp", wg[:], w_gate[e].rearrange("(k p) f -> p k f", p=128), writes=[wg])
            fw.dma("sp", wu[:], w_up[e].rearrange("(k p) f -> p k f", p=128), writes=[wu])
            fw.dma("sp", wd[:], w_down[e].rearrange("(k p) f -> p k f", p=128), writes=[wd])
            pgb = ps.get()
            fw.op("pe", lambda: TE.matmul(pgb[:, :T], lhsT=esel[:, e, :], rhs=gT[:], start=True, stop=True),
                  reads=[esel, gT], writes=[pgb])
            for f in range(2):
                p1 = ps.get(); p3 = ps.get()
                for k in range(8):
                    fw.op("pe", lambda: TE.matmul(p1[:, :T], lhsT=wg[:, k, f * 128:(f + 1) * 128], rhs=hn[:, k, :],
                                                  start=(k == 0), stop=(k == 7)), reads=[wg, hnres[k]], writes=[p1], sig=(k == 7))
                for k in range(8):
                    fw.op("pe", lambda: TE.matmul(p3[:, :T], lhsT=wu[:, k, f * 128:(f + 1) * 128], rhs=hn[:, k, :],
                                                  start=(k == 0), stop=(k == 7)), reads=[wu, hnres[k]], writes=[p3], sig=(k == 7))
                s_ = sl[f]; a_ = am[f]
                fw.op("act", lambda: A.activation(out=s_[:], in_=p1[:, :T], func=AF.Silu), reads=[p1], writes=[s_])
                fw.op("dve", lambda: V.tensor_tensor(out=a_[:], in0=s_[:], in1=p3[:, :T], op=ALU.mult), reads=[s_, p3], writes=[a_])
                fw.op("dve", lambda: V.tensor_tensor(out=ab[:, f, :], in0=a_[:], in1=pgb[:, :T], op=ALU.mult),
                      reads=[a_, pgb], writes=[ab])
            for dd in range(8):
                pb = ps.get()
                for f in range(2):
                    fw.op("pe", lambda: TE.matmul(pb[:, :T], lhsT=wd[:, f, dd * 128:(dd + 1) * 128], rhs=ab[:, f, :],
                                                  start=(f == 0), stop=(f == 1)), reads=[wd, ab], writes=[pb], sig=(f == 1))
                fw.op("dve", lambda: V.tensor_tensor(out=hT[:, dd, :], in0=hT[:, dd, :], in1=pb[:, :T], op=ALU.add),
                      reads=[hT, pb], writes=[hT])
        fw.dma("sp", ov[:, :, c0:c0 + T], hT[:], reads=[hT])
    fw.finish("sp")
    print(mode, ": ninst", fw.ninst, "nwait", fw.nwait, "dsems", fw.ndsem)
    return nc


def host_CE_weights(d, layer, wb=None):
    if wb is not None:
        return {"gm": np.ascontiguousarray(d['moe_norm'][layer].reshape(8, 128).T),
                "w_r": np.ascontiguousarray(np.concatenate([d['moe_w_group'][layer], d['moe_w_expert'][layer]], 1)),
                "b_r": np.concatenate([d['moe_b_group'][layer], d['moe_b_expert'][layer]]).reshape(20, 1).astype(np.float32),
                "w_gate": np.ascontiguousarray(wb[0][layer]), "w_up": np.ascontiguousarray(wb[1][layer]),
                "w_down": np.ascontiguousarray(wb[2][layer])}
    return {"gm": np.ascontiguousarray(d['moe_norm'][layer].reshape(8, 128).T),
            "w_r": np.ascontiguousarray(np.concatenate([d['moe_w_group'][layer], d['moe_w_expert'][layer]], 1)),
            "b_r": np.concatenate([d['moe_b_group'][layer], d['moe_b_expert'][layer]]).reshape(20, 1).astype(np.float32),
            "w_gate": d['moe_w_gate'][layer], "w_up": d['moe_w_up'][layer], "w_down": d['moe_w_down'][layer]}


T = 512
NFM = 14
NORMED = [True] * 10 + [False, False, True, True]


def build_P(NCH=8):
    NT = T * NCH
    nc = bass.Bass("TRN2", target_bir_lowering=False)
    V, A, P, TE = nc.vector, nc.scalar, nc.gpsimd, nc.tensor
    di = lambda n, s, dt=F32: nc.dram_tensor(n, s, dt, kind="ExternalInput").ap()
    hT_in = di("hT", [1024, NT])
    w_fm = di("w_fm", [1024, NFM * 128])
    w_tm = di("w_tm", [1024, 548])
    g1 = di("g1", [128, 8])
    G = di("G", [128, NFM])
    FM = nc.dram_tensor("FM", [NFM * 128, NT], BF16, kind="ExternalOutput").ap()
    TM = nc.dram_tensor("TM", [NT, 512], BF16, kind="ExternalOutput").ap()
    GT = nc.dram_tensor("GT", [NT, 36], F32, kind="ExternalOutput").ap()
    fw = FW(nc)
    ps = PS(nc)

    def sb(name, shape, dt=F32):
        return nc.alloc_sbuf_tensor(name, shape, dt)
    eps_t = sb("eps", [128, 1])
    fw.op("pool", lambda: P.memset(eps_t[:], EPS), writes=["eps"])
    fw.eps_ap = eps_t[:, 0:1]
    ones_bf = sb("ones_bf", [128, 128], BF16)
    fw.op("pool", lambda: P.memset(ones_bf[:], 1.0), writes=[ones_bf])
    blk = sb("blk", [128, 128], BF16)
    fw.op("pool", lambda: P.memset(blk[:], 1.0), writes=[blk])
    fw.op("pool", lambda: P.memset(blk[0:64, 64:128], 0.0), reads=[blk], writes=[blk])
    fw.op("pool", lambda: P.memset(blk[64:128, 0:64], 0.0), reads=[blk], writes=[blk])
    g_sb = sb("g_sb", [128, 8]); G_sb = sb("G_sb", [128, NFM])
    fw.dma("sp", g_sb[:], g1, writes=[g_sb])
    fw.dma("sp", G_sb[:], G, writes=[G_sb])
    wf_bf = sb("wf_bf", [128, 8, NFM * 128], BF16)
    wt_bf = sb("wt_bf", [128, 8, 548], BF16)
    wst = [sb("wst%d" % i, [128, NFM * 128]) for i in range(2)]
    for k in range(8):
        st = wst[k % 2]
        fw.dma("sp", st[:], w_fm[k * 128:(k + 1) * 128, :], writes=[st])
        fw.op("dve", lambda: V.tensor_scalar(wf_bf[:, k, :], st[:], g_sb[:, k:k + 1], None, op0=ALU.mult),
              reads=[st, g_sb], writes=[("wf", k)])
    for k in range(8):
        st = wst[k % 2]
        fw.dma("sp", st[:, 0:548], w_tm[k * 128:(k + 1) * 128, :], writes=[st])
        fw.op("dve", lambda: V.tensor_scalar(wt_bf[:, k, :], st[:, 0:548], g_sb[:, k:k + 1], None, op0=ALU.mult),
              reads=[st, g_sb], writes=[("wt", k)])
    hT = [sb("hT%d" % i, [128, 8, T]) for i in range(2)]
    hn = sb("hn", [128, 8, T], BF16)
    sq = sb("sq", [128, 8, T], BF16); s_sb = sb("s_sb", [128, T]); rstd = sb("rstd", [128, T])
    sq2 = [sb("sq2_%d" % i, [128, T], BF16) for i in range(2)]
    s2 = [sb("s2_%d" % i, [128, T]) for i in range(2)]
    fo = [sb("fo%d" % i, [128, T], BF16) for i in range(3)]
    to = [sb("to%d" % i, [128, 512], BF16) for i in range(2)]
    go = [sb("go%d" % i, [128, 36]) for i in range(2)]
    hv = hT_in.rearrange("(k p) t -> p k t", p=128)
    fw.dma("sp", hT[0][:], hv[:, :, 0:T], writes=[hT[0]])
    cnt = 0
    for ci in range(NCH):
        c0 = ci * T
        h = hT[ci % 2]
        if ci + 1 < NCH:
            fw.dma("sp", hT[(ci + 1) % 2][:], hv[:, :, c0 + T:c0 + 2 * T], writes=[hT[(ci + 1) % 2]])
        norm_chunk(fw, nc, ps, h, hn, T, ones_bf, sq, s_sb, rstd)
        for i in range(NFM):
            pb = ps.get()
            for k in range(8):
                fw.op("pe", lambda: TE.matmul(pb[:, :T], lhsT=wf_bf[:, k, i * 128:(i + 1) * 128], rhs=hn[:, k, :],
                                              start=(k == 0), stop=(k == 7)), reads=[("wf", k), (hn.name, k)], writes=[pb], sig=(k == 7))
            o = fo[cnt % 3]
            if NORMED[i]:
                q2 = sq2[cnt % 2]; s_ = s2[cnt % 2]
                fw.op("act", lambda: A.activation(out=q2[:], in_=pb[:, :T], func=AF.Square), reads=[pb], writes=[q2])
                p2 = ps.get()
                fw.op("pe", lambda: TE.matmul(p2[:, :T], lhsT=blk[:], rhs=q2[:], start=True, stop=True), reads=[blk, q2], writes=[p2])
                fw.op("act", lambda: A.activation(out=s_[:], in_=p2[:, :T], func=AF.Sqrt, scale=1.0 / 64, bias=fw.eps_ap),
                      reads=[p2, "eps"], writes=[s_])
                fw.op("dve", lambda: V.reciprocal(s_[:], s_[:]), reads=[s_], writes=[s_])
                fw.op("dve", lambda: V.scalar_tensor_tensor(out=o[:], in0=pb[:, :T], scalar=G_sb[:, i:i + 1], in1=s_[:],
                                                            op0=ALU.mult, op1=ALU.mult), reads=[pb, G_sb, s_], writes=[o])
            else:
                fw.op("act", lambda: A.copy(out=o[:], in_=pb[:, :T]), reads=[pb], writes=[o])
            fw.dma("act", FM[i * 128:(i + 1) * 128, c0:c0 + T], o[:], reads=[o])
            cnt += 1
        for j in range(4):
            pb = ps.get()
            for k in range(8):
                fw.op("pe", lambda: TE.matmul(pb[:, 0:512], lhsT=hn[:, k, j * 128:(j + 1) * 128], rhs=wt_bf[:, k, 0:512],
                                              start=(k == 0), stop=(k == 7)), reads=[("wt", k), (hn.name, k)], writes=[pb], sig=(k == 7))
            t_ = to[j % 2]
            fw.op("act", lambda: A.copy(out=t_[:], in_=pb[:, 0:512]), reads=[pb], writes=[t_])
            fw.dma("act", TM[c0 + j * 128:c0 + (j + 1) * 128, :], t_[:], reads=[t_])
            pg = ps.get()
            for k in range(8):
                fw.op("pe", lambda: TE.matmul(pg[:, 0:36], lhsT=hn[:, k, j * 128:(j + 1) * 128], rhs=wt_bf[:, k, 512:548],
                                              start=(k == 0), stop=(k == 7)), reads=[("wt", k), (hn.name, k)], writes=[pg], sig=(k == 7))
            g_ = go[j % 2]
            fw.op("act", lambda: A.activation(out=g_[:], in_=pg[:, 0:36], func=AF.Sigmoid), reads=[pg], writes=[g_])
            fw.dma("act", GT[c0 + j * 128:c0 + (j + 1) * 128, :], g_[:], reads=[g_])
    fw.finish("sp")
    print("P: ninst", fw.ninst, "nwait", fw.nwait, "dsems", fw.ndsem)
    return nc


FM_COLS = np.concatenate([np.arange(0, 512), np.arange(768, 1536), np.arange(1536, 1792), np.arange(1792, 1920), np.arange(2048, 2176)])
TM_COLS = np.concatenate([np.arange(512, 768), np.arange(1920, 2048), np.arange(2176, 2304), np.arange(2304, 2340)])


def host_P_weights(d):
    w = d['od_w_in'][0]
    t2 = lambda v: np.tile(v, 2)
    one = np.ones(128, np.float32)
    cols = [t2(d['moba_q_norm'][0])] * 2 + [t2(d['moba_k_norm'][0])] * 2 + [t2(d['nsa_q_norm'][0])] * 6 + [one, one] + \
           [t2(d['nsa_ksel_norm'][0]), t2(d['nsa_kwin_norm'][0])]
    return {"w_fm": np.ascontiguousarray(w[:, FM_COLS]), "w_tm": np.ascontiguousarray(w[:, TM_COLS]),
            "g1": np.ascontiguousarray(d['od_norm_mix'][0].reshape(8, 128).T),
            "G": np.ascontiguousarray(np.stack(cols, 1).astype(np.float32))}


L = 16384
NKT = 128
R_KM, R_QM, R_QD, R_KC, R_VC, R_KS, R_KW = 0, 64, 128, 512, 576, 640, 704
C_VM, C_VS, C_VW = 0, 64, 128
NFR = 768
GELU_S = 1.5957691216057308


def asel(fw, nc, ap, pattern, op, fill, base, cm, reads, writes):
    P = nc.gpsimd
    npart = ap.shape[0]
    lo = base + min(0, cm * (npart - 1)) + sum(min(0, st * (n - 1)) for st, n in pattern)
    hi = base + max(0, cm * (npart - 1)) + sum(max(0, st * (n - 1)) for st, n in pattern)
    if op == ALU.is_ge:
        all_t, all_f = lo >= 0, hi < 0
    elif op == ALU.is_gt:
        all_t, all_f = lo > 0, hi <= 0
    else:
        all_t, all_f = False, False
    if all_t:
        return
    if all_f:
        fw.op("pool", lambda: P.memset(ap, fill), reads=reads, writes=writes)
        return
    regs = fw.__dict__.setdefault("fill_regs", {})
    if fill not in regs:
        regs[fill] = P.to_reg(float(fill))
    fr = regs[fill]
    fw.op("pool", lambda: P.affine_select(out=ap, in_=ap, pattern=pattern, compare_op=op, fill=fr, base=base,
                                          channel_multiplier=cm), reads=reads, writes=writes)


def build_D(groups=tuple(range(32)), do_moba=True, do_nsa=True):
    nc = bass.Bass("TRN2", target_bir_lowering=False)
    V, A, P, TE = nc.vector, nc.scalar, nc.gpsimd, nc.tensor
    di = lambda n, s, dt=F32: nc.dram_tensor(n, s, dt, kind="ExternalInput").ap()
    FM = di("FM", [NFR, L], BF16)
    TM = di("TM", [L, 192], BF16)
    GT = di("GT", [L, 9])
    peT = di("peT", [2, 64, 32])
    w1 = di("w1", [2, 2048, 256])
    w2 = di("w2", [2, 256, 64])
    gk = di("gk", [64, 1])
    NS = len(groups)
    Oout = nc.dram_tensor("Oout", [NS * 512, 256], BF16, kind="ExternalOutput").ap()
    fw = FW(nc)

    def sb(name, shape, dt=F32):
        return nc.alloc_sbuf_tensor(name, shape, dt)
    psS = [nc.alloc_psum_tensor("psS%d" % i, [128, 512], F32) for i in range(3)]
    psA = [nc.alloc_psum_tensor("psA%d" % i, [128, 512], F32) for i in range(3)]
    psM = [nc.alloc_psum_tensor("psM%d" % i, [128, 512], F32) for i in range(1)]
    psB = nc.alloc_psum_tensor("psB", [128, 1024], BF16)
    cS = [0]; cM = [0]

    def getS():
        cS[0] += 1
        return psS[cS[0] % 3]

    def getM():
        cM[0] += 1
        return psM[cM[0] % len(psM)]
    eps_t = sb("eps", [128, 1])
    fw.op("pool", lambda: P.memset(eps_t[:], EPS), writes=["eps"])
    identf = make_ident(fw, nc, F32, "identf")
    identb = sb("identb", [128, 128], BF16)
    fw.op("dve", lambda: V.tensor_copy(identb[:], identf[:]), reads=[identf], writes=[identb])
    ones_bf = sb("ones_bf", [128, 128], BF16)
    fw.op("pool", lambda: P.memset(ones_bf[:], 1.0), writes=[ones_bf])
    sel64 = sb("sel64", [65, 128])
    fw.op("pool", lambda: P.memset(sel64[:], 0.0), writes=[sel64])
    fw.op("pool", lambda: P.memset(sel64[64:65, :], 1.0), reads=[sel64], writes=[sel64])
    tri = sb("tri", [128, 4, 512], BF16)
    wmask = sb("wmask", [128, 4, 512], BF16)
    fw.op("pool", lambda: P.memset(tri[:], 1.0), writes=[tri])
    fw.op("pool", lambda: P.memset(wmask[:], 1.0), writes=[wmask])
    for rel in range(4):
        fw.op("pool", lambda: P.affine_select(out=tri[:, rel, :], in_=tri[:, rel, :], pattern=[[1, 512]], compare_op=ALU.is_ge,
                                              fill=0.0, base=-128 * rel, channel_multiplier=-1), reads=[tri], writes=[tri])
        rr = rel - 4
        fw.op("pool", lambda: P.affine_select(out=wmask[:, rel, :], in_=wmask[:, rel, :], pattern=[[-1, 512]], compare_op=ALU.is_gt,
                                              fill=0.0, base=512 + 128 * rr, channel_multiplier=1), reads=[wmask], writes=[wmask])
    Kaug = sb("Kaug", [128, L], BF16)
    Vext = sb("Vext", [128, NKT, 65], BF16)
    fw.op("pool", lambda: P.memset(Vext[:, :, 64:65], 1.0), writes=[Vext])
    Rw = [sb("R%d" % w, [128, 6, 512], BF16) for w in range(4)]
    Pb = [sb("Pb%d" % i, [128, 512], BF16) for i in range(3)]
    cP = [0]
    OT_sb = sb("OT_sb", [65, 512])
    rl = sb("rl", [128, 4]); rg = sb("rg", [128, 4])
    o_acc = sb("o_acc", [128, 4, 192])
    o_bf = sb("o_bf", [128, 4, 192], BF16)
    MT = sb("MT", [128, 320], BF16)
    fw.op("pool", lambda: P.memset(MT[:], 0.0), writes=[MT])
    sc = sb("sc", [128, 256]); top8 = sb("top8", [128, 8])
    gt = sb("gt", [128, 4, 9])

    def exp_to(S, n=512):
        cP[0] += 1
        pb = Pb[cP[0] % 3]
        fw.op("act", lambda: A.activation(out=pb[:, :n], in_=S[:, :n], func=AF.Exp, scale=0.125), reads=[S], writes=[pb])
        return pb

    def finalize(acc, dst_cols, first, gate_col=None, dst=None):
        dst = o_acc if dst is None else dst
        fw.op("act", lambda: A.copy(out=OT_sb[:], in_=acc[0:65, :]), reads=[acc], writes=[OT_sb])
        pm = getM()
        for j in range(4):
            fw.op("pe", lambda: TE.transpose(pm[:, j * 65:(j + 1) * 65], OT_sb[0:65, j * 128:(j + 1) * 128], identf[0:65, 0:65]),
                  reads=[OT_sb, identf], writes=[pm])
        fw.op("dve", lambda: V.tensor_scalar(rl[:], pm[:, 64:260:65], 1e-30, None, op0=ALU.max), reads=[pm], writes=[rl])
        fw.op("dve", lambda: V.reciprocal(rl[:], rl[:]), reads=[rl], writes=[rl])
        if gate_col is not None:
            fw.op("dve", lambda: V.tensor_tensor(out=rg[:], in0=rl[:], in1=gt[:, :, gate_col], op=ALU.mult), reads=[rl, gt], writes=[rg])
            sc_ = rg
        else:
            sc_ = rl
        c0, c1 = dst_cols
        for j in range(4):
            if first:
                fw.op("dve", lambda: V.tensor_scalar(dst[:, j, c0:c1], pm[:, j * 65:j * 65 + 64], sc_[:, j:j + 1], None, op0=ALU.mult),
                      reads=[pm, sc_], writes=[dst])
            else:
                fw.op("dve", lambda: V.scalar_tensor_tensor(out=dst[:, j, c0:c1], in0=pm[:, j * 65:j * 65 + 64], scalar=sc_[:, j:j + 1],
                                                            in1=dst[:, j, c0:c1], op0=ALU.mult, op1=ALU.add),
                      reads=[pm, sc_, dst], writes=[dst])


    def pipe(steps, look=2):
        Ss = {}
        n = len(steps)
        for i in range(n + look):
            if i < n:
                S = getS()
                steps[i][0](S)
                Ss[i] = S
            j = i - look
            if j >= 0:
                steps[j][1](Ss.pop(j))

    if do_nsa:
        KcT = sb("KcT", [64, 1, 1024], BF16)
        Vc_ext = sb("Vc_ext", [128, 1, 8, 65], BF16)
        fw.op("pool", lambda: P.memset(Vc_ext[:], 0.0), writes=[Vc_ext])
        fw.op("pool", lambda: P.memset(Vc_ext[:, :, :, 64:65], 1.0), reads=[Vc_ext], writes=[Vc_ext])
        fw.op("pool", lambda: P.memset(KcT[:], 0.0), writes=[KcT])
        Amat = sb("Amat", [128, 8, 256], BF16)
        fw.op("pool", lambda: P.memset(Amat[:], 1.0), writes=[Amat])
        fw.op("pool", lambda: P.affine_select(out=Amat[:], in_=Amat[:], pattern=[[128, 8], [-4, 256]], compare_op=ALU.is_ge, fill=0.0,
                                              base=1, channel_multiplier=1), reads=[Amat], writes=[Amat])
        fw.op("pool", lambda: P.affine_select(out=Amat[:], in_=Amat[:], pattern=[[-128, 8], [4, 256]], compare_op=ALU.is_ge, fill=0.0,
                                              base=3, channel_multiplier=-1), reads=[Amat], writes=[Amat])
        gk_sb = sb("gk_sb", [64, 1])
        fw.dma("sp", gk_sb[:], gk, writes=[gk_sb])
        w1st = sb("w1st", [64, 8, 256]); w1b = sb("w1b", [64, 32, 256], BF16)
        w2st = sb("w2st", [128, 2, 64]); w2b = sb("w2b", [128, 2, 64], BF16)
        pest = sb("pest", [64, 32]); peb = sb("peb", [64, 32], BF16)
        hb = sb("hb", [128, 2])
        hid = sb("hid", [128, 2, 1024], BF16)
        hx = sb("hx", [128, 512]); hx2 = sb("hx2", [128, 512])
        ksq = sb("ksq", [64, 512], BF16); ks_ = sb("ks_", [64, 512])
        fw.op("pool", lambda: P.memset(hid[:], 0.0), writes=[hid])
        for kv in range(2):
            for half in range(4):
                fw.dma("sp", w1st[:], w1[kv].rearrange("(r d) f -> d r f", d=64)[:, half * 8:(half + 1) * 8, :], writes=[w1st])
                fw.op("dve", lambda: V.tensor_copy(w1b[:, half * 8:(half + 1) * 8, :], w1st[:]), reads=[w1st], writes=[w1b])
            fw.dma("sp", w2st[:], w2[kv].rearrange("(k p) f -> p k f", p=128), writes=[w2st])
            fw.op("dve", lambda: V.tensor_copy(w2b[:], w2st[:]), reads=[w2st], writes=[w2b])
            fw.dma("sp", pest[:], peT[kv], writes=[pest])
            fw.op("dve", lambda: V.tensor_copy(peb[:], pest[:]), reads=[pest], writes=[peb])
            pm = getM()
            for hh in range(2):
                for r_ in range(32):
                    fw.op("pe", lambda: TE.matmul(pm[:, hh:hh + 1], lhsT=w1b[:, r_, hh * 128:(hh + 1) * 128], rhs=peb[:, r_:r_ + 1],
                                                  start=(r_ == 0), stop=(r_ == 31)), reads=[w1b, peb], writes=[pm], sig=(r_ == 31))
            fw.op("dve", lambda: V.tensor_copy(hb[:], pm[:, 0:2]), reads=[pm], writes=[hb])
            for kvh in range(1):
                row0 = (R_KC if kv == 0 else R_VC)
                fw.dma("sp", Kaug[0:64, :], FM[row0:row0 + 64, :], writes=[Kaug])
                for (n0, cnt) in ((0, 512), (512, 511)):
                    for hh in range(2):
                        S = getS()
                        for r_ in range(32):
                            st0 = r_ + 16 * n0
                            fw.op("pe", lambda: TE.matmul(S[:, :cnt], lhsT=w1b[:, r_, hh * 128:(hh + 1) * 128],
                                                          rhs=Kaug[0:64, st0:st0 + 16 * (cnt - 1) + 1:16],
                                                          start=(r_ == 0), stop=(r_ == 31)), reads=[w1b, Kaug], writes=[S], sig=(r_ == 31))
                        fw.op("act", lambda: A.activation(out=hx[:, :cnt], in_=S[:, :cnt], func=AF.Identity, bias=hb[:, hh:hh + 1], scale=1.0),
                              reads=[S, hb], writes=[hx])
                        fw.op("dve", lambda: V.tensor_tensor(out=hx2[:, :cnt], in0=hx[:, :cnt], in1=hx[:, :cnt], op=ALU.mult), reads=[hx], writes=[hx2])
                        fw.op("dve", lambda: V.tensor_scalar(hx2[:, :cnt], hx2[:, :cnt], 0.044715, 1.0, op0=ALU.mult, op1=ALU.add), reads=[hx2], writes=[hx2])
                        fw.op("dve", lambda: V.tensor_tensor(out=hx2[:, :cnt], in0=hx2[:, :cnt], in1=hx[:, :cnt], op=ALU.mult), reads=[hx2, hx], writes=[hx2])
                        fw.op("act", lambda: A.activation(out=hx2[:, :cnt], in_=hx2[:, :cnt], func=AF.Sigmoid, scale=GELU_S), reads=[hx2], writes=[hx2])
                        fw.op("dve", lambda: V.tensor_tensor(out=hid[:, hh, n0:n0 + cnt], in0=hx[:, :cnt], in1=hx2[:, :cnt], op=ALU.mult),
                              reads=[hx, hx2], writes=[hid])
                if kv == 0:
                    for (n0, cnt) in ((0, 512), (512, 511)):
                        S = getS()
                        for hh in range(2):
                            fw.op("pe", lambda: TE.matmul(S[0:64, :cnt], lhsT=w2b[:, hh, :], rhs=hid[:, hh, n0:n0 + cnt],
                                                          start=(hh == 0), stop=(hh == 1)), reads=[w2b, hid], writes=[S], sig=(hh == 1))
                        fw.op("act", lambda: A.activation(out=ksq[:, :cnt], in_=S[0:64, :cnt], func=AF.Square), reads=[S], writes=[ksq])
                        S2 = getS()
                        fw.op("pe", lambda: TE.matmul(S2[0:64, :cnt], lhsT=ones_bf[0:64, 0:64], rhs=ksq[:, :cnt], start=True, stop=True),
                              reads=[ones_bf, ksq], writes=[S2])
                        fw.op("act", lambda: A.activation(out=ks_[:, :cnt], in_=S2[0:64, :cnt], func=AF.Sqrt, scale=1.0 / 64, bias=eps_t[0:64, 0:1]),
                              reads=[S2, "eps"], writes=[ks_])
                        fw.op("dve", lambda: V.reciprocal(ks_[:, :cnt], ks_[:, :cnt]), reads=[ks_], writes=[ks_])
                        fw.op("dve", lambda: V.scalar_tensor_tensor(out=KcT[:, kvh, n0:n0 + cnt], in0=S[0:64, :cnt], scalar=gk_sb[:, 0:1],
                                                                    in1=ks_[:, :cnt], op0=ALU.mult, op1=ALU.mult),
                              reads=[S, gk_sb, ks_], writes=[KcT])
                else:
                    for nt_ in range(8):
                        cnt = 128 if nt_ < 7 else 127
                        pm = getM()
                        for hh in range(2):
                            fw.op("pe", lambda: TE.matmul(pm[0:cnt, 0:64], lhsT=hid[:, hh, nt_ * 128:nt_ * 128 + cnt], rhs=w2b[:, hh, :],
                                                          start=(hh == 0), stop=(hh == 1)), reads=[hid, w2b], writes=[pm], sig=(hh == 1))
                        fw.op("act", lambda: A.copy(out=Vc_ext[0:cnt, kvh, nt_, 0:64], in_=pm[0:cnt, 0:64]), reads=[pm], writes=[Vc_ext])

    if do_moba:
        fw.op("pool", lambda: P.memset(Kaug[64:128, :], 1.0), reads=[Kaug], writes=[("Kaug", "E")])
        fw.op("pool", lambda: P.affine_select(out=Kaug[64:128, :], in_=Kaug[64:128, :], pattern=[[1, L]], compare_op=ALU.is_ge, fill=0.0,
                                              base=0, channel_multiplier=-256), reads=[("Kaug", "E")], writes=[("Kaug", "E")])
        fw.op("pool", lambda: P.affine_select(out=Kaug[64:128, :], in_=Kaug[64:128, :], pattern=[[-1, L]], compare_op=ALU.is_ge, fill=0.0,
                                              base=255, channel_multiplier=256), reads=[("Kaug", "E")], writes=[("Kaug", "E")])
        kmf = sb("kmf", [64, 64]); kmb = sb("kmb", [64, 64], BF16)
        gsc = sb("gsc", [128, 64])
        om = sb("om", [128, 4, 64]); omb = sb("omb", [128, 4, 64], BF16)
        for h in range(1):
            fw.dma("sp", Kaug[0:64, :], FM[R_KM + 64 * h:R_KM + 64 * h + 64, :], writes=[Kaug])
            fw.dma("sp", Vext[:, :, 0:64], TM[:, C_VM + 64 * h:C_VM + 64 * h + 64].rearrange("(kt p) c -> p kt c", p=128), writes=[Vext])
            fw.op("dve", lambda: V.tensor_reduce(out=kmf[:], in_=Kaug[0:64, :].rearrange("p (n s) -> p n s", s=256), axis=AX.X, op=ALU.add),
                  reads=[Kaug], writes=[kmf])
            fw.op("dve", lambda: V.tensor_scalar(kmb[:], kmf[:], 1.0 / 256, None, op0=ALU.mult), reads=[kmf], writes=[kmb])
            for si, j_ in enumerate(groups):
                G = j_
                t0 = 512 * G
                R0 = Rw[0]
                fw.dma("sp", R0[0:64, 0, :], FM[R_QM + 64 * h:R_QM + 64 * h + 64, t0:t0 + 512], writes=[("R", 0, "q")])
                for j in range(4):
                    cur = 2 * G + j // 2
                    pm = getM()
                    fw.op("pe", lambda: TE.matmul(pm[:, 0:64], lhsT=R0[0:64, 0, j * 128:(j + 1) * 128], rhs=kmb[:], start=True, stop=True),
                          reads=[("R", 0, "q"), kmb], writes=[pm])
                    fw.op("act", lambda: A.copy(out=gsc[:], in_=pm[:, 0:64]), reads=[pm], writes=[gsc])
                    asel(fw, nc, gsc[:], [[-1, 64]], ALU.is_ge, -1e9, cur - 1, 0, [gsc], [gsc])
                    fw.op("dve", lambda: V.max(out=top8[:], in_=gsc[:]), reads=[gsc], writes=[top8])
                    asel(fw, nc, gsc[:], [[-1, 64]], ALU.is_ge, 1e9, cur - 1, 0, [gsc, top8], [gsc])
                    fw.op("dve", lambda: V.tensor_scalar(MT[:, 64:128], gsc[:], top8[:, 2:3], 30000.0, op0=ALU.is_ge, op1=ALU.mult),
                          reads=[gsc, top8], writes=[MT])
                    fw.op("dve", lambda: V.tensor_scalar(MT[:, 64:128], MT[:, 64:128], -30000.0, None, op0=ALU.add), reads=[MT], writes=[MT])
                    fw.op("pe", lambda: TE.transpose(psB[:, 0:128], MT[:, 0:128], identb[:]), reads=[MT, identb], writes=[psB])
                    fw.op("act", lambda: A.copy(out=R0[64:128, 0, j * 128:(j + 1) * 128], in_=psB[64:128, 0:128]), reads=[psB], writes=[("R", 0, "m")])
                acc = psA[0]
                nk = 4 * G + 4
                steps = []
                for kt in range(nk):
                    def qk(S, kt=kt):
                        fw.op("pe", lambda: TE.matmul(S[:, :], lhsT=Kaug[:, kt * 128:(kt + 1) * 128], rhs=R0[:, 0, :], start=True, stop=True),
                              reads=[Kaug, ("Kaug", "E"), ("R", 0, "q"), ("R", 0, "m")], writes=[S])

                    def post(S, kt=kt, G=G, nk=nk, acc=acc):
                        pb = exp_to(S)
                        if kt >= 4 * G:
                            fw.op("dve", lambda: V.tensor_tensor(out=pb[:], in0=pb[:], in1=tri[:, kt - 4 * G, :], op=ALU.mult), reads=[pb, tri], writes=[pb])
                        fw.op("pe", lambda: TE.matmul(acc[0:65, :], lhsT=Vext[:, kt, :], rhs=pb[:], start=(kt == 0), stop=(kt == nk - 1)),
                              reads=[Vext, pb], writes=[acc])
                    steps.append((qk, post))
                pipe(steps)
                finalize(acc, (0, 64), True, None, dst=om)
                fw.op("act", lambda: A.copy(out=omb[:], in_=om[:]), reads=[om], writes=[omb])
                fw.dma("act", Oout[si * 512:(si + 1) * 512, 64 * h:64 * h + 64].rearrange("(j p) c -> p j c", p=128), omb[:], reads=[omb])

    if do_nsa:
        fw.op("pool", lambda: P.memset(Kaug[64:128, :], 1.0), reads=[Kaug, ("Kaug", "E")], writes=[("Kaug", "E")])
        fw.op("pool", lambda: P.affine_select(out=Kaug[64:128, :].rearrange("p (w a x) -> p w a x", w=4, a=64),
                                              in_=Kaug[64:128, :].rearrange("p (w a x) -> p w a x", w=4, a=64),
                                              pattern=[[0, 4], [1, 64], [0, 64]], compare_op=ALU.is_equal, fill=0.0,
                                              base=0, channel_multiplier=-1), reads=[("Kaug", "E")], writes=[("Kaug", "E")])
        Pc_all = sb("Pc_all", [128, 8, 6, 512], BF16)
        keepb = [sb("keepb%d" % i, [128, 4, 256]) for i in range(2)]
        biasb = [sb("biasb%d" % i, [128, 4, 256]) for i in range(2)]

        def gen_masks(G, bi):
            kp, bs = keepb[bi], biasb[bi]
            fw.op("pool", lambda: P.memset(kp[:], 1.0), writes=[kp])
            fw.op("pool", lambda: P.memset(bs[:], -1e9), writes=[bs])
            for j in range(4):
                for hf in range(2):
                    cur = 8 * G + 2 * j + hf
                    rows = slice(64 * hf, 64 * hf + 64)
                    asel(fw, nc, kp[rows, j, :], [[-1, 256]], ALU.is_ge, 0.0, cur - 2, 0, [kp], [kp])
                    asel(fw, nc, bs[rows, j, :], [[1, 256]], ALU.is_ge, 1e4, -cur - 1, 0, [bs], [bs])
                    asel(fw, nc, bs[rows, j, :], [[1, 256]], ALU.is_ge, 0.0, -(cur - 1), 0, [bs], [bs])
            fw.op("pool", lambda: P.memset(kp[:, :, 0:1], 0.0), reads=[kp], writes=[kp])
            fw.op("pool", lambda: P.memset(bs[:, :, 0:1], 1e4), reads=[bs], writes=[bs])
        gen_masks(groups[0], 0)
        rlb = sb("rlb", [128, 512])
        Kw = sb("Kw", [64, 1024], BF16)
        Vw = sb("Vw", [128, 8, 65], BF16)
        fw.op("pool", lambda: P.memset(Vw[:, :, 64:65], 1.0), writes=[Vw])
        for kvh in range(1):
            fw.dma("sp", Kaug[0:64, :], FM[R_KS + 64 * kvh:R_KS + 64 * kvh + 64, :], writes=[Kaug])
            fw.dma("sp", Vext[:, :, 0:64], TM[:, C_VS + 64 * kvh:C_VS + 64 * kvh + 64].rearrange("(kt p) c -> p kt c", p=128), writes=[Vext])
            for si, j_ in enumerate(groups):
                G = j_
                t0 = 512 * G
                wmax = (8 * G + 7) // 64
                for w in range(wmax + 1):
                    ng = 6 if w == 0 else 3
                    fw.dma("sp", Rw[w][0:64, 0:ng, :], FM[R_QD:R_QD + 64 * ng, t0:t0 + 512].rearrange("(g d) t -> d g t", d=64), writes=[("R", w, "q")])
                fw.dma("sp", gt[:], GT[t0:t0 + 512, :].rearrange("(j p) c -> p j c", p=128), writes=[gt])
                R0 = Rw[0]
                nkc = (32 * G + 30) // 128 + 1
                for half in range(2):
                    steps = []
                    for kc in range(nkc):
                        for gi in range(3):
                            g = half * 3 + gi

                            def qk(S, kc=kc, g=g):
                                fw.op("pe", lambda: TE.matmul(S[:, :], lhsT=KcT[:, kvh, kc * 128:(kc + 1) * 128], rhs=R0[0:64, g, :], start=True, stop=True),
                                      reads=[KcT, ("R", 0, "q")], writes=[S])

                            def post(S, kc=kc, g=g, gi=gi, nkc=nkc, t0=t0):
                                pc = Pc_all[:, kc, g, :]
                                fw.op("act", lambda: A.activation(out=pc, in_=S[:, :], func=AF.Exp, scale=0.125), reads=[S], writes=[("Pc", kc, g)])
                                asel(fw, nc, pc, [[1, 512]], ALU.is_ge, 0.0, t0 - 31 - 2048 * kc, -16, [("Pc", kc, g)], [("Pc", kc, g)])
                                fw.op("pe", lambda: TE.matmul(psA[gi][0:65, :], lhsT=Vc_ext[:, kvh, kc, :], rhs=pc, start=(kc == 0), stop=(kc == nkc - 1)),
                                      reads=[Vc_ext, ("Pc", kc, g)], writes=[psA[gi]])
                            steps.append((qk, post))
                    pipe(steps)
                    for gi in range(3):
                        g = half * 3 + gi
                        if half == 0:
                            finalize(psA[gi], (g * 64, g * 64 + 64), True, gate_col=0 * 3 + g)
                        else:
                            fw.op("act", lambda: A.copy(out=OT_sb[:], in_=psA[gi][0:65, :]), reads=[psA[gi]], writes=[OT_sb])
                        pm = getM()
                        fw.op("pe", lambda: TE.matmul(pm[:, :], lhsT=sel64[:], rhs=OT_sb[:], start=True, stop=True), reads=[sel64, OT_sb], writes=[pm])
                        fw.op("dve", lambda: V.tensor_scalar(rlb[:], pm[:, :], 1e-30, None, op0=ALU.max), reads=[pm], writes=[rlb])
                        fw.op("dve", lambda: V.reciprocal(rlb[:], rlb[:]), reads=[rlb], writes=[rlb])
                        for kc in range(nkc):
                            pc = Pc_all[:, kc, g, :]
                            fw.op("dve", lambda: V.tensor_tensor(out=pc, in0=pc, in1=rlb[:], op=ALU.mult), reads=[("Pc", kc, g), rlb], writes=[("Pc", kc, g)])
                for j in range(4):
                    pm = getM()
                    n_mm = nkc * 6
                    i_mm = 0
                    for kc in range(nkc):
                        for g in range(6):
                            fw.op("pe", lambda: TE.matmul(pm[:, 0:256], lhsT=Pc_all[:, kc, g, j * 128:(j + 1) * 128], rhs=Amat[:, kc, :],
                                                          start=(i_mm == 0), stop=(i_mm == n_mm - 1)), reads=[("Pc", kc, g), Amat], writes=[pm], sig=(i_mm == n_mm - 1))
                            i_mm += 1
                    kp, bs = keepb[si % 2], biasb[si % 2]
                    fw.op("dve", lambda: V.tensor_tensor(out=sc[:], in0=pm[:, 0:256], in1=kp[:, j, :], op=ALU.mult), reads=[pm, kp], writes=[sc])
                    fw.op("dve", lambda: V.tensor_tensor(out=sc[:], in0=sc[:], in1=bs[:, j, :], op=ALU.add), reads=[sc, bs], writes=[sc])
                    fw.op("dve", lambda: V.max(out=top8[:], in_=sc[:]), reads=[sc], writes=[top8])
                    fw.op("dve", lambda: V.tensor_scalar(MT[:, 64:320], sc[:], top8[:, 7:8], -30000.0, op0=ALU.is_lt, op1=ALU.mult),
                          reads=[sc, top8], writes=[MT])
                    for w in range(wmax + 1):
                        fw.op("pe", lambda: TE.transpose(psB[:, 0:128], MT[:, 64 * w:64 * w + 128], identb[:]), reads=[MT, identb], writes=[psB])
                        for g in range(3):
                            eng = "act" if g % 2 == 0 else "dve"
                            if eng == "act":
                                fw.op("act", lambda: A.copy(out=Rw[w][64:128, g, j * 128:(j + 1) * 128], in_=psB[64:128, 0:128]),
                                      reads=[psB], writes=[("R", w, "m")])
                            else:
                                fw.op("dve", lambda: V.tensor_copy(Rw[w][64:128, g, j * 128:(j + 1) * 128], psB[64:128, 0:128]),
                                      reads=[psB], writes=[("R", w, "m")])
                if si + 1 < len(groups):
                    gen_masks(groups[si + 1], (si + 1) % 2)
                nk = 4 * G + 4
                for half in range(1):
                    steps = []
                    for kt in range(nk):
                        w = kt // 32
                        for gi in range(3):
                            g = half * 3 + gi

                            def qk(S, kt=kt, w=w, g=g):
                                fw.op("pe", lambda: TE.matmul(S[:, :], lhsT=Kaug[:, kt * 128:(kt + 1) * 128], rhs=Rw[w][:, g, :], start=True, stop=True),
                                      reads=[Kaug, ("Kaug", "E"), ("R", w, "q"), ("R", w, "m")], writes=[S])

                            def post(S, kt=kt, gi=gi, G=G, nk=nk):
                                pb = exp_to(S)
                                if kt >= 4 * G:
                                    fw.op("dve", lambda: V.tensor_tensor(out=pb[:], in0=pb[:], in1=tri[:, kt - 4 * G, :], op=ALU.mult), reads=[pb, tri], writes=[pb])
                                fw.op("pe", lambda: TE.matmul(psA[gi][0:65, :], lhsT=Vext[:, kt, :], rhs=pb[:], start=(kt == 0), stop=(kt == nk - 1)),
                                      reads=[Vext, pb], writes=[psA[gi]])
                            steps.append((qk, post))
                    pipe(steps)
                    for gi in range(3):
                        g = half * 3 + gi
                        finalize(psA[gi], (g * 64, g * 64 + 64), False, gate_col=1 * 3 + g)
                k0 = max(0, 4 * G - 4)
                nkw = 4 * G + 4 - k0
                fw.dma("sp", Kw[:, 0:nkw * 128], FM[R_KW + 64 * kvh:R_KW + 64 * kvh + 64, k0 * 128:(k0 + nkw) * 128], writes=[Kw])
                fw.dma("sp", Vw[:, 0:nkw, 0:64], TM[k0 * 128:(k0 + nkw) * 128, C_VW + 64 * kvh:C_VW + 64 * kvh + 64].rearrange("(kt p) c -> p kt c", p=128),
                       writes=[Vw])
                for half in range(1):
                    steps = []
                    for ki in range(nkw):
                        kt = k0 + ki
                        rel = kt - 4 * G
                        for gi in range(3):
                            g = half * 3 + gi

                            def qk(S, ki=ki, g=g):
                                fw.op("pe", lambda: TE.matmul(S[:, :], lhsT=Kw[:, ki * 128:(ki + 1) * 128], rhs=R0[0:64, g, :], start=True, stop=True),
                                      reads=[Kw, ("R", 0, "q")], writes=[S])

                            def post(S, ki=ki, gi=gi, rel=rel, nkw=nkw):
                                pb = exp_to(S)
                                mk = tri[:, rel, :] if rel >= 0 else wmask[:, rel + 4, :]
                                fw.op("dve", lambda: V.tensor_tensor(out=pb[:], in0=pb[:], in1=mk, op=ALU.mult), reads=[pb, tri, wmask], writes=[pb])
                                fw.op("pe", lambda: TE.matmul(psA[gi][0:65, :], lhsT=Vw[:, ki, :], rhs=pb[:], start=(ki == 0), stop=(ki == nkw - 1)),
                                      reads=[Vw, pb], writes=[psA[gi]])
                            steps.append((qk, post))
                    pipe(steps)
                    for gi in range(3):
                        g = half * 3 + gi
                        finalize(psA[gi], (g * 64, g * 64 + 64), False, gate_col=2 * 3 + g)
                fw.op("act", lambda: A.copy(out=o_bf[:], in_=o_acc[:]), reads=[o_acc], writes=[o_bf])
                fw.dma("act", Oout[si * 512:(si + 1) * 512, 64:256].rearrange("(j p) c -> p j c", p=128), o_bf[:], reads=[o_bf])
    fw.finish("sp")
    print("D: ninst", fw.ninst, "nwait", fw.nwait, "dsems", fw.ndsem)
    return nc


def host_D_weights(d):
    return {"peT": np.ascontiguousarray(np.stack([d['cmp_pe_k'][0].T, d['cmp_pe_v'][0].T], 0)),
            "w1": np.ascontiguousarray(np.stack([d['cmp_w1_k'][0], d['cmp_w1_v'][0]], 0)),
            "w2": np.ascontiguousarray(np.stack([d['cmp_w2_k'][0], d['cmp_w2_v'][0]], 0)),
            "gk": d['nsa_kcmp_norm'][0].reshape(64, 1).astype(np.float32)}


def core_inputs(FMb, TMb, GTb, rr):
    h = rr; kvh = rr // 2; half = rr % 2
    mine = [kvh * 6 + half * 3 + i for i in range(3)]
    other = [kvh * 6 + (1 - half) * 3 + i for i in range(3)]
    rows = [np.arange(256 + 64 * h, 256 + 64 * h + 64), np.arange(64 * h, 64 * h + 64)]
    for q in mine + other:
        rows.append(np.arange(512 + 64 * q, 512 + 64 * q + 64))
    rows += [np.arange(1280 + 64 * kvh, 1280 + 64 * kvh + 64), np.arange(1408 + 64 * kvh, 1408 + 64 * kvh + 64),
             np.arange(1536 + 64 * kvh, 1536 + 64 * kvh + 64), np.arange(1664 + 64 * kvh, 1664 + 64 * kvh + 64)]
    rows = np.concatenate(rows)
    cols = np.concatenate([np.arange(64 * h, 64 * h + 64), np.arange(256 + 64 * kvh, 256 + 64 * kvh + 64), np.arange(384 + 64 * kvh, 384 + 64 * kvh + 64)])
    gcols = np.array([br * 12 + q for br in range(3) for q in mine])
    ocols = np.concatenate([np.arange(64 * h, 64 * h + 64)] + [np.arange(256 + 64 * q, 256 + 64 * q + 64) for q in mine])
    return {"FM": np.ascontiguousarray(FMb[rows]), "TM": np.ascontiguousarray(TMb[:, cols]), "GT": np.ascontiguousarray(GTb[:, gcols])}, ocols


BF = ml_dtypes.bfloat16
_CACHE = {}


def _prog(name, fn):
    if name not in _CACHE:
        _CACHE[name] = fn()
    return _CACHE[name]


def _run(nc, in_maps, n=8):
    res = run_bass_kernel_spmd(nc, in_maps, core_ids=list(range(n)))
    return res.results


def build_W():
    nc = bass.Bass("TRN2", target_bir_lowering=False)
    V, A = nc.vector, nc.scalar
    gu = nc.dram_tensor("gu", [2, 4096, 256], F32, kind="ExternalInput").ap()
    dn = nc.dram_tensor("dn", [1024, 1024], F32, kind="ExternalInput").ap()
    gu_o = nc.dram_tensor("gu_o", [2, 4096, 256], BF16, kind="ExternalOutput").ap()
    dn_o = nc.dram_tensor("dn_o", [1024, 1024], BF16, kind="ExternalOutput").ap()
    fw = FW(nc)
    for i in range(2):
        st = nc.alloc_sbuf_tensor("st%d" % i, [128, 32, 256], F32)
        ob = nc.alloc_sbuf_tensor("ob%d" % i, [128, 32, 256], BF16)
        fw.dma("sp", st[:], gu[i].rearrange("(p k) f -> p k f", p=128), writes=[st])
        fw.op("dve", lambda: V.tensor_copy(ob[:, 0:16, :], st[:, 0:16, :]), reads=[st], writes=[(ob.name, 0)])
        fw.op("act", lambda: A.copy(out=ob[:, 16:32, :], in_=st[:, 16:32, :]), reads=[st], writes=[(ob.name, 1)])
        fw.dma("sp", gu_o[i].rearrange("(p k) f -> p k f", p=128), ob[:], reads=[(ob.name, 0), (ob.name, 1)])
    st = nc.alloc_sbuf_tensor("std", [128, 8, 1024], F32)
    ob = nc.alloc_sbuf_tensor("obd", [128, 8, 1024], BF16)
    fw.dma("sp", st[:], dn.rearrange("(p k) f -> p k f", p=128), writes=[st])
    fw.op("dve", lambda: V.tensor_copy(ob[:, 0:4, :], st[:, 0:4, :]), reads=[st], writes=[(ob.name, 0)])
    fw.op("act", lambda: A.copy(out=ob[:, 4:8, :], in_=st[:, 4:8, :]), reads=[st], writes=[(ob.name, 1)])
    fw.dma("sp", dn_o.rearrange("(p k) f -> p k f", p=128), ob[:], reads=[(ob.name, 0), (ob.name, 1)])
    fw.finish("sp")
    return nc


def run_W(d):
    g = d['moe_w_gate'].reshape(8, 4096, 256)
    u = d['moe_w_up'].reshape(8, 4096, 256)
    dn = d['moe_w_down'].reshape(8, 1024, 1024)
    in_maps = [{"gu": np.ascontiguousarray(np.stack([g[c], u[c]], 0)), "dn": np.ascontiguousarray(dn[c])} for c in range(8)]
    r = _run(build_W(), in_maps)
    gb = np.stack([r[c]["gu_o"][0] for c in range(8)], 0).reshape(2, 16, 1024, 256)
    ub = np.stack([r[c]["gu_o"][1] for c in range(8)], 0).reshape(2, 16, 1024, 256)
    db = np.stack([r[c]["dn_o"] for c in range(8)], 0).reshape(2, 16, 256, 1024)
    return gb, ub, db

def kernel(**inp):
    d = {k: np.asarray(v) for k, v in inp.items()}
    x = d['x'].astype(np.float32, copy=False)
    B, Lq, D = x.shape
    Q = 4096
    in_maps = []
    for c in range(8):
        b, q = c // 4, c % 4
        xs = x[b, q * Q:(q + 1) * Q]
        halo = x[b, q * Q - 128:q * Q] if q > 0 else np.zeros((128, D), np.float32)
        in_maps.append({"xT": np.ascontiguousarray(np.concatenate([halo, xs], 0).T), "w_in": d['ev_w_in'][0],
                        "g0": np.ascontiguousarray(d['ev_norm_mix'][0].reshape(8, 128).T),
                        "cw": np.ascontiguousarray(d['conv_w'][0].reshape(3, 6, 128).transpose(2, 1, 0)),
                        "cb": np.ascontiguousarray(d['conv_b'][0].reshape(6, 128).T)})
    rA = _run(build_A(), in_maps)
    uT_full = np.concatenate([rA[c]["uT"] for c in range(8)], axis=1)
    g_ = d['moe_w_gate'].reshape(8, 4096, 256)
    u_ = d['moe_w_up'].reshape(8, 4096, 256)
    dn_ = d['moe_w_down'].reshape(8, 1024, 1024)
    in_maps = []
    for c in range(8):
        m = host_B_inputs(d, c, uT_full)
        m["gu"] = np.ascontiguousarray(np.stack([g_[c], u_[c]], 0))
        m["dn"] = np.ascontiguousarray(dn_[c])
        in_maps.append(m)
    rB = _run(build_B(), in_maps)
    wb = (np.stack([rB[c]["gu_o"][0] for c in range(8)], 0).reshape(2, 16, 1024, 256),
          np.stack([rB[c]["gu_o"][1] for c in range(8)], 0).reshape(2, 16, 1024, 256),
          np.stack([rB[c]["dn_o"] for c in range(8)], 0).reshape(2, 16, 256, 1024))
    ys5 = np.concatenate([rB[c]["yT"] for c in range(8)], axis=0)
    W0 = host_CE_weights(d, 0, wb)
    in_maps = []
    for c in range(8):
        b, q = c // 4, c % 4
        m = {"xT": np.ascontiguousarray(x[b, q * Q:(q + 1) * Q].T), "ysT": np.ascontiguousarray(ys5[:, c * Q:(c + 1) * Q]),
             "ybT": rA[c]["ybT"], "w_glu": d['s5_w_glu'][0], "w_out": d['ev_w_out'][0]}
        m.update(W0)
        in_maps.append(m)
    rC = _run(build_CE("C"), in_maps)
    h2T = [rC[c]["outT"] for c in range(8)]
    WP = host_P_weights(d)
    in_maps = []
    for c in range(8):
        m = {"hT": h2T[c]}
        m.update(WP)
        in_maps.append(m)
    rP = _run(build_P(), in_maps)
    WD = host_D_weights(d)
    in_maps = []
    ocs = []
    for b in range(2):
        FMb = np.concatenate([rP[4 * b + q]["FM"] for q in range(4)], axis=1)
        TMb = np.concatenate([rP[4 * b + q]["TM"] for q in range(4)], axis=0)
        GTb = np.concatenate([rP[4 * b + q]["GT"] for q in range(4)], axis=0)
        for rr in range(4):
            m, ocols = core_inputs(FMb, TMb, GTb, rr)
            m.update(WD)
            in_maps.append(m)
            ocs.append(ocols)
    rD = _run(build_D(), in_maps)
    o = np.zeros((2, Lq, D), BF)
    for c in range(8):
        o[c // 4][:, ocs[c]] = rD[c]["Oout"]
    W1 = host_CE_weights(d, 1, wb)
    in_maps = []
    for c in range(8):
        b, q = c // 4, c % 4
        m = {"xT": h2T[c], "oT": np.ascontiguousarray(o[b, q * Q:(q + 1) * Q].T), "w_out": d['od_w_out'][0]}
        m.update(W1)
        in_maps.append(m)
    rE = _run(build_CE("E"), in_maps)
    out = np.empty((B, Lq, D), np.float32)
    for c in range(8):
        b, q = c // 4, c % 4
        out[b, q * Q:(q + 1) * Q] = rE[c]["outT"].T
    return out
```
